# Optimizing a Trainium2 kernel written in Bass

```python
import jax
import jax.numpy as jnp
from jax import lax
import numpy as np

D_MODEL = 1024
BATCH = 4
SEQ = 4096
DEPTH = 4

GRID_W = 64
CTX_LEN = 256
N_MIXERS = 3
CHUNK = 128
A_DIM = 2 * D_MODEL
A_GROUPS = 8
N_HEADS = 16
N_KV = 4
HEAD_DIM = D_MODEL // N_HEADS
ROPE_FREQS = HEAD_DIM // 4
ROPE_THETA = 10000.0
Q_BLOCK = 128
POOL_WINDOWS = (2, 4, 8, 16)
POOL_GROUP = D_MODEL // 4
FFN_DIM = 2816
N_EXPERTS = 8
TOP_K = 2
EXPERT_DIM = 3584
EPS = 1e-6

N_A = (DEPTH + 2) // 3
N_B = (DEPTH + 1) // 3
N_C = DEPTH // 3
N_DENSE = (DEPTH + 1) // 2
N_MOE = DEPTH // 2

kernel_name = 'hybrid_gmlp_gqa_pool_moe_diffusion_trunk'


def rms_norm(x, g):
    xf = x.astype(jnp.float32)
    y = xf * lax.rsqrt(jnp.mean(xf * xf, axis=-1, keepdims=True) + EPS)
    return (y * g.astype(jnp.float32)).astype(x.dtype)


def adaln(cond, w, b):
    m = jax.nn.silu(cond) @ w + b
    m = m.reshape(m.shape[:-1] + (1, m.shape[-1]))
    if m.ndim == 2:
        m = m[None]
    return jnp.split(m, 6, axis=-1)


def modulate(x, g, shift, scale):
    return rms_norm(x, g) * (1 + scale) + shift


def swiglu(t, wg, wu, wd):
    return (jax.nn.silu(t @ wg) * (t @ wu)) @ wd


def moe_swiglu(h, router, wg, wu, wd):
    b, l, d = h.shape
    t = h.reshape(b * l, d)
    logits = (t @ router).astype(jnp.float32)
    top_v, top_i = lax.top_k(logits, TOP_K)
    gates = jax.nn.softmax(top_v, axis=-1)
    combine = jnp.sum(jax.nn.one_hot(top_i, N_EXPERTS, dtype=jnp.float32) * gates[..., None], axis=1)
    combine = combine.astype(t.dtype)
    out = jnp.zeros_like(t)
    for e in range(N_EXPERTS):
        out = out + combine[:, e:e + 1] * swiglu(t, wg[e], wu[e], wd[e])
    return out.reshape(b, l, d)


def chunk_mlp(h, w_in, v_g, w_s, b_s, w_out):
    b, l, _ = h.shape
    z = jax.nn.gelu(h @ w_in)
    u, v = jnp.split(z, 2, axis=-1)
    v = rms_norm(v, v_g).reshape(b, l // CHUNK, CHUNK, A_GROUPS, A_DIM // A_GROUPS)
    mixed = jnp.einsum('gts,bcsgd->bctgd', w_s, v) + b_s.T[:, :, None]
    return (u * mixed.reshape(b, l, A_DIM)) @ w_out


def centred_pool_minus_identity(x, w):
    b, l, ch = x.shape
    xf = x.astype(jnp.float32)
    cs = jnp.concatenate([jnp.zeros((b, 1, ch), jnp.float32), jnp.cumsum(xf, axis=1)], axis=1)
    t = jnp.arange(l)
    lo = jnp.clip(t - w // 2, 0, l - 1)
    hi = jnp.clip(t + w // 2 - 1, 0, l - 1)
    cnt = (hi - lo + 1).astype(jnp.float32)
    s = jnp.take(cs, hi + 1, axis=1) - jnp.take(cs, lo, axis=1)
    return (s / cnt[None, :, None] - xf).astype(x.dtype)


def pool_mixer(h, p_w, p_scale):
    groups = jnp.split(h, len(POOL_WINDOWS), axis=-1)
    outs = [centred_pool_minus_identity(grp, w) @ p_w[j]
            for j, (grp, w) in enumerate(zip(groups, POOL_WINDOWS))]
    return jnp.concatenate(outs, axis=-1) * p_scale


def axial_rope_tables(rows):
    r = jnp.repeat(jnp.arange(rows), GRID_W)
    col = jnp.tile(jnp.arange(GRID_W), rows)
    inv = ROPE_THETA ** (-jnp.arange(ROPE_FREQS, dtype=jnp.float32) / ROPE_FREQS)
    ang = jnp.stack([r, col], axis=-1).astype(jnp.float32)[..., None] * inv
    return jnp.cos(ang), jnp.sin(ang)


def apply_rope(x, cos, sin):
    shp = x.shape
    xr = x.astype(jnp.float32).reshape(shp[:-1] + (2, 2, ROPE_FREQS))
    x1, x2 = xr[..., 0, :], xr[..., 1, :]
    bshape = (1, shp[1]) + (1,) * (len(shp) - 3) + cos.shape[1:]
    c, s = cos.reshape(bshape), sin.reshape(bshape)
    out = jnp.stack([x1 * c - x2 * s, x2 * c + x1 * s], axis=-2)
    return out.reshape(shp).astype(x.dtype)


def blocked_attention(q, k, v):
    b, lq = q.shape[:2]
    nb = lq // Q_BLOCK
    qb = jnp.moveaxis(q.reshape((b, nb, Q_BLOCK) + q.shape[2:]), 1, 0)
    scale = HEAD_DIM ** -0.5

    def one_block(qi):
        s = jnp.einsum('bqgrd,bkgd->bgrqk', qi, k, preferred_element_type=jnp.float32) * scale
        p = jax.nn.softmax(s, axis=-1).astype(v.dtype)
        return jnp.einsum('bgrqk,bkgd->bqgrd', p, v)

    o = lax.map(one_block, qb)
    return jnp.moveaxis(o, 0, 1).reshape(b, lq, N_HEADS * HEAD_DIM)


def attention_mixer(hl, hc, w_qkv, q_g, k_g, w_o, cos, sin, ctx_out):
    nq = N_HEADS * HEAD_DIM
    rep = N_HEADS // N_KV

    def q_heads(qf):
        return rms_norm(qf.reshape(qf.shape[0], qf.shape[1], N_KV, rep, HEAD_DIM), q_g)

    def kv_heads(kvf):
        kf, vf = jnp.split(kvf, 2, axis=-1)
        kk = rms_norm(kf.reshape(kf.shape[0], kf.shape[1], N_KV, HEAD_DIM), k_g)
        return kk, vf.reshape(vf.shape[0], vf.shape[1], N_KV, HEAD_DIM)

    qkv_l = hl @ w_qkv
    ql = apply_rope(q_heads(qkv_l[..., :nq]), cos, sin)
    kl, vl = kv_heads(qkv_l[..., nq:])
    kl = apply_rope(kl, cos, sin)
    kc, vc = kv_heads(hc @ w_qkv[:, nq:])
    yl = blocked_attention(ql, jnp.concatenate([kc, kl], axis=1), jnp.concatenate([vc, vl], axis=1)) @ w_o
    yc = blocked_attention(q_heads(hc @ w_qkv[:, :nq]), kc, vc) @ w_o if ctx_out else None
    return yl, yc


def setup_inputs(seed: int = 0) -> dict:
    key = jax.random.key(seed)
    ks = jax.random.split(key, 32)
    nrm = jax.random.normal
    f32 = jnp.float32
    qkv_w = (N_HEADS + 2 * N_KV) * HEAD_DIM
    return {
        'x': nrm(ks[0], (BATCH, SEQ, D_MODEL), f32),
        'c': nrm(ks[1], (BATCH, D_MODEL), f32),
        'ctx': nrm(ks[2], (BATCH, CTX_LEN, D_MODEL), f32),
        'c_ctx': nrm(ks[3], (D_MODEL,), f32),
        'ada_w': nrm(ks[4], (DEPTH, D_MODEL, 6 * D_MODEL), f32) * (0.5 * D_MODEL ** -0.5),
        'ada_b': nrm(ks[5], (DEPTH, 6 * D_MODEL), f32) * 0.01,
        'norm_g': 1.0 + 0.02 * nrm(ks[6], (DEPTH, 2, D_MODEL), f32),
        'a_w_in': nrm(ks[7], (N_A, D_MODEL, 2 * A_DIM), f32) * D_MODEL ** -0.5,
        'a_v_g': 1.0 + 0.02 * nrm(ks[8], (N_A, A_DIM), f32),
        'a_ws': nrm(ks[9], (N_A, A_GROUPS, CHUNK, CHUNK), f32) * CHUNK ** -0.5,
        'a_bs': 1.0 + 0.02 * nrm(ks[10], (N_A, A_GROUPS, CHUNK), f32),
        'a_w_out': nrm(ks[11], (N_A, A_DIM, D_MODEL), f32) * A_DIM ** -0.5,
        'b_w_qkv': nrm(ks[12], (N_B, D_MODEL, qkv_w), f32) * D_MODEL ** -0.5,
        'b_q_g': 1.0 + 0.02 * nrm(ks[13], (N_B, HEAD_DIM), f32),
        'b_k_g': 1.0 + 0.02 * nrm(ks[14], (N_B, HEAD_DIM), f32),
        'b_w_o': nrm(ks[15], (N_B, N_HEADS * HEAD_DIM, D_MODEL), f32) * (N_HEADS * HEAD_DIM) ** -0.5,
        'p_w': nrm(ks[16], (N_C, len(POOL_WINDOWS), POOL_GROUP, POOL_GROUP), f32) * POOL_GROUP ** -0.5,
        'p_scale': 1.0 + 0.02 * nrm(ks[17], (N_C, D_MODEL), f32),
        'f_w_gate': nrm(ks[18], (N_DENSE, D_MODEL, FFN_DIM), f32) * D_MODEL ** -0.5,
        'f_w_up': nrm(ks[19], (N_DENSE, D_MODEL, FFN_DIM), f32) * D_MODEL ** -0.5,
        'f_w_down': nrm(ks[20], (N_DENSE, FFN_DIM, D_MODEL), f32) * FFN_DIM ** -0.5,
        'm_router': nrm(ks[21], (N_MOE, D_MODEL, N_EXPERTS), f32) * D_MODEL ** -0.5,
        'm_w_gate': nrm(ks[22], (N_MOE, N_EXPERTS, D_MODEL, EXPERT_DIM), f32) * D_MODEL ** -0.5,
        'm_w_up': nrm(ks[23], (N_MOE, N_EXPERTS, D_MODEL, EXPERT_DIM), f32) * D_MODEL ** -0.5,
        'm_w_down': nrm(ks[24], (N_MOE, N_EXPERTS, EXPERT_DIM, D_MODEL), f32) * EXPERT_DIM ** -0.5,
    }


def reference(x, c, ctx, c_ctx, ada_w, ada_b, norm_g, a_w_in, a_v_g, a_ws, a_bs, a_w_out,
              b_w_qkv, b_q_g, b_k_g, b_w_o, p_w, p_scale, f_w_gate, f_w_up, f_w_down,
              m_router, m_w_gate, m_w_up, m_w_down):
    ROWS = x.shape[1] // GRID_W
    cos, sin = axial_rope_tables(ROWS)
    ctx_layers = [i for i in range(DEPTH) if i % N_MIXERS == 1]
    last_ctx_read = max(ctx_layers) if ctx_layers else -1
    xc = ctx
    for i in range(DEPTH):
        kind = i % N_MIXERS
        j = i // N_MIXERS
        adv = i < last_ctx_read
        need_ctx = adv or i == last_ctx_read
        sl, scl, gl, sfl, scfl, gfl = adaln(c, ada_w[i], ada_b[i])
        hl = modulate(x, norm_g[i, 0], sl, scl)
        if need_ctx:
            sc, scc, gc, sfc, scfc, gfc = adaln(c_ctx, ada_w[i], ada_b[i])
            hc = modulate(xc, norm_g[i, 0], sc, scc)
        if kind == 0:
            yl = chunk_mlp(hl, a_w_in[j], a_v_g[j], a_ws[j], a_bs[j], a_w_out[j])
            yc = chunk_mlp(hc, a_w_in[j], a_v_g[j], a_ws[j], a_bs[j], a_w_out[j]) if adv else None
        elif kind == 1:
            yl, yc = attention_mixer(hl, hc, b_w_qkv[j], b_q_g[j], b_k_g[j], b_w_o[j], cos, sin, adv)
        else:
            yl = pool_mixer(hl, p_w[j], p_scale[j])
            yc = pool_mixer(hc, p_w[j], p_scale[j]) if adv else None
        x = x + gl * yl
        f = i // 2
        hl2 = modulate(x, norm_g[i, 1], sfl, scfl)
        if i % 2 == 0:
            x = x + gfl * swiglu(hl2, f_w_gate[f], f_w_up[f], f_w_down[f])
        else:
            x = x + gfl * moe_swiglu(hl2, m_router[f], m_w_gate[f], m_w_up[f], m_w_down[f])
        if adv:
            xc = xc + gc * yc
            hc2 = modulate(xc, norm_g[i, 1], sfc, scfc)
            if i % 2 == 0:
                xc = xc + gfc * swiglu(hc2, f_w_gate[f], f_w_up[f], f_w_down[f])
            else:
                xc = xc + gfc * moe_swiglu(hc2, m_router[f], m_w_gate[f], m_w_up[f], m_w_down[f])
    return x
```

```python
import contextlib
import numpy as np
import ml_dtypes
import concourse.bass as bass
import concourse.mybir as mybir
from concourse.bass_utils import run_bass_kernel_spmd

F32 = mybir.dt.float32
BF16 = mybir.dt.bfloat16
AF = mybir.ActivationFunctionType
ALU = mybir.AluOpType
AX = mybir.AxisListType

COMPUTE = ("pe", "act", "dve", "pool")
EPS = 1e-6
D = 1024
KD = 8
NCTX = 256
GRID_W = 64
POOL_WINDOWS = (2, 4, 8, 16)

FULL_CFG = dict(NT=2048, FFN=2816, EDIM=3584, NE=8)


class _Op:
    __slots__ = ("idx", "eng", "fn", "is_dma", "chan", "deps", "sig", "count")

    def __init__(self, idx, eng, fn, is_dma, chan):
        self.idx, self.eng, self.fn, self.is_dma, self.chan = idx, eng, fn, is_dma, chan
        self.deps = set()
        self.sig = False
        self.count = 0


class Sched:
    PHASE_ID = 0

    def __init__(self, nc):
        self.nc = nc
        self.ops = []
        self.last_w = {}
        self.readers = {}
        self.n_chan = 0
        self.auto_chan = {}

    def new_chan(self):
        self.n_chan += 1
        return self.n_chan - 1

    def _add(self, eng, fn, reads, writes, is_dma=False, chan=None):
        op = _Op(len(self.ops), eng, fn, is_dma, chan)
        for r in reads:
            w = self.last_w.get(r)
            if w is not None:
                op.deps.add(w)
        for r in writes:
            w = self.last_w.get(r)
            if w is not None:
                op.deps.add(w)
            for i in self.readers.get(r, {}).values():
                op.deps.add(i)
        for r in reads:
            self.readers.setdefault(r, {})[("dma", op.idx) if is_dma else eng] = op.idx
        for r in writes:
            self.last_w[r] = op.idx
            self.readers[r] = {}
        op.deps.discard(op.idx)
        self.ops.append(op)
        return op

    def op(self, eng, fn, reads=(), writes=()):
        return self._add(eng, fn, reads, writes)

    def dma(self, q, chan, fn, reads=(), writes=()):
        if writes:
            if writes[0] not in self.auto_chan:
                self.auto_chan[writes[0]] = self.new_chan()
            chan = self.auto_chan[writes[0]]
        else:
            chan = self.new_chan()
        return self._add(q, fn, reads, writes, is_dma=True, chan=chan)

    def emit(self):
        nc, ops = self.nc, self.ops
        if not ops:
            return
        for op in ops:
            for d in list(op.deps):
                a = ops[d]
                if (not a.is_dma) and a.eng == "pe" and op.eng == "pe" and not op.is_dma:
                    op.deps.discard(d)
                    continue
                a.sig = True
        eng_cnt = {e: 0 for e in COMPUTE}
        chan_cnt = [0] * self.n_chan
        for op in ops:
            if op.is_dma:
                chan_cnt[op.chan] += 16
                op.count = chan_cnt[op.chan]
            elif op.sig:
                eng_cnt[op.eng] += 1
                op.count = eng_cnt[op.eng]
        SEM_MAX = 30000
        Sched.PHASE_ID += 1
        pid = Sched.PHASE_ID
        eng_sems = {e: [nc.alloc_semaphore(name=f"p{pid}_{e}{i}")
                        for i in range(eng_cnt[e] // SEM_MAX + 1)] for e in COMPUTE}
        chan_sems = [nc.alloc_semaphore(name=f"p{pid}_ch{i}") for i in range(self.n_chan)]
        all_sems = [s for l in eng_sems.values() for s in l] + chan_sems
        assert all(c < 60000 for c in chan_cnt), chan_cnt
        with contextlib.ExitStack() as es:
            block = es.enter_context(nc.Block())

            def semval(a):
                if a.is_dma:
                    return chan_sems[a.chan], ("c", a.chan), a.count
                k = (a.count - 1) // SEM_MAX
                return eng_sems[a.eng][k], (a.eng, k), a.count - k * SEM_MAX

            def gen(engname):
                def body(eng):
                    waited = {}
                    for op in ops:
                        if op.eng != engname:
                            continue
                        need = {}
                        for d in op.deps:
                            s, key, v = semval(ops[d])
                            if waited.get(key, 0) >= v:
                                continue
                            if need.get(key, (None, 0))[1] < v:
                                need[key] = (s, v)
                        for key, (s, v) in need.items():
                            eng.wait_ge(s, v)
                            waited[key] = v
                        ins = op.fn(eng)
                        if op.is_dma:
                            ins.then_inc(semval(op)[0], 16)
                        elif op.sig:
                            ins.then_inc(semval(op)[0], 1)
                    finals = set(op.chan for op in ops if op.is_dma and op.eng == engname)
                    for c in finals:
                        if waited.get(("c", c), 0) < chan_cnt[c]:
                            eng.wait_ge(chan_sems[c], chan_cnt[c])
                return body

            used = {op.eng for op in ops}
            if "pe" in used:
                block.tensor(gen("pe"))
            if "act" in used:
                block.scalar(gen("act"))
            if "dve" in used:
                block.vector(gen("dve"))
            if "pool" in used:
                block.gpsimd(gen("pool"))
            if "sp" in used:
                block.sync(gen("sp"))
        nc.clear_and_free_semaphores(all_sems)
        nc.all_engine_barrier()


def _fslices(nchunks, maxn=4):
    out, f = [], 0
    while f < nchunks:
        n = min(maxn, nchunks - f)
        out.append((f, n))
        f += n
    return out


class Builder:
    def __init__(self, cfg, mode):
        self.cfg, self.mode = cfg, mode
        self.NT = cfg["NT"]
        self.NTOT = self.NT + NCTX
        self.nc = bass.Bass("TRN2", target_bir_lowering=False)
        self.es = contextlib.ExitStack()
        self.dram = {}
        self.input_names = set()
        self.uid = 0

    def din(self, name, shape, dt=F32):
        if name not in self.dram:
            self.dram[name] = self.nc.dram_tensor(name, list(shape), dt, kind="ExternalInput").ap()
            self.input_names.add(name)
        return self.dram[name]

    def dout(self, name, shape, dt=F32):
        self.dram[name] = self.nc.dram_tensor(name, list(shape), dt, kind="ExternalOutput").ap()
        return self.dram[name]

    def dint(self, name, shape, dt=F32):
        self.dram[name] = self.nc.dram_tensor(name, list(shape), dt).ap()
        return self.dram[name]

    def sb(self, es, name, shape, dt):
        self.uid += 1
        return es.enter_context(self.nc.sbuf_tensor(f"{name}_{self.uid}", list(shape), dt))

    @contextlib.contextmanager
    def phase(self):
        S = Sched(self.nc)
        with contextlib.ExitStack() as es:
            yield S, (lambda name, shape, dt: self.sb(es, name, shape, dt))
            S.emit()

    def modc(self, layer, slot, k, which):
        li = self.layers.index(layer)
        return self.MODC[:, li, slot * 8 + k, which:which + 1]

    def build(self):
        nc, cfg, mode, NT = self.nc, self.cfg, self.mode, self.NT
        es = self.es
        self.layers = {"A": [0, 1], "B": [1, 2], "C": [2, 3], "F": [0, 1, 2, 3]}[mode]
        self.X = self.sb(es, "X", [128, KD, NT], F32)
        self.H = self.sb(es, "H", [128, KD, self.NTOT], BF16)
        self.MODC = self.sb(es, "MODC", [128, len(self.layers), 48, 2], F32)
        self.ident = self.sb(es, "ident", [128, 128], F32)
        self.ones_f = self.sb(es, "ones_f", [128, 128], F32)
        self.ones_b = self.sb(es, "ones_b", [128, 128], BF16)
        self.one_b = self.sb(es, "one_b", [128, 128], BF16)
        self.epsc = self.sb(es, "epsc", [128, 1], F32)
        self.ps = [es.enter_context(nc.psum_tensor(f"ps{i}", [128, 512], F32)) for i in range(8)]
        self.LOG = self.sb(es, "LOG", [128, NT // 128, cfg["NE"]], F32)
        self.CMB = self.sb(es, "CMB", [128, NT // 128, cfg["NE"]], F32)

        if mode == "F":
            self.kT_mine = self.dint("kT_mine", [256, NT], BF16)
            self.v_mine = self.dint("v_mine", [NT, 256], BF16)
            self.kcT = self.dint("kcT", [256, NCTX], BF16)
            self.vc = self.dint("vc", [NCTX, 256], BF16)
            self.kT_all = self.dint("kT_all", [512, NT], BF16)
            self.v_all = self.dint("v_all", [2 * NT, 256], BF16)
            self.dint("halo_mine", [D, 16])
            self.halo_all = self.dint("halo_all", [2 * D, 16])
        self.prologue()
        if mode in ("A", "F"):
            with contextlib.ExitStack() as es1:
                self.XC = self.sb(es1, "XC", [128, KD, NCTX], F32)
                self.load_stream(self.X, self.din("xT", [D, NT]), NT)
                self.load_stream(self.XC, self.din("ctxT", [D, NCTX]), NCTX)
                lat = [(t0, 512, "X") for t0 in range(0, NT, 512)]
                ctx = [(NT, NCTX, "XC")]
                self.norm(0, 0, lat + ctx)
                if self.dbg("norm00"):
                    return self.finish()
                self.gmlp(0, 0, lat + ctx)
                if self.dbg("gmlp0") or self.cfg.get("stop") in ("gmlp_dbg", "gmlp_dbg2"):
                    return self.finish()
                self.norm(0, 1, lat + ctx)
                self.ffn(0, lat + ctx, [(self.din("f_w_gate_0", [D, cfg["FFN"]]), self.din("f_w_up_0", [D, cfg["FFN"]]),
                                         self.din("f_w_down_0", [cfg["FFN"], D]))], cfg["FFN"])
                self.norm(1, 0, lat + ctx)
                if mode == "A":
                    self.kT_mine = self.dout("kT_mine", [256, NT], BF16)
                    self.v_mine = self.dout("v_mine", [NT, 256], BF16)
                    self.kcT = self.dout("kcT", [256, NCTX], BF16)
                    self.vc = self.dout("vc", [NCTX, 256], BF16)
                    self.qkv(want_q=False)
                    self.store_stream(self.X, self.dout("xT_out", [D, NT]), NT)
                    return self.finish()
            lat = [(t0, 512, "X") for t0 in range(0, NT, 512)]
            es2 = contextlib.ExitStack()
            self.QT = self.sb(es2, "QT", [128, KD, NT], BF16)
            self.qkv(want_q=True, want_kv=True, nbuf=1)
            self.allgather([(self.kT_mine, self.kT_all), (self.v_mine, self.v_all)])
            if self.cfg.get("ext_scratch", False):
                with self.phase() as (S, sb):
                    new = {}
                    for nm, T in [("kT_all", self.kT_all), ("v_all", self.v_all), ("kcT", self.kcT), ("vc", self.vc)]:
                        dd = self.dout("o_" + nm, list(T.shape), BF16)
                        S.dma("sp", None, lambda e, dd=dd, T=T: e.dma_start(out=dd, in_=T))
                        new[nm] = dd
                self.kT_all, self.v_all, self.kcT, self.vc = new["kT_all"], new["v_all"], new["kcT"], new["vc"]
            if self.cfg.get("stop") == "f1d":
                with self.phase() as (S, sb):
                    for nm, T in [("o_kT_all", self.kT_all), ("o_v_all", self.v_all), ("o_kcT", self.kcT), ("o_vc", self.vc)]:
                        dd = self.dout(nm, list(T.shape), BF16)
                        S.dma("sp", None, lambda e, dd=dd, T=T: e.dma_start(out=dd, in_=T))
                self.store_stream(self.X, self.dout("yT", [D, NT]), NT)
                return self.finish()
            if self.cfg.get("stop") == "f1":
                self.store_stream(self.X, self.dout("yT", [D, NT]), NT)
                return self.finish()
            with es2:
                if self.cfg.get("stop") == "f1b":
                    self.dump("dbg_q", self.QT[:].rearrange("p k n -> p (k n)"), [128, KD * NT], BF16)
                    self.dump("dbg_h", self.H[:].rearrange("p k n -> p (k n)"), [128, KD * self.NTOT], BF16)
                    self.store_stream(self.X, self.dout("yT", [D, NT]), NT)
                    return self.finish()
                self.attention()
                if self.cfg.get("stop") == "f2a":
                    self.dump("dbg_h", self.H[:].rearrange("p k n -> p (k n)"), [128, KD * self.NTOT], BF16)
                    self.dump("dbg_q", self.QT[:].rearrange("p k n -> p (k n)"), [128, KD * NT], BF16)
                    self.store_stream(self.X, self.dout("yT", [D, NT]), NT)
                    return self.finish()
            self.oproj()
            if self.cfg.get("stop") == "f2":
                self.store_stream(self.X, self.dout("yT", [D, NT]), NT)
                return self.finish()
            self.norm(1, 1, lat, router=self.din("m_router_0", [D, cfg["NE"]]))
            self.ffn(1, lat, [(self.din("m_w_gate_0", [cfg["NE"], D, cfg["EDIM"]])[e],
                               self.din("m_w_up_0", [cfg["NE"], D, cfg["EDIM"]])[e],
                               self.din("m_w_down_0", [cfg["NE"], cfg["EDIM"], D])[e]) for e in range(cfg["NE"])],
                     cfg["EDIM"], moe=True)
            if self.cfg.get("stop") == "f3":
                self.store_stream(self.X, self.dout("yT", [D, NT]), NT)
                return self.finish()
            self.pool_norm_halo()
            self.allgather([(self.dram["halo_mine"], self.halo_all)])
            if self.cfg.get("stop") == "f4":
                self.store_stream(self.X, self.dout("yT", [D, NT]), NT)
                return self.finish()
            self.pool_mixer()
            self.norm(2, 1, lat)
            self.ffn(2, lat, [(self.din("f_w_gate_1", [D, cfg["FFN"]]), self.din("f_w_up_1", [D, cfg["FFN"]]),
                               self.din("f_w_down_1", [cfg["FFN"], D]))], cfg["FFN"])
            if self.cfg.get("stop") == "f5":
                self.store_stream(self.X, self.dout("yT", [D, NT]), NT)
                return self.finish()
            self.norm(3, 0, lat)
            self.gmlp(3, 1, lat)
            self.norm(3, 1, lat, router=self.din("m_router_1", [D, cfg["NE"]]))
            self.ffn(3, lat, [(self.din("m_w_gate_1", [cfg["NE"], D, cfg["EDIM"]])[e],
                               self.din("m_w_up_1", [cfg["NE"], D, cfg["EDIM"]])[e],
                               self.din("m_w_down_1", [cfg["NE"], cfg["EDIM"], D])[e]) for e in range(cfg["NE"])],
                     cfg["EDIM"], moe=True)
            self.store_stream(self.X, self.dout("yT", [D, NT]), NT)
            return self.finish()
        if mode == "B":
            with contextlib.ExitStack() as es1:
                self.load_stream(self.X, self.din("xT", [D, NT]), NT)
                lat = [(t0, 512, "X") for t0 in range(0, NT, 512)]
                self.norm(1, 0, lat)
                self.kT_all = self.din("kT_all", [512, NT], BF16)
                self.v_all = self.din("v_all", [2 * NT, 256], BF16)
                self.kcT = self.din("kcT", [256, NCTX], BF16)
                self.vc = self.din("vc", [NCTX, 256], BF16)
                self.QT = self.sb(es1, "QT", [128, KD, NT], BF16)
                self.qkv(want_q=True, want_kv=False)
                self.attention()
            self.oproj()
            if self.cfg.get("stop") == "attn":
                self.store_stream(self.X, self.dout("xT_out", [D, NT]), NT)
                return self.finish()
            self.norm(1, 1, lat, router=self.din("m_router_0", [D, cfg["NE"]]))
            if self.cfg.get("stop") == "route":
                self.store_stream(self.X, self.dout("xT_out", [D, NT]), NT)
                self.dump("d_LOG", self.LOG[:].rearrange("p c e -> p (c e)"), [128, (NT // 128) * cfg["NE"]])
                self.dump("d_CMB", self.CMB[:].rearrange("p c e -> p (c e)"), [128, (NT // 128) * cfg["NE"]])
                self.dump("dbg_h", self.H[:].rearrange("p k n -> p (k n)"), [128, KD * self.NTOT], BF16)
                return self.finish()
            self.ffn(1, lat, [(self.din("m_w_gate_0", [cfg["NE"], D, cfg["EDIM"]])[e],
                               self.din("m_w_up_0", [cfg["NE"], D, cfg["EDIM"]])[e],
                               self.din("m_w_down_0", [cfg["NE"], cfg["EDIM"], D])[e]) for e in range(cfg["NE"])],
                     cfg["EDIM"], moe=True)
            self.store_stream(self.X, self.dout("xT_out", [D, NT]), NT)
            self.pool_norm_halo(store_only=True)
            return self.finish()
        if mode == "C":
            self.load_stream(self.X, self.din("xT", [D, NT]), NT)
            lat = [(t0, 512, "X") for t0 in range(0, NT, 512)]
            if self.cfg.get("stop") == "f4":
                self.store_stream(self.X, self.dout("yT", [D, NT]), NT)
                return self.finish()
            self.pool_mixer()
            self.norm(2, 1, lat)
            self.ffn(2, lat, [(self.din("f_w_gate_1", [D, cfg["FFN"]]), self.din("f_w_up_1", [D, cfg["FFN"]]),
                               self.din("f_w_down_1", [cfg["FFN"], D]))], cfg["FFN"])
            if self.cfg.get("stop") == "f5":
                self.store_stream(self.X, self.dout("yT", [D, NT]), NT)
                return self.finish()
            self.norm(3, 0, lat)
            self.gmlp(3, 1, lat)
            self.norm(3, 1, lat, router=self.din("m_router_1", [D, cfg["NE"]]))
            self.ffn(3, lat, [(self.din("m_w_gate_1", [cfg["NE"], D, cfg["EDIM"]])[e],
                               self.din("m_w_up_1", [cfg["NE"], D, cfg["EDIM"]])[e],
                               self.din("m_w_down_1", [cfg["NE"], cfg["EDIM"], D])[e]) for e in range(cfg["NE"])],
                     cfg["EDIM"], moe=True)
            self.store_stream(self.X, self.dout("yT", [D, NT]), NT)
            return self.finish()

    def finish(self):
        return self.nc

    def allgather(self, pairs):
        nc = self.nc
        if self.cfg.get("fake_gather"):
            with self.phase() as (S, sb):
                for src, dst in pairs:
                    n = src.shape[0]
                    for r in range(2):
                        S.dma("sp", None, lambda e, src=src, dst=dst, r=r, n=n: e.dma_start(out=dst[r * n:(r + 1) * n, :], in_=src))
            return
        with contextlib.ExitStack() as es:
            sems = [self.es.enter_context(nc.semaphore(f"cc{self.uid}_{i}")) for i in range(len(pairs))]
            self.uid += 1
            blk = es.enter_context(nc.Block())

            def body(g):
                for (src, dst), s in zip(pairs, sems):
                    g.collective_compute("AllGather", ALU.bypass, replica_groups=[[0, 1], [2, 3], [4, 5], [6, 7]],
                                         ins=[src], outs=[dst]).then_inc(s)
                for s in sems:
                    g.wait_ge(s, 1)
            blk.gpsimd(body)

    def dump(self, name, T, shape, dt=F32):
        d = self.dout(name, shape, dt)
        with self.phase() as (S, sb):
            c = S.new_chan()
            S.dma("sp", c, lambda e: e.dma_start(out=d, in_=T))

    def dbg(self, tag):
        if self.cfg.get("stop") != tag:
            return False
        NT = self.NT
        self.dump("dbg_modc", self.MODC[:].rearrange("p l c w -> p (l c w)"), [128, len(self.layers) * 96])
        self.dump("dbg_h", self.H[:].rearrange("p k n -> p (k n)"), [128, KD * self.NTOT], BF16)
        self.dump("dbg_x", self.X[:].rearrange("p k n -> p (k n)"), [128, KD * NT])
        if hasattr(self, "XC"):
            self.dump("dbg_xc", self.XC[:].rearrange("p k n -> p (k n)"), [128, KD * NCTX])
        return True


    def load_stream(self, T, src, n):
        with self.phase() as (S, sb):
            c = S.new_chan()
            S.dma("sp", c, lambda e: e.dma_start(out=T[:, :, 0:n], in_=src.rearrange("(k p) n -> p k n", p=128)),
                  writes=["T"])

    def store_stream(self, T, dst, n):
        with self.phase() as (S, sb):
            c = S.new_chan()
            S.dma("sp", c, lambda e: e.dma_start(out=dst.rearrange("(k p) n -> p k n", p=128), in_=T[:, :, 0:n]),
                  reads=["T"])

    def prologue(self):
        nc = self.nc
        crow = self.din("crow", [2, D])
        ident_d = self.din("ident", [128, 128])
        ps = self.ps
        with self.phase() as (S, sb):
            c0 = S.new_chan()
            S.dma("sp", c0, lambda e: e.dma_start(out=self.ident[:], in_=ident_d), writes=["ident"])
            S.op("dve", lambda e: e.memset(self.ones_f[:], 1.0), writes=["ones_f"])
            S.op("dve", lambda e: e.memset(self.ones_b[:], 1.0 / D), writes=["ones_b"])
            S.op("dve", lambda e: e.memset(self.one_b[:], 1.0), writes=["one_b"])
            S.op("dve", lambda e: e.memset(self.epsc[:], EPS), writes=["epsc"])
            crow_sb = sb("crow", [2, D], F32)
            srow = sb("srow", [2, D], F32)
            scol = sb("scol", [128, KD, 2], F32)
            S.dma("sp", c0, lambda e: e.dma_start(out=crow_sb[:], in_=crow), writes=["crow"])
            S.op("act", lambda e: e.activation(out=srow[:], in_=crow_sb[:], func=AF.Silu), reads=["crow"], writes=["srow"])
            for k in range(KD):
                S.op("pe", lambda e, k=k: e.matmul(ps[0][:, 2 * k:2 * k + 2], lhsT=srow[0:2, k * 128:(k + 1) * 128],
                                                   rhs=self.ident[0:2, 0:2], start=True, stop=True),
                     reads=["srow", "ident"], writes=[("ps", 0)])
            S.op("dve", lambda e: e.tensor_copy(out=scol[:].rearrange("p k w -> p (k w)"), in_=ps[0][:, 0:16]),
                 reads=[("ps", 0)], writes=["scol"])
            wb = [sb(f"adaw{i}", [128, KD, 512], F32) for i in range(2)]
            R = sb("R", [2, 6 * D], F32)
            adab = sb("adab", [2, 6 * D], F32)
            g2 = sb("g2", [2, 2, D], F32)
            cnt = 0
            for li, layer in enumerate(self.layers):
                adaw = self.din(f"ada_w_{layer}", [D, 6 * D])
                adab_d = self.din(f"ada_b_{layer}", [1, 6 * D])
                ng_d = self.din(f"norm_g_{layer}", [1, 2 * D])
                for r in range(2):
                    S.dma("sp", c0, lambda e, r=r, adab_d=adab_d: e.dma_start(out=adab[r:r + 1, :], in_=adab_d), writes=["adab"])
                    S.dma("sp", c0, lambda e, r=r, ng_d=ng_d: e.dma_start(out=g2[r:r + 1, :, :].rearrange("p a d -> p (a d)"), in_=ng_d),
                          writes=["g2"])
                for j in range(12):
                    b = cnt % 2
                    cnt += 1
                    S.dma("sp", None, lambda e, b=b, j=j, adaw=adaw: e.dma_start(
                        out=wb[b][:], in_=adaw[:, j * 512:(j + 1) * 512].rearrange("(k p) n -> p k n", p=128)), writes=[("wb", b)])
                    pb = 1 + (j % 2)
                    for k in range(KD):
                        S.op("pe", lambda e, b=b, k=k, pb=pb: e.matmul(ps[pb][0:2, :], lhsT=scol[:, k, :], rhs=wb[b][:, k, :],
                                                                      start=(k == 0), stop=(k == KD - 1)),
                             reads=["scol", ("wb", b)], writes=[("ps", pb)])
                    S.op("dve", lambda e, j=j, pb=pb: e.tensor_tensor(out=R[:, j * 512:(j + 1) * 512], in0=ps[pb][0:2, :],
                                                                      in1=adab[:, j * 512:(j + 1) * 512], op=ALU.add),
                         reads=[("ps", pb), "adab"], writes=["R"])
                for s in range(2):
                    sl = slice((3 * s + 1) * D, (3 * s + 2) * D)
                    S.op("dve", lambda e, s=s, sl=sl: e.scalar_tensor_tensor(out=R[:, sl], in0=R[:, sl], scalar=1.0, in1=g2[:, s, :],
                                                                             op0=ALU.add, op1=ALU.mult),
                         reads=["R", "g2"], writes=["R"])
                for c in range(48):
                    S.op("pe", lambda e, c=c: e.matmul(ps[3][:, 2 * c:2 * c + 2], lhsT=R[0:2, c * 128:(c + 1) * 128],
                                                       rhs=self.ident[0:2, 0:2], start=True, stop=True),
                         reads=["R", "ident"], writes=[("ps", 3)])
                S.op("dve", lambda e, li=li: e.tensor_copy(out=self.MODC[:, li, :, :].rearrange("p c w -> p (c w)"), in_=ps[3][:, 0:96]),
                     reads=[("ps", 3)], writes=["MODC"])

    def norm(self, layer, sub, tiles, router=None, out32=None, out32_off=0):
        ps = self.ps
        NE = self.cfg["NE"]
        with self.phase() as (S, sb):
            sq = [sb(f"sq{i}", [128, KD, 512], BF16) for i in range(2)]
            rstd = [sb(f"rstd{i}", [128, 512], F32) for i in range(2)]
            tt = [sb(f"tt{i}", [128, KD, 512], F32) for i in range(2)]
            if router is not None:
                RW = sb("RW", [128, KD, NE], F32)
                h32 = [sb(f"h32{i}", [128, KD, 512], F32) for i in range(2)]
                c0 = S.new_chan()
                S.dma("sp", c0, lambda e: e.dma_start(out=RW[:], in_=router.rearrange("(k p) e -> p k e", p=128)), writes=["RW"])
            for ti, (t0, n, st) in enumerate(tiles):
                b = ti % 2
                which = 1 if st == "XC" else 0
                src = self.XC if st == "XC" else self.X
                s0 = t0 - self.NT if st == "XC" else t0
                S.op("act", lambda e, b=b, src=src, s0=s0, n=n: e.activation(out=sq[b][:, :, 0:n], in_=src[:, :, s0:s0 + n], func=AF.Square),
                     reads=[("X", st, s0)], writes=[("sq", b)])
                for k in range(KD):
                    S.op("pe", lambda e, b=b, k=k, n=n: e.matmul(ps[b][:, 0:n], lhsT=self.ones_b[:], rhs=sq[b][:, k, 0:n],
                                                                 start=(k == 0), stop=(k == KD - 1)),
                         reads=[("sq", b)], writes=[("ps", b)])
                S.op("act", lambda e, b=b, n=n: e.activation(out=rstd[b][:, 0:n], in_=ps[b][:, 0:n], func=AF.Sqrt, bias=self.epsc[:, 0:1], scale=1.0),
                     reads=[("ps", b)], writes=[("rstd", b)])
                S.op("dve", lambda e, b=b, n=n: e.reciprocal(out=rstd[b][:, 0:n], in_=rstd[b][:, 0:n]),
                     reads=[("rstd", b)], writes=[("rstd", b)])
                for k in range(KD):
                    S.op("dve", lambda e, b=b, k=k, n=n, src=src, s0=s0, which=which: e.scalar_tensor_tensor(
                        out=tt[b][:, k, 0:n], in0=src[:, k, s0:s0 + n], scalar=self.modc(layer, 3 * sub + 1, k, which),
                        in1=rstd[b][:, 0:n], op0=ALU.mult, op1=ALU.mult),
                        reads=[("X", st, s0), ("rstd", b)], writes=[("tt", b, k)])
                    if out32 is not None:
                        S.op("act", lambda e, b=b, k=k, n=n, t0=t0, which=which: e.activation(
                            out=out32[:, k, out32_off + t0:out32_off + t0 + n], in_=tt[b][:, k, 0:n], func=AF.Identity,
                            bias=self.modc(layer, 3 * sub, k, which), scale=1.0),
                            reads=[("tt", b, k)], writes=[("H32P", t0)])
                        continue
                    S.op("act", lambda e, b=b, k=k, n=n, t0=t0, which=which: e.activation(
                        out=self.H[:, k, t0:t0 + n], in_=tt[b][:, k, 0:n], func=AF.Identity,
                        bias=self.modc(layer, 3 * sub, k, which), scale=1.0),
                        reads=[("tt", b, k)], writes=[("H", t0)])
                    if router is not None:
                        S.op("dve", lambda e, b=b, k=k, n=n, which=which: e.tensor_scalar(
                            out=h32[b][:, k, 0:n], in0=tt[b][:, k, 0:n], scalar1=self.modc(layer, 3 * sub, k, which), scalar2=None,
                            op0=ALU.add),
                            reads=[("tt", b, k)], writes=[("h32", b)])
                if router is not None:
                    for c in range(n // 128):
                        ch = t0 // 128 + c
                        pb = 2 + (ch % 2)
                        for k in range(KD):
                            S.op("pe", lambda e, b=b, k=k, c=c, pb=pb: e.matmul(ps[pb][:, 0:NE], lhsT=h32[b][:, k, c * 128:(c + 1) * 128],
                                                                               rhs=RW[:, k, :], start=(k == 0), stop=(k == KD - 1)),
                                 reads=[("h32", b), "RW"], writes=[("ps", pb)])
                        S.op("act", lambda e, ch=ch, pb=pb: e.activation(out=self.LOG[:, ch, :], in_=ps[pb][:, 0:NE], func=AF.Copy),
                             reads=[("ps", pb)], writes=["LOG"])
            if router is not None:
                self.route(S, sb)

    def route(self, S, sb):
        NE = self.cfg["NE"]
        nch = self.NT // 128
        LOG = self.LOG
        CMB = self.CMB
        v1 = sb("v1", [128, nch], F32)
        v2 = sb("v2", [128, nch], F32)
        m1 = sb("m1", [128, nch, NE], F32)
        m2 = sb("m2", [128, nch, NE], F32)
        L2 = sb("L2", [128, nch, NE], F32)
        dd = sb("dd", [128, nch], F32)
        e2 = sb("e2", [128, nch], F32)
        g1 = sb("g1", [128, nch], F32)
        g2 = sb("g2", [128, nch], F32)

        def bc(t):
            return t[:].unsqueeze(2).broadcast_to([128, nch, NE])
        S.op("dve", lambda e: e.tensor_reduce(out=v1[:], in_=LOG[:], axis=AX.X, op=ALU.max), reads=["LOG"], writes=["v1"])
        S.op("dve", lambda e: e.tensor_tensor(out=m1[:], in0=LOG[:], in1=bc(v1), op=ALU.is_equal), reads=["LOG", "v1"], writes=["m1"])
        S.op("dve", lambda e: e.scalar_tensor_tensor(out=L2[:], in0=m1[:], scalar=-1e30, in1=LOG[:], op0=ALU.mult, op1=ALU.add),
             reads=["m1", "LOG"], writes=["L2"])
        S.op("dve", lambda e: e.tensor_reduce(out=v2[:], in_=L2[:], axis=AX.X, op=ALU.max), reads=["L2"], writes=["v2"])
        S.op("dve", lambda e: e.tensor_tensor(out=m2[:], in0=L2[:], in1=bc(v2), op=ALU.is_equal), reads=["L2", "v2"], writes=["m2"])
        S.op("dve", lambda e: e.tensor_tensor(out=dd[:], in0=v2[:], in1=v1[:], op=ALU.subtract), reads=["v1", "v2"], writes=["dd"])
        S.op("act", lambda e: e.activation(out=e2[:], in_=dd[:], func=AF.Exp), reads=["dd"], writes=["e2"])
        S.op("dve", lambda e: e.tensor_scalar(out=g2[:], in0=e2[:], scalar1=1.0, scalar2=None, op0=ALU.add), reads=["e2"], writes=["g2"])
        S.op("dve", lambda e: e.reciprocal(out=g1[:], in_=g2[:]), reads=["g2"], writes=["g1"])
        S.op("dve", lambda e: e.tensor_tensor(out=g2[:], in0=e2[:], in1=g1[:], op=ALU.mult), reads=["e2", "g1"], writes=["g2"])
        S.op("dve", lambda e: e.tensor_tensor(out=m1[:], in0=m1[:], in1=bc(g1), op=ALU.mult), reads=["m1", "g1"], writes=["m1"])
        S.op("dve", lambda e: e.tensor_tensor(out=m2[:], in0=m2[:], in1=bc(g2), op=ALU.mult), reads=["m2", "g2"], writes=["m2"])
        S.op("dve", lambda e: e.tensor_tensor(out=CMB[:], in0=m1[:], in1=m2[:], op=ALU.add), reads=["m1", "m2"], writes=["CMB"])

    def ffn(self, layer, tiles, experts, F, moe=False):
        ps = self.ps
        NT = self.NT
        nfc = F // 128
        slices = _fslices(nfc)
        with self.phase() as (S, sb):
            WG = [sb(f"WG{i}", [128, KD, 512], BF16) for i in range(3)]
            WU = [sb(f"WU{i}", [128, KD, 512], BF16) for i in range(3)]
            WD = [sb(f"WD{i}", [128, 4, D], BF16) for i in range(3)]
            A = [sb(f"A{i}", [128, 4, 512], BF16) for i in range(2)]
            sg = [sb(f"sg{i}", [128, 512], BF16 if not moe else F32) for i in range(2)]
            wch = [S.new_chan() for _ in range(3)]
            if moe:
                BE = [sb(f"BE{i}", [128, NT], F32) for i in range(2)]
                Dg = [sb(f"Dg{i}", [128, 128], F32) for i in range(2)]
            work = [(e, s) for e in range(len(experts)) for s in range(len(slices))]

            def load(q):
                e, s = work[q]
                f0, nf = slices[s]
                wg, wu, wd = experts[e]
                r = q % 3
                S.dma("pool", wch[r], lambda en: en.dma_start(
                    out=WG[r][:, :, 0:nf * 128], in_=wg[:, f0 * 128:(f0 + nf) * 128].rearrange("(k p) n -> p k n", p=128)),
                    writes=[("WG", r)])
                S.dma("pool", wch[r], lambda en: en.dma_start(
                    out=WU[r][:, :, 0:nf * 128], in_=wu[:, f0 * 128:(f0 + nf) * 128].rearrange("(k p) n -> p k n", p=128)),
                    writes=[("WU", r)])
                S.dma("pool", wch[r], lambda en: en.dma_start(
                    out=WD[r][:, 0:nf, :], in_=wd[f0 * 128:(f0 + nf) * 128, :].rearrange("(c p) n -> p c n", p=128)),
                    writes=[("WD", r)])

            def build_be(e):
                eb = e % 2
                for ch in range(NT // 128):
                    db = ch % 2
                    pb = 6 + ((ch // 4) % 2)
                    S.op("dve", lambda en, ch=ch, db=db, e=e: en.tensor_scalar(
                        out=Dg[db][:], in0=self.ident[:], scalar1=self.CMB[:, ch, e:e + 1], scalar2=None, op0=ALU.mult),
                        reads=["CMB", "ident"], writes=[("Dg", db)])
                    S.op("pe", lambda en, ch=ch, db=db, pb=pb: en.matmul(ps[pb][:, (ch % 4) * 128:(ch % 4 + 1) * 128], lhsT=self.ones_f[:],
                                                                        rhs=Dg[db][:], start=True, stop=True),
                         reads=[("Dg", db)], writes=[("ps", pb)])
                    if ch % 4 == 3:
                        t0 = (ch // 4) * 512
                        S.op("act", lambda en, eb=eb, pb=pb, t0=t0: en.activation(out=BE[eb][:, t0:t0 + 512], in_=ps[pb][:], func=AF.Copy),
                             reads=[("ps", pb)], writes=[("BE", eb, t0)])

            items = [(q, ti) for q in range(len(work)) for ti in range(len(tiles))]
            cnt = {"gu": 0}

            def gu(idx):
                q, ti = items[idx]
                e, s = work[q]
                f0, nf = slices[s]
                t0, n, st = tiles[ti]
                r = q % 3
                ab = idx % 2
                for fc in range(nf):
                    j = cnt["gu"] % 2
                    cnt["gu"] += 1
                    for k in range(KD):
                        S.op("pe", lambda en, r=r, fc=fc, k=k, j=j, t0=t0, n=n: en.matmul(
                            ps[j][:, 0:n], lhsT=WG[r][:, k, fc * 128:(fc + 1) * 128], rhs=self.H[:, k, t0:t0 + n],
                            start=(k == 0), stop=(k == KD - 1)), reads=[("WG", r), ("H", t0)], writes=[("ps", j)])
                    for k in range(KD):
                        S.op("pe", lambda en, r=r, fc=fc, k=k, j=j, t0=t0, n=n: en.matmul(
                            ps[2 + j][:, 0:n], lhsT=WU[r][:, k, fc * 128:(fc + 1) * 128], rhs=self.H[:, k, t0:t0 + n],
                            start=(k == 0), stop=(k == KD - 1)), reads=[("WU", r), ("H", t0)], writes=[("ps", 2 + j)])
                    S.op("act", lambda en, j=j, n=n: en.activation(out=sg[j][:, 0:n], in_=ps[j][:, 0:n], func=AF.Silu),
                         reads=[("ps", j)], writes=[("sg", j)])
                    if not moe:
                        S.op("dve", lambda en, j=j, n=n, ab=ab, fc=fc: en.tensor_tensor(
                            out=A[ab][:, fc, 0:n], in0=sg[j][:, 0:n], in1=ps[2 + j][:, 0:n], op=ALU.mult),
                            reads=[("sg", j), ("ps", 2 + j)], writes=[("A", ab)])
                    else:
                        S.op("dve", lambda en, j=j, n=n: en.tensor_tensor(
                            out=sg[j][:, 0:n], in0=sg[j][:, 0:n], in1=ps[2 + j][:, 0:n], op=ALU.mult),
                            reads=[("sg", j), ("ps", 2 + j)], writes=[("sg", j)])
                        S.op("dve", lambda en, j=j, n=n, ab=ab, fc=fc, e=e, t0=t0: en.tensor_tensor(
                            out=A[ab][:, fc, 0:n], in0=sg[j][:, 0:n], in1=BE[e % 2][:, t0:t0 + n], op=ALU.mult),
                            reads=[("sg", j), ("BE", e % 2, t0)], writes=[("A", ab)])

            def down(idx):
                q, ti = items[idx]
                e, s = work[q]
                f0, nf = slices[s]
                t0, n, st = tiles[ti]
                r = q % 3
                ab = idx % 2
                which = 1 if st == "XC" else 0
                dst = self.XC if st == "XC" else self.X
                s0 = t0 - NT if st == "XC" else t0
                for dc in range(KD):
                    pb = 4 + (dc % 2)
                    for fc in range(nf):
                        S.op("pe", lambda en, r=r, fc=fc, dc=dc, pb=pb, ab=ab, n=n, nf=nf: en.matmul(
                            ps[pb][:, 0:n], lhsT=WD[r][:, fc, dc * 128:(dc + 1) * 128], rhs=A[ab][:, fc, 0:n],
                            start=(fc == 0), stop=(fc == nf - 1)), reads=[("WD", r), ("A", ab)], writes=[("ps", pb)])
                    S.op("dve", lambda en, dc=dc, pb=pb, n=n, dst=dst, s0=s0, which=which: en.scalar_tensor_tensor(
                        out=dst[:, dc, s0:s0 + n], in0=ps[pb][:, 0:n], scalar=self.modc(layer, 5, dc, which),
                        in1=dst[:, dc, s0:s0 + n], op0=ALU.mult, op1=ALU.add),
                        reads=[("ps", pb), ("X", st, s0)], writes=[("X", st, s0)])

            load(0)
            if len(work) > 1:
                load(1)
            if moe:
                build_be(0)
            for idx in range(len(items)):
                q, ti = items[idx]
                gu(idx)
                if idx > 0:
                    down(idx - 1)
                if ti == 0:
                    if q + 2 < len(work):
                        load(q + 2)
                    e, s = work[q]
                    if moe and s == min(1, len(slices) - 1) and e + 1 < len(experts):
                        build_be(e + 1)
            down(len(items) - 1)

    def row_to_cols(self, S, row, nchunks, dst, pb, rname, wname):
        ps = self.ps
        for c in range(nchunks):
            S.op("pe", lambda e, c=c: e.matmul(ps[pb][:, c:c + 1], lhsT=row[0:1, c * 128:(c + 1) * 128], rhs=self.ident[0:1, 0:1],
                                               start=True, stop=True), reads=[rname, "ident"], writes=[("ps", pb)])
        S.op("dve", lambda e: e.tensor_copy(out=dst[:, 0:nchunks], in_=ps[pb][:, 0:nchunks]), reads=[("ps", pb)], writes=[wname])

    def gmlp(self, layer, j, tiles):
        ps = self.ps
        NT = self.NT
        w_in = self.din(f"a_w_in_{j}", [D, 4096])
        v_g = self.din(f"a_v_g_{j}", [1, 2048])
        w_s = self.din(f"a_ws_{j}", [8, 128, 128])
        b_s = self.din(f"a_bs_{j}", [1, 1024])
        w_out = self.din(f"a_w_out_{j}", [2048, D])
        with self.phase() as (S, sb):
            WIN = [sb(f"WIN{i}", [128, KD, 512], BF16) for i in range(2)]
            WOUT = [sb(f"WOUT{i}", [128, 16, 128], BF16) for i in range(2)]
            U = sb("U", [128, 16, 512], BF16)
            vg = sb("vg", [128, 4, 2048], F32)
            vn = [sb(f"vn{i}", [128, 2048], BF16) for i in range(2)]
            ssq = sb("ssq", [128, 4], F32)
            VGC = sb("VGC", [128, 16], F32)
            BSB = sb("BSB", [128, 8, 128], F32)
            WST = sb("WST", [128, 8, 128], BF16)
            mx = [sb(f"mx{i}", [128, 4, 128], F32) for i in range(2)]
            WS = vg[:, 0, 0:1024].rearrange("p (g s) -> p g s", g=8)
            vrow = vg[0:1, 1, 0:2048]
            c0 = S.new_chan()
            wch = [S.new_chan() for _ in range(2)]
            och = [S.new_chan() for _ in range(2)]
            S.dma("sp", c0, lambda e: e.dma_start(out=vrow, in_=v_g), writes=[("vg", 1)])
            S.dma("sp", c0, lambda e: e.dma_start(out=BSB[:].rearrange("p g t -> p (g t)"), in_=b_s.broadcast_to([128, 1024])), writes=["BSB"])
            S.dma("sp", c0, lambda e: e.dma_start(out=WS, in_=w_s.rearrange("g t s -> t g s")), writes=[("vg", 0)])
            for g in range(8):
                pb = 6 + g // 4
                S.op("pe", lambda e, g=g, pb=pb: e.matmul(ps[pb][:, (g % 4) * 128:(g % 4 + 1) * 128], lhsT=WS[:, g, :], rhs=self.ident[:],
                                                          start=True, stop=True), reads=[("vg", 0), "ident"], writes=[("ps", pb)])
            for h in range(2):
                S.op("dve", lambda e, h=h: e.tensor_copy(out=WST[:, 4 * h:4 * h + 4, :].rearrange("p g t -> p (g t)"), in_=ps[6 + h][:]),
                     reads=[("ps", 6 + h)], writes=["WST"])
            self.row_to_cols(S, vrow, 16, VGC, 5, ("vg", 1), "VGC")
            wcnt = {"in": 0, "out": 0, "pv": 0, "pm": 0}

            def load_in(sl):
                r = wcnt["in"] % 2
                wcnt["in"] += 1
                S.dma("pool", wch[r], lambda e, r=r, sl=sl: e.dma_start(
                    out=WIN[r][:], in_=w_in[:, sl * 512:(sl + 1) * 512].rearrange("(k p) n -> p k n", p=128)), writes=[("WIN", r)])
                return r

            def load_out(dc):
                r = wcnt["out"] % 2
                wcnt["out"] += 1
                S.dma("pool", och[r], lambda e, r=r, dc=dc: e.dma_start(
                    out=WOUT[r][:], in_=w_out[:, dc * 128:(dc + 1) * 128].rearrange("(c p) n -> p c n", p=128)), writes=[("WOUT", r)])
                return r

            for (t0, n, st) in tiles:
                which = 1 if st == "XC" else 0
                dst = self.XC if st == "XC" else self.X
                s0 = t0 - NT if st == "XC" else t0
                nch = n // 128
                for sl in range(4):
                    r = load_in(4 + sl)
                    for c in range(nch):
                        pb = wcnt["pv"] % 2
                        wcnt["pv"] += 1
                        for k in range(KD):
                            S.op("pe", lambda e, r=r, k=k, c=c, pb=pb, t0=t0: e.matmul(
                                ps[pb][:], lhsT=self.H[:, k, t0 + c * 128:t0 + (c + 1) * 128], rhs=WIN[r][:, k, :],
                                start=(k == 0), stop=(k == KD - 1)), reads=[("WIN", r), ("H", t0)], writes=[("ps", pb)])
                        S.op("act", lambda e, c=c, sl=sl, pb=pb: e.activation(out=vg[:, c, sl * 512:(sl + 1) * 512], in_=ps[pb][:],
                                                                            func=AF.Gelu_apprx_tanh),
                             reads=[("ps", pb)], writes=[("vg", c)])
                for sl in range(4):
                    r = load_in(sl)
                    for fc in range(4):
                        pb = 2 + (fc % 2)
                        for k in range(KD):
                            S.op("pe", lambda e, r=r, k=k, fc=fc, pb=pb, t0=t0, n=n: e.matmul(
                                ps[pb][:, 0:n], lhsT=WIN[r][:, k, fc * 128:(fc + 1) * 128], rhs=self.H[:, k, t0:t0 + n],
                                start=(k == 0), stop=(k == KD - 1)), reads=[("WIN", r), ("H", t0)], writes=[("ps", pb)])
                        S.op("act", lambda e, sl=sl, fc=fc, pb=pb, n=n: e.activation(out=U[:, sl * 4 + fc, 0:n], in_=ps[pb][:, 0:n],
                                                                                  func=AF.Gelu_apprx_tanh),
                             reads=[("ps", pb)], writes=["U"])
                for c in range(nch):
                    S.op("act", lambda e, c=c: e.activation(out=vn[c % 2][:], in_=vg[:, c, :], func=AF.Square, accum_out=ssq[:, c:c + 1]),
                         reads=[("vg", c)], writes=[("vn", c % 2), "ssq"])
                S.op("act", lambda e, nch=nch: e.activation(out=ssq[:, 0:nch], in_=ssq[:, 0:nch], func=AF.Sqrt, bias=self.epsc[:, 0:1], scale=1.0 / 2048),
                     reads=["ssq"], writes=["ssq"])
                S.op("dve", lambda e, nch=nch: e.reciprocal(out=ssq[:, 0:nch], in_=ssq[:, 0:nch]), reads=["ssq"], writes=["ssq"])
                for c in range(nch):
                    vb = c % 2
                    S.op("dve", lambda e, c=c, vb=vb: e.tensor_scalar(out=vn[vb][:], in0=vg[:, c, :], scalar1=ssq[:, c:c + 1], scalar2=None,
                                                                     op0=ALU.mult),
                         reads=[("vg", c), "ssq"], writes=[("vn", vb)])
                    for q4 in range(4):
                        pb = 4 + (wcnt["pm"] % 2)
                        mb = wcnt["pm"] % 2
                        wcnt["pm"] += 1
                        for f4 in range(4):
                            fc = q4 * 4 + f4
                            S.op("pe", lambda e, vb=vb, fc=fc, f4=f4, pb=pb: e.matmul(
                                ps[pb][:, f4 * 128:(f4 + 1) * 128], lhsT=vn[vb][:, fc * 128:(fc + 1) * 128], rhs=WST[:, fc // 2, :],
                                start=True, stop=True), reads=[("vn", vb), "WST"], writes=[("ps", pb)])
                        for f4 in range(4):
                            fc = q4 * 4 + f4
                            S.op("dve", lambda e, fc=fc, f4=f4, pb=pb, mb=mb: e.scalar_tensor_tensor(
                                out=mx[mb][:, f4, :], in0=ps[pb][:, f4 * 128:(f4 + 1) * 128], scalar=VGC[:, fc:fc + 1],
                                in1=BSB[:, fc // 2, :], op0=ALU.mult, op1=ALU.add),
                                reads=[("ps", pb), "BSB", "VGC"], writes=[("mx", mb)])
                        S.op("dve", lambda e, q4=q4, c=c, mb=mb: e.tensor_tensor(
                            out=U[:, 4 * q4:4 * q4 + 4, c * 128:(c + 1) * 128], in0=U[:, 4 * q4:4 * q4 + 4, c * 128:(c + 1) * 128],
                            in1=mx[mb][:], op=ALU.mult), reads=[("mx", mb), "U"], writes=["U"])
                if self.cfg.get("stop") == "gmlp_dbg":
                    cd = S.new_chan()
                    for nm, T, shp, dt in [("d_vg", vg[:].rearrange("p c f -> p (c f)"), [128, 4 * 2048], F32), ("d_ssq", ssq[:], [128, 4], F32),
                                           ("d_U", U[:].rearrange("p c f -> p (c f)"), [128, 16 * 512], BF16), ("d_VGC", VGC[:], [128, 16], F32),
                                           ("d_WST", WST[:].rearrange("p c f -> p (c f)"), [128, 1024], BF16),
                                           ("d_vn", vn[1][:], [128, 2048], BF16)]:
                        dd = self.dout(nm, shp, dt)
                        S.dma("sp", cd, lambda e, dd=dd, T=T: e.dma_start(out=dd, in_=T),
                              reads=[("vg", c) for c in range(4)] + ["ssq", "U", "VGC", "WST", ("vn", 0), ("vn", 1)])
                    break
                for dc in range(KD):
                    r = load_out(dc)
                    pb = 6 + (dc % 2)
                    for fc in range(16):
                        S.op("pe", lambda e, r=r, fc=fc, pb=pb, n=n: e.matmul(
                            ps[pb][:, 0:n], lhsT=WOUT[r][:, fc, :], rhs=U[:, fc, 0:n],
                            start=(fc == 0), stop=(fc == 15)), reads=[("WOUT", r), "U"], writes=[("ps", pb)])
                    S.op("dve", lambda e, dc=dc, pb=pb, n=n, dst=dst, s0=s0, which=which: e.scalar_tensor_tensor(
                        out=dst[:, dc, s0:s0 + n], in0=ps[pb][:, 0:n], scalar=self.modc(layer, 2, dc, which),
                        in1=dst[:, dc, s0:s0 + n], op0=ALU.mult, op1=ALU.add),
                        reads=[("ps", pb), ("X", st, s0)], writes=[("X", st, s0)])
                if self.cfg.get("stop") == "gmlp_dbg2":
                    cd = S.new_chan()
                    for nm, T, shp, dt in [("d_W0", WOUT[0][:].rearrange("p c f -> p (c f)"), [128, 2048], BF16),
                                           ("d_W1", WOUT[1][:].rearrange("p c f -> p (c f)"), [128, 2048], BF16),
                                           ("d_X", self.X[:, :, 0:512], [128, 8, 512], F32)]:
                        dd = self.dout(nm, shp, dt)
                        S.dma("sp", cd, lambda e, dd=dd, T=T: e.dma_start(out=dd, in_=T),
                              reads=[("WOUT", 0), ("WOUT", 1), ("X", "X", 0)])
                    break

    def qkv(self, want_q=True, want_kv=True, nbuf=2):
        ps = self.ps
        NT = self.NT
        wqkv = self.din("b_w_qkv_0", [D, 1536])
        qg = self.din("b_q_g_0", [64, 1])
        kg = self.din("b_k_g_0", [64, 1])
        cosd = self.din("cosT", [128, NT])
        sind = self.din("sinT", [128, NT])
        rotd = self.din("rotm", [128, 128])
        blkd = self.din("blk64", [128, 128])
        with self.phase() as (S, sb):
            W = sb("Wqkv", [128, KD, 1536], BF16)
            COS = sb("COS", [128, NT], F32)
            SIN = sb("SIN", [128, NT], F32)
            ROT = sb("ROT", [128, 128], F32)
            BLKf = sb("BLKf", [128, 128], F32)
            BLK = sb("BLK", [128, 128], BF16)
            GQ = sb("GQ", [128, 1], F32)
            GK = sb("GK", [128, 1], F32)
            qf = [sb(f"qf{i}", [128, 512], F32) for i in range(nbuf)]
            sq = [sb(f"sqq{i}", [128, 512], BF16) for i in range(nbuf)]
            rs = [sb(f"rsq{i}", [128, 512], F32) for i in range(nbuf)]
            t1 = [sb(f"t1{i}", [128, 512], F32) for i in range(nbuf)]
            c0 = S.new_chan()
            c1 = S.new_chan()
            c2 = S.new_chan()
            for i in range(3):
                S.dma("pool", c0, lambda e, i=i: e.dma_start(out=W[:, :, i * 512:(i + 1) * 512],
                                                            in_=wqkv[:, i * 512:(i + 1) * 512].rearrange("(k p) n -> p k n", p=128)),
                      writes=[("W", i)])
            S.dma("sp", c1, lambda e: e.dma_start(out=COS[:], in_=cosd), writes=["COS"])
            S.dma("sp", c1, lambda e: e.dma_start(out=SIN[:], in_=sind), writes=["SIN"])
            S.dma("sp", c1, lambda e: e.dma_start(out=ROT[:], in_=rotd), writes=["ROT"])
            S.dma("sp", c1, lambda e: e.dma_start(out=BLKf[:], in_=blkd), writes=["BLKf"])
            for h in range(2):
                S.dma("sp", c1, lambda e, h=h: e.dma_start(out=GQ[h * 64:(h + 1) * 64, :], in_=qg), writes=["GQ"])
                S.dma("sp", c1, lambda e, h=h: e.dma_start(out=GK[h * 64:(h + 1) * 64, :], in_=kg), writes=["GK"])
            S.op("dve", lambda e: e.tensor_copy(out=BLK[:], in_=BLKf[:]), reads=["BLKf"], writes=["BLK"])
            cnt = {"n": 0}

            def qk_chunk(t0, n, col0, gcol, rope, cpos, out_ap, wres):
                b = cnt["n"] % nbuf
                cnt["n"] += 1
                pq, pm, pr = ps[b], ps[2 + b], ps[4 + b]
                wi = col0 // 512
                for k in range(KD):
                    S.op("pe", lambda e, k=k: e.matmul(pq[:, 0:n], lhsT=W[:, k, col0:col0 + 128], rhs=self.H[:, k, t0:t0 + n],
                                                       start=(k == 0), stop=(k == KD - 1)), reads=[("W", wi), ("H", t0)], writes=[("ps", b)])
                S.op("act", lambda e: e.activation(out=qf[b][:, 0:n], in_=pq[:, 0:n], func=AF.Copy), reads=[("ps", b)], writes=[("qf", b)])
                S.op("act", lambda e: e.activation(out=sq[b][:, 0:n], in_=pq[:, 0:n], func=AF.Square), reads=[("ps", b)], writes=[("sq", b)])
                S.op("pe", lambda e: e.matmul(pm[:, 0:n], lhsT=BLK[:], rhs=sq[b][:, 0:n], start=True, stop=True),
                     reads=[("sq", b), "BLK"], writes=[("ps", 2 + b)])
                S.op("act", lambda e: e.activation(out=rs[b][:, 0:n], in_=pm[:, 0:n], func=AF.Sqrt, bias=self.epsc[:, 0:1], scale=1.0),
                     reads=[("ps", 2 + b)], writes=[("rs", b)])
                S.op("dve", lambda e: e.reciprocal(out=rs[b][:, 0:n], in_=rs[b][:, 0:n]),
                     reads=[("rs", b)], writes=[("rs", b)])
                S.op("dve", lambda e: e.scalar_tensor_tensor(out=qf[b][:, 0:n], in0=qf[b][:, 0:n], scalar=gcol[:, 0:1], in1=rs[b][:, 0:n],
                                                             op0=ALU.mult, op1=ALU.mult),
                     reads=[("qf", b), ("rs", b), "GQ", "GK"], writes=[("qf", b)])
                if not rope:
                    S.op("act", lambda e: e.activation(out=out_ap, in_=qf[b][:, 0:n], func=AF.Copy), reads=[("qf", b)], writes=[wres])
                    return
                S.op("pe", lambda e: e.matmul(pr[:, 0:n], lhsT=ROT[:], rhs=qf[b][:, 0:n], start=True, stop=True),
                     reads=[("qf", b), "ROT"], writes=[("ps", 4 + b)])
                S.op("dve", lambda e: e.tensor_tensor(out=t1[b][:, 0:n], in0=qf[b][:, 0:n], in1=COS[:, cpos:cpos + n], op=ALU.mult),
                     reads=[("qf", b), "COS"], writes=[("t1", b)])
                S.op("dve", lambda e: e.tensor_tensor(out=rs[b][:, 0:n], in0=pr[:, 0:n], in1=SIN[:, cpos:cpos + n], op=ALU.mult),
                     reads=[("ps", 4 + b), "SIN"], writes=[("rs", b)])
                S.op("dve", lambda e: e.tensor_tensor(out=out_ap, in0=t1[b][:, 0:n], in1=rs[b][:, 0:n], op=ALU.add),
                     reads=[("t1", b), ("rs", b)], writes=[wres])

            if want_kv:
                KM = sb("KM", [128, 2, NT], BF16)
                KC = sb("KC", [128, 2, NCTX], BF16)
                VM = sb("VM", [128, self.NTOT // 128, 256], BF16)
                for t0 in range(0, NT, 512):
                    for kc in range(2):
                        qk_chunk(t0, 512, 1024 + kc * 128, GK, True, t0, KM[:, kc, t0:t0 + 512], "KM")
                for kc in range(2):
                    qk_chunk(NT, NCTX, 1024 + kc * 128, GK, False, 0, KC[:, kc, :], "KC")
                for c in range(self.NTOT // 128):
                    pb = 6 + (c % 2)
                    for k in range(KD):
                        S.op("pe", lambda e, k=k, c=c, pb=pb: e.matmul(ps[pb][:, 0:256], lhsT=self.H[:, k, c * 128:(c + 1) * 128],
                                                                      rhs=W[:, k, 1280:1536], start=(k == 0), stop=(k == KD - 1)),
                             reads=[("W", 2), ("H", (c * 128) // 512 * 512)], writes=[("ps", pb)])
                    S.op("act", lambda e, c=c, pb=pb: e.activation(out=VM[:, c, :], in_=ps[pb][:, 0:256], func=AF.Copy),
                         reads=[("ps", pb)], writes=["VM"])
                nlc = NT // 128
                S.dma("sp", c2, lambda e: e.dma_start(out=self.kT_mine.rearrange("(k p) n -> p k n", p=128), in_=KM[:]), reads=["KM"])
                S.dma("sp", c2, lambda e: e.dma_start(out=self.kcT.rearrange("(k p) n -> p k n", p=128), in_=KC[:]), reads=["KC"])
                S.dma("sp", c2, lambda e: e.dma_start(out=self.v_mine.rearrange("(c p) f -> p c f", p=128), in_=VM[:, 0:nlc, :]), reads=["VM"])
                S.dma("sp", c2, lambda e: e.dma_start(out=self.vc.rearrange("(c p) f -> p c f", p=128), in_=VM[:, nlc:nlc + 2, :]), reads=["VM"])
            if want_q:
                for t0 in range(0, NT, 512):
                    for c in range(KD):
                        qk_chunk(t0, 512, c * 128, GQ, True, t0, self.QT[:, c, t0:t0 + 512], ("QT", t0))

    def attention(self):
        ps = self.ps
        NT = self.NT
        NK = 2 * NT + NCTX
        nkc = NK // 128
        OT = self.H
        with self.phase() as (S, sb):
            KDp = [sb(f"KD{i}", [128, NK], BF16) for i in range(2)]
            VA = [sb(f"VA{i}", [128, nkc, 128], BF16) for i in range(2)]
            Pm = [sb(f"Pm{i}", [128, 512], BF16) for i in range(3)]
            rinv = [sb(f"rinv{i}", [64, 512], F32) for i in range(2)]
            kch = [S.new_chan() for _ in range(2)]
            cnt = {"s": 0, "o": 0}
            for i in range(2):
                S.op("dve", lambda e, i=i: e.memset(VA[i][:, :, 64:128], 1.0), writes=[("VA", i)])
            for g in range(4):
                gb = g % 2
                for half in range(2):
                    prt = slice(half * 64, half * 64 + 64)
                    S.dma("sp", kch[gb], lambda e, prt=prt, g=g, gb=gb: e.dma_start(out=KDp[gb][prt, 0:NCTX], in_=self.kcT[g * 64:(g + 1) * 64, :]),
                          writes=[("KD", gb)])
                    for r in range(2):
                        S.dma("sp", kch[gb], lambda e, prt=prt, g=g, gb=gb, r=r: e.dma_start(
                            out=KDp[gb][prt, NCTX + r * NT:NCTX + (r + 1) * NT], in_=self.kT_all[r * 256 + g * 64:r * 256 + (g + 1) * 64, :]),
                            writes=[("KD", gb)])
                S.dma("sp", kch[gb], lambda e, g=g, gb=gb: e.dma_start(
                    out=VA[gb][:, 0:2, 0:64], in_=self.vc[:, g * 64:(g + 1) * 64].rearrange("(c p) f -> p c f", p=128)), writes=[("VA", gb)])
                S.dma("sp", kch[gb], lambda e, g=g, gb=gb: e.dma_start(
                    out=VA[gb][:, 2:nkc, 0:64], in_=self.v_all[:, g * 64:(g + 1) * 64].rearrange("(c p) f -> p c f", p=128)), writes=[("VA", gb)])
                for hh in range(4):
                    h = g * 4 + hh
                    ch, hb = h // 2, 64 * (h % 2)
                    for t0 in range(0, NT, 512):
                        ob = 6 + (cnt["o"] % 2)
                        rb = cnt["o"] % 2
                        cnt["o"] += 1

                        def s_mm(kc):
                            sbk = cnt["s"] % 3
                            cnt["s"] += 1
                            S.op("pe", lambda e, kc=kc, sbk=sbk, gb=gb, hb=hb, ch=ch, t0=t0: e.matmul(
                                ps[sbk][:], lhsT=KDp[gb][hb:hb + 64, kc * 128:(kc + 1) * 128], rhs=self.QT[hb:hb + 64, ch, t0:t0 + 512],
                                start=True, stop=True), reads=[("KD", gb), ("QT", t0)], writes=[("ps", sbk)])
                            S.op("act", lambda e, sbk=sbk: e.activation(out=Pm[sbk][:], in_=ps[sbk][:], func=AF.Exp, scale=0.125),
                                 reads=[("ps", sbk)], writes=[("Pm", sbk)])
                            return sbk

                        def pv_mm(kc, sbk):
                            S.op("pe", lambda e, kc=kc, sbk=sbk, ob=ob, gb=gb: e.matmul(ps[ob][:], lhsT=VA[gb][:, kc, :], rhs=Pm[sbk][:],
                                                                         start=(kc == 0), stop=(kc == nkc - 1)),
                                 reads=[("VA", gb), ("Pm", sbk)], writes=[("ps", ob)])
                        prev = s_mm(0)
                        for kc in range(1, nkc):
                            cur = s_mm(kc)
                            pv_mm(kc - 1, prev)
                            prev = cur
                        pv_mm(nkc - 1, prev)
                        S.op("dve", lambda e, ob=ob, rb=rb: e.reciprocal(out=rinv[rb][:], in_=ps[ob][64:128, :]),
                             reads=[("ps", ob)], writes=[("rinv", rb)])
                        S.op("dve", lambda e, ob=ob, rb=rb, ch=ch, hb=hb, t0=t0: e.tensor_tensor(
                            out=OT[hb:hb + 64, ch, t0:t0 + 512], in0=ps[ob][0:64, :], in1=rinv[rb][:], op=ALU.mult),
                            reads=[("ps", ob), ("rinv", rb)], writes=[("H", t0)])

    def oproj(self):
        ps = self.ps
        NT = self.NT
        wo = self.din("b_w_o_0", [D, D])
        with self.phase() as (S, sb):
            WO = sb("WO", [128, KD, D], BF16)
            c0 = S.new_chan()
            for i in range(2):
                S.dma("pool", c0, lambda e, i=i: e.dma_start(out=WO[:, :, i * 512:(i + 1) * 512],
                                                            in_=wo[:, i * 512:(i + 1) * 512].rearrange("(k p) n -> p k n", p=128)),
                      writes=[("WO", i)])
            for t0 in range(0, NT, 512):
                for dc in range(KD):
                    pb = dc % 2
                    for c in range(KD):
                        S.op("pe", lambda e, c=c, dc=dc, pb=pb, t0=t0: e.matmul(ps[pb][:], lhsT=WO[:, c, dc * 128:(dc + 1) * 128],
                                                                              rhs=self.H[:, c, t0:t0 + 512], start=(c == 0), stop=(c == KD - 1)),
                             reads=[("WO", dc // 4), ("H", t0)], writes=[("ps", pb)])
                    S.op("dve", lambda e, dc=dc, pb=pb, t0=t0: e.scalar_tensor_tensor(
                        out=self.X[:, dc, t0:t0 + 512], in0=ps[pb][:], scalar=self.modc(1, 2, dc, 0), in1=self.X[:, dc, t0:t0 + 512],
                        op0=ALU.mult, op1=ALU.add), reads=[("ps", pb), ("X", "X", t0)], writes=[("X", "X", t0)])

    def pool_norm_halo(self, store_only=False):
        NT = self.NT
        halo = self.dout("halo_mine", [D, 16]) if self.mode != "F" else self.dram["halo_mine"]
        with contextlib.ExitStack() as es1:
            HPA = self.sb(es1, "HPA", [128, KD, 512], F32)
            HPB = self.sb(es1, "HPB", [128, KD, 512], F32)
            self.norm(2, 0, [(0, 512, "X")], out32=HPA, out32_off=0)
            self.norm(2, 0, [(NT - 512, 512, "X")], out32=HPB, out32_off=-(NT - 512))
            with self.phase() as (S, sb):
                c0 = S.new_chan()
                hv = halo.rearrange("(k p) n -> p k n", p=128)
                S.dma("sp", c0, lambda e: e.dma_start(out=hv[:, :, 0:8], in_=HPA[:, :, 0:8]))
                S.dma("sp", c0, lambda e: e.dma_start(out=hv[:, :, 8:16], in_=HPB[:, :, 504:512]))

    def pool_mixer(self):
        ps = self.ps
        NT = self.NT
        lat = [(t0, 512, "X") for t0 in range(0, NT, 512)]
        pw = self.din("p_w_0", [4, 256, 256])
        pscale = self.din("p_scale_0", [1, D])
        flags = self.din("flags", [128, 2])
        halo_all = self.halo_all if self.mode == "F" else self.din("halo_all", [2 * D, 16])
        W2 = NT + 16
        li2 = self.layers.index(2)
        with contextlib.ExitStack() as es1:
            with self.phase() as (S, sb):
                RSTD = sb("RSTD", [128, NT], F32)
                sq = [sb(f"psq{i}", [128, KD, 512], BF16) for i in range(2)]
                HPk = [sb(f"HPk{i}", [128, W2], F32) for i in range(2)]
                TA = sb("TA", [128, W2], F32)
                TB = sb("TB", [128, W2], F32)
                ON = sb("ON", [128, W2], F32)
                RC = sb("RC", [128, NT], F32)
                FL = sb("FL", [128, 2], F32)
                HL = sb("HL", [128, KD, 16], F32)
                PW = sb("PW", [128, 4, 2, 256], BF16)
                PSR = sb("PSR", [1, D], F32)
                PSC = sb("PSC", [128, KD], F32)
                PG = sb("PG", [128, KD], F32)
                for ti, t0 in enumerate(range(0, NT, 512)):
                    b = ti % 2
                    S.op("act", lambda e, b=b, t0=t0: e.activation(out=sq[b][:], in_=self.X[:, :, t0:t0 + 512], func=AF.Square),
                         reads=[("X", "X", t0)], writes=[("sq", b)])
                    for k in range(KD):
                        S.op("pe", lambda e, b=b, k=k: e.matmul(ps[2 + b][:], lhsT=self.ones_b[:], rhs=sq[b][:, k, :],
                                                                start=(k == 0), stop=(k == KD - 1)), reads=[("sq", b)], writes=[("ps", 2 + b)])
                    S.op("act", lambda e, b=b, t0=t0: e.activation(out=RSTD[:, t0:t0 + 512], in_=ps[2 + b][:], func=AF.Sqrt, bias=self.epsc[:, 0:1], scale=1.0),
                         reads=[("ps", 2 + b)], writes=[("RSTD", t0)])
                    S.op("dve", lambda e, t0=t0: e.reciprocal(out=RSTD[:, t0:t0 + 512], in_=RSTD[:, t0:t0 + 512]),
                         reads=[("RSTD", t0)], writes=[("RSTD", t0)])
                c0 = S.new_chan()
                c1 = S.new_chan()
                S.dma("sp", c0, lambda e: e.dma_start(out=FL[:], in_=flags), writes=["FL"])
                S.dma("sp", c0, lambda e: e.dma_start(out=PSR[:], in_=pscale), writes=["PSR"])
                S.dma("sp", c0, lambda e: e.dma_start(out=HL[:, :, 0:8], in_=halo_all[0:D, 8:16].rearrange("(k p) n -> p k n", p=128)), writes=["HL"])
                S.dma("sp", c0, lambda e: e.dma_start(out=HL[:, :, 8:16], in_=halo_all[D:2 * D, 0:8].rearrange("(k p) n -> p k n", p=128)), writes=["HL"])
                S.dma("pool", c1, lambda e: e.dma_start(out=PW[:].rearrange("p j c n -> p (j c) n"),
                                                        in_=pw.rearrange("j (c p) n -> p (j c) n", p=128)), writes=["PW"])
                self.row_to_cols(S, PSR, KD, PSC, 7, "PSR", "PSC")
                S.op("dve", lambda e: e.tensor_tensor(out=PG[:], in0=PSC[:], in1=self.MODC[:, self.layers.index(2), 16:24, 0], op=ALU.mult),
                     reads=["PSC"], writes=["PG"])
                S.op("dve", lambda e: e.tensor_scalar(out=HL[:, :, 0:8], in0=HL[:, :, 0:8], scalar1=FL[:, 0:1], scalar2=None, op0=ALU.mult),
                     reads=["HL", "FL"], writes=["HL"])
                S.op("dve", lambda e: e.tensor_scalar(out=HL[:, :, 8:16], in0=HL[:, :, 8:16], scalar1=FL[:, 1:2], scalar2=None, op0=ALU.mult),
                     reads=["HL", "FL"], writes=["HL"])
                S.op("dve", lambda e: e.memset(ON[:], 1.0), writes=["ON"])
                S.op("dve", lambda e: e.tensor_scalar(out=ON[:, 0:8], in0=ON[:, 0:8], scalar1=FL[:, 0:1], scalar2=None, op0=ALU.mult),
                     reads=["FL", "ON"], writes=["ON"])
                S.op("dve", lambda e: e.tensor_scalar(out=ON[:, NT + 8:NT + 16], in0=ON[:, NT + 8:NT + 16], scalar1=FL[:, 1:2], scalar2=None, op0=ALU.mult),
                     reads=["FL", "ON"], writes=["ON"])

                def wsum(srcf, j, eng, rname):
                    bufs = [TA, TB]
                    cur = srcf
                    lo, hi = 0, W2
                    steps = [(1, 0)] + [(2 ** (l - 1), 2 ** (l - 1)) for l in range(1, j + 1)]
                    for si, (a, b) in enumerate(steps):
                        dst = bufs[si % 2]
                        nlo, nhi = lo + a, hi - b
                        S.op(eng, lambda e, cur=cur, dst=dst, a=a, b=b, nlo=nlo, nhi=nhi: e.tensor_tensor(
                            out=dst[:, nlo:nhi], in0=cur[:, nlo - a:nhi - a], in1=cur[:, nlo + b:nhi + b], op=ALU.add),
                            reads=rname + ["TA", "TB"], writes=["TA" if si % 2 == 0 else "TB"])
                        cur, lo, hi = dst, nlo, nhi
                    return cur
                xall = [("X", "X", t0) for t0 in range(0, NT, 512)]
                for k in range(KD):
                    j = k // 2
                    hb = k % 2
                    if k % 2 == 0:
                        res = wsum(ON, j, "dve", ["ON"])
                        S.op("dve", lambda e, res=res: e.reciprocal(out=RC[:], in_=res[:, 8:8 + NT]), reads=["TA", "TB"], writes=["RC"])
                    hp = HPk[hb]
                    S.op("dve", lambda e, k=k, hp=hp: e.scalar_tensor_tensor(
                        out=hp[:, 8:8 + NT], in0=self.X[:, k, 0:NT], scalar=self.MODC[:, li2, 8 + k, 0:1], in1=RSTD[:],
                        op0=ALU.mult, op1=ALU.mult), reads=xall + [("RSTD", t0) for t0 in range(0, NT, 512)], writes=[("HPk", hb)])
                    S.op("act", lambda e, k=k, hp=hp: e.activation(out=hp[:, 8:8 + NT], in_=hp[:, 8:8 + NT], func=AF.Identity,
                                                                   bias=self.MODC[:, li2, k, 0:1], scale=1.0),
                         reads=[("HPk", hb)], writes=[("HPk", hb)])
                    S.op("act", lambda e, k=k, hp=hp: e.activation(out=hp[:, 0:8], in_=HL[:, k, 0:8], func=AF.Copy), reads=["HL"], writes=[("HPk", hb)])
                    S.op("act", lambda e, k=k, hp=hp: e.activation(out=hp[:, NT + 8:NT + 16], in_=HL[:, k, 8:16], func=AF.Copy), reads=["HL"], writes=[("HPk", hb)])
                    res = wsum(hp, j, "dve", [("HPk", hb)])
                    S.op("dve", lambda e, res=res: e.tensor_tensor(out=res[:, 8:8 + NT], in0=res[:, 8:8 + NT], in1=RC[:], op=ALU.mult),
                         reads=["TA", "TB", "RC"], writes=["TA", "TB"])
                    S.op("dve", lambda e, k=k, res=res, hp=hp: e.tensor_tensor(out=self.H[:, k, 0:NT], in0=res[:, 8:8 + NT], in1=hp[:, 8:8 + NT], op=ALU.subtract),
                         reads=["TA", "TB", ("HPk", hb)], writes=[("H", t0) for t0 in range(0, NT, 512)])
                for t0 in range(0, NT, 512):
                    for dc in range(KD):
                        j, m = dc // 2, dc % 2
                        pb = dc % 2
                        for kk in range(2):
                            S.op("pe", lambda e, j=j, m=m, kk=kk, pb=pb, t0=t0: e.matmul(
                                ps[pb][:], lhsT=PW[:, j, kk, m * 128:(m + 1) * 128], rhs=self.H[:, 2 * j + kk, t0:t0 + 512],
                                start=(kk == 0), stop=(kk == 1)), reads=["PW", ("H", t0)], writes=[("ps", pb)])
                        S.op("dve", lambda e, dc=dc, pb=pb, t0=t0: e.scalar_tensor_tensor(
                            out=self.X[:, dc, t0:t0 + 512], in0=ps[pb][:], scalar=PG[:, dc:dc + 1], in1=self.X[:, dc, t0:t0 + 512],
                            op0=ALU.mult, op1=ALU.add), reads=[("ps", pb), "PG", ("X", "X", t0)], writes=[("X", "X", t0)])


def _consts(NT, hf):
    ident = np.eye(128, dtype=np.float32)
    blk = np.zeros((128, 128), np.float32)
    blk[:64, :64] = 1.0 / 64
    blk[64:, 64:] = 1.0 / 64
    rot = np.zeros((128, 128), np.float32)
    for p in range(128):
        d = p % 64
        b = (d // 16) % 2
        if b == 0:
            rot[p + 16, p] = -1.0
        else:
            rot[p - 16, p] = 1.0
    pos = hf * NT + np.arange(NT)
    r, c = pos // GRID_W, pos % GRID_W
    inv = (10000.0 ** (-np.arange(16, dtype=np.float32) / 16)).astype(np.float32)
    ang = np.stack([r, c], -1).astype(np.float32)[..., None] * inv
    cosT = np.zeros((128, NT), np.float32)
    sinT = np.zeros((128, NT), np.float32)
    for p in range(128):
        d = p % 64
        a, f = d // 32, d % 16
        cosT[p] = np.cos(ang[:, a, f])
        sinT[p] = np.sin(ang[:, a, f])
    flags = np.zeros((128, 2), np.float32)
    flags[:, 0] = 1.0 if hf == 1 else 0.0
    flags[:, 1] = 1.0 if hf == 0 else 0.0
    return dict(ident=ident, blk64=blk, rotm=rot, cosT=cosT, sinT=sinT, flags=flags)


_NC_CACHE = {}


def _get_nc(cfg, mode):
    key = (tuple(sorted(cfg.items())), mode)
    if key not in _NC_CACHE:
        b = Builder(cfg, mode)
        b.build()
        _NC_CACHE[key] = b
    return _NC_CACHE[key]


def _run(cfg, mode, in_maps):
    b = _get_nc(cfg, mode)
    maps = []
    for m in in_maps:
        maps.append({k: np.ascontiguousarray(m[k]) for k in b.input_names})
    res = run_bass_kernel_spmd(b.nc, maps, core_ids=list(range(len(maps))))
    return res.results


def kernel_impl(cfg, inp):
    NT = cfg["NT"]
    SEQ = 2 * NT
    x = np.asarray(inp["x"])
    B = x.shape[0]
    ncores = 2 * B
    w = {k: np.asarray(v) for k, v in inp.items()}
    common = []
    for core in range(ncores):
        b, hf = core // 2, core % 2
        m = dict(_consts(NT, hf))
        m["crow"] = np.stack([w["c"][b], w["c_ctx"]], 0)
        for i in range(4):
            m[f"ada_w_{i}"] = w["ada_w"][i]
            m[f"ada_b_{i}"] = w["ada_b"][i][None, :]
            m[f"norm_g_{i}"] = w["norm_g"][i].reshape(1, 2 * D)
        for j in range(2):
            m[f"a_w_in_{j}"] = w["a_w_in"][j]
            m[f"a_v_g_{j}"] = w["a_v_g"][j][None, :]
            m[f"a_ws_{j}"] = w["a_ws"][j]
            m[f"a_bs_{j}"] = w["a_bs"][j].reshape(1, 1024)
            m[f"a_w_out_{j}"] = w["a_w_out"][j]
            m[f"f_w_gate_{j}"] = w["f_w_gate"][j]
            m[f"f_w_up_{j}"] = w["f_w_up"][j]
            m[f"f_w_down_{j}"] = w["f_w_down"][j]
            m[f"m_router_{j}"] = w["m_router"][j]
            m[f"m_w_gate_{j}"] = w["m_w_gate"][j]
            m[f"m_w_up_{j}"] = w["m_w_up"][j]
            m[f"m_w_down_{j}"] = w["m_w_down"][j]
        m["b_w_qkv_0"] = w["b_w_qkv"][0]
        m["b_q_g_0"] = w["b_q_g"][0].reshape(64, 1)
        m["b_k_g_0"] = w["b_k_g"][0].reshape(64, 1)
        m["b_w_o_0"] = w["b_w_o"][0]
        m["p_w_0"] = w["p_w"][0]
        m["p_scale_0"] = w["p_scale"][0][None, :]
        m["xT"] = x[b, hf * NT:(hf + 1) * NT, :].T
        m["ctxT"] = w["ctx"][b].T
        common.append(m)
    if cfg.get("fused", True):
        rf = _run(cfg, "F", common)
        out = np.empty((B, SEQ, D), np.float32)
        for core in range(ncores):
            b, hf = core // 2, core % 2
            out[b, hf * NT:(hf + 1) * NT, :] = rf[core]["yT"].T
        return out
    ra = _run(cfg, "A", common)
    for core in range(ncores):
        pair = [ra[(core // 2) * 2], ra[(core // 2) * 2 + 1]]
        m = common[core]
        m["xT"] = ra[core]["xT_out"]
        m["kT_all"] = np.concatenate([pair[0]["kT_mine"], pair[1]["kT_mine"]], 0)
        m["v_all"] = np.concatenate([pair[0]["v_mine"], pair[1]["v_mine"]], 0)
        m["kcT"] = ra[core]["kcT"]
        m["vc"] = ra[core]["vc"]
    rb = _run(cfg, "B", common)
    for core in range(ncores):
        pair = [rb[(core // 2) * 2], rb[(core // 2) * 2 + 1]]
        m = common[core]
        m["xT"] = rb[core]["xT_out"]
        m["halo_all"] = np.concatenate([pair[0]["halo_mine"], pair[1]["halo_mine"]], 0)
    rc = _run(cfg, "C", common)
    out = np.empty((B, SEQ, D), np.float32)
    for core in range(ncores):
        b, hf = core // 2, core % 2
        out[b, hf * NT:(hf + 1) * NT, :] = rc[core]["yT"].T
    return out


def kernel(**inputs):
    return kernel_impl(FULL_CFG, inputs)
```

```python
import contextlib
import numpy as np
import ml_dtypes
import concourse.bass as bass
import concourse.mybir as mybir
from concourse.bass_utils import run_bass_kernel_spmd

F32 = mybir.dt.float32
BF16 = mybir.dt.bfloat16
AF = mybir.ActivationFunctionType
ALU = mybir.AluOpType
AX = mybir.AxisListType

COMPUTE = ("pe", "act", "dve", "pool")
EPS = 1e-6
D = 1024
KD = 8
NCTX = 256
GRID_W = 64
POOL_WINDOWS = (2, 4, 8, 16)

FULL_CFG = dict(NT=2048, FFN=2816, EDIM=3584, NE=8)


class _Op:
    __slots__ = ("idx", "eng", "fn", "is_dma", "chan", "deps", "sig", "count")

    def __init__(self, idx, eng, fn, is_dma, chan):
        self.idx, self.eng, self.fn, self.is_dma, self.chan = idx, eng, fn, is_dma, chan
        self.deps = set()
        self.sig = False
        self.count = 0


class Sched:
    PHASE_ID = 0

    def __init__(self, nc):
        self.nc = nc
        self.ops = []
        self.last_w = {}
        self.readers = {}
        self.n_chan = 0
        self.auto_chan = {}

    def new_chan(self):
        self.n_chan += 1
        return self.n_chan - 1

    def _add(self, eng, fn, reads, writes, is_dma=False, chan=None):
        op = _Op(len(self.ops), eng, fn, is_dma, chan)
        for r in reads:
            w = self.last_w.get(r)
            if w is not None:
                op.deps.add(w)
        for r in writes:
            w = self.last_w.get(r)
            if w is not None:
                op.deps.add(w)
            for i in self.readers.get(r, {}).values():
                op.deps.add(i)
        for r in reads:
            self.readers.setdefault(r, {})[("dma", op.idx) if is_dma else eng] = op.idx
        for r in writes:
            self.last_w[r] = op.idx
            self.readers[r] = {}
        op.deps.discard(op.idx)
        self.ops.append(op)
        return op

    def op(self, eng, fn, reads=(), writes=()):
        return self._add(eng, fn, reads, writes)

    def dma(self, q, chan, fn, reads=(), writes=()):
        if writes:
            if writes[0] not in self.auto_chan:
                self.auto_chan[writes[0]] = self.new_chan()
            chan = self.auto_chan[writes[0]]
        else:
            chan = self.new_chan()
        return self._add(q, fn, reads, writes, is_dma=True, chan=chan)

    def emit(self):
        nc, ops = self.nc, self.ops
        if not ops:
            return
        for op in ops:
            for d in list(op.deps):
                a = ops[d]
                if (not a.is_dma) and a.eng == "pe" and op.eng == "pe" and not op.is_dma:
                    op.deps.discard(d)
                    continue
                a.sig = True
        eng_cnt = {e: 0 for e in COMPUTE}
        chan_cnt = [0] * self.n_chan
        for op in ops:
            if op.is_dma:
                chan_cnt[op.chan] += 16
                op.count = chan_cnt[op.chan]
            elif op.sig:
                eng_cnt[op.eng] += 1
                op.count = eng_cnt[op.eng]
        SEM_MAX = 30000
        Sched.PHASE_ID += 1
        pid = Sched.PHASE_ID
        eng_sems = {e: [nc.alloc_semaphore(name=f"p{pid}_{e}{i}")
                        for i in range(eng_cnt[e] // SEM_MAX + 1)] for e in COMPUTE}
        chan_sems = [nc.alloc_semaphore(name=f"p{pid}_ch{i}") for i in range(self.n_chan)]
        all_sems = [s for l in eng_sems.values() for s in l] + chan_sems
        assert all(c < 60000 for c in chan_cnt), chan_cnt
        with contextlib.ExitStack() as es:
            block = es.enter_context(nc.Block())

            def semval(a):
                if a.is_dma:
                    return chan_sems[a.chan], ("c", a.chan), a.count
                k = (a.count - 1) // SEM_MAX
                return eng_sems[a.eng][k], (a.eng, k), a.count - k * SEM_MAX

            def gen(engname):
                def body(eng):
                    waited = {}
                    for op in ops:
                        if op.eng != engname:
                            continue
                        need = {}
                        for d in op.deps:
                            s, key, v = semval(ops[d])
                            if waited.get(key, 0) >= v:
                                continue
                            if need.get(key, (None, 0))[1] < v:
                                need[key] = (s, v)
                        for key, (s, v) in need.items():
                            eng.wait_ge(s, v)
                            waited[key] = v
                        ins = op.fn(eng)
                        if op.is_dma:
                            ins.then_inc(semval(op)[0], 16)
                        elif op.sig:
                            ins.then_inc(semval(op)[0], 1)
                    finals = set(op.chan for op in ops if op.is_dma and op.eng == engname)
                    for c in finals:
                        if waited.get(("c", c), 0) < chan_cnt[c]:
                            eng.wait_ge(chan_sems[c], chan_cnt[c])
                return body

            used = {op.eng for op in ops}
            if "pe" in used:
                block.tensor(gen("pe"))
            if "act" in used:
                block.scalar(gen("act"))
            if "dve" in used:
                block.vector(gen("dve"))
            if "pool" in used:
                block.gpsimd(gen("pool"))
            if "sp" in used:
                block.sync(gen("sp"))
        nc.clear_and_free_semaphores(all_sems)
        nc.all_engine_barrier()


def _fslices(nchunks, maxn=4):
    out, f = [], 0
    while f < nchunks:
        n = min(maxn, nchunks - f)
        out.append((f, n))
        f += n
    return out


class Builder:
    def __init__(self, cfg, mode):
        self.cfg, self.mode = cfg, mode
        self.NT = cfg["NT"]
        self.NTOT = self.NT + NCTX
        self.nc = bass.Bass("TRN2", target_bir_lowering=False)
        self.es = contextlib.ExitStack()
        self.dram = {}
        self.input_names = set()
        self.uid = 0

    def din(self, name, shape, dt=F32):
        if name not in self.dram:
            self.dram[name] = self.nc.dram_tensor(name, list(shape), dt, kind="ExternalInput").ap()
            self.input_names.add(name)
        return self.dram[name]

    def dout(self, name, shape, dt=F32):
        self.dram[name] = self.nc.dram_tensor(name, list(shape), dt, kind="ExternalOutput").ap()
        return self.dram[name]

    def dint(self, name, shape, dt=F32):
        self.dram[name] = self.nc.dram_tensor(name, list(shape), dt).ap()
        return self.dram[name]

    def sb(self, es, name, shape, dt):
        self.uid += 1
        return es.enter_context(self.nc.sbuf_tensor(f"{name}_{self.uid}", list(shape), dt))

    @contextlib.contextmanager
    def phase(self):
        S = Sched(self.nc)
        with contextlib.ExitStack() as es:
            yield S, (lambda name, shape, dt: self.sb(es, name, shape, dt))
            S.emit()

    def modc(self, layer, slot, k, which):
        li = self.layers.index(layer)
        return self.MODC[:, li, slot * 8 + k, which:which + 1]

    def build(self):
        nc, cfg, mode, NT = self.nc, self.cfg, self.mode, self.NT
        es = self.es
        self.layers = {"A": [0, 1], "B": [1, 2], "C": [2, 3], "F": [0, 1, 2, 3]}[mode]
        self.X = self.sb(es, "X", [128, KD, NT], F32)
        self.H = self.sb(es, "H", [128, KD, self.NTOT], BF16)
        self.MODC = self.sb(es, "MODC", [128, len(self.layers), 48, 2], F32)
        self.ident = self.sb(es, "ident", [128, 128], F32)
        self.ones_f = self.sb(es, "ones_f", [128, 128], F32)
        self.ones_b = self.sb(es, "ones_b", [128, 128], BF16)
        self.one_b = self.sb(es, "one_b", [128, 128], BF16)
        self.epsc = self.sb(es, "epsc", [128, 1], F32)
        self.ps = [es.enter_context(nc.psum_tensor(f"ps{i}", [128, 512], F32)) for i in range(8)]
        self.LOG = self.sb(es, "LOG", [128, NT // 128, cfg["NE"]], F32)
        self.CMB = self.sb(es, "CMB", [128, NT // 128, cfg["NE"]], F32)

        if mode == "F":
            self.kT_mine = self.dint("kT_mine", [256, NT], BF16)
            self.v_mine = self.dint("v_mine", [NT, 256], BF16)
            self.kcT = self.dint("kcT", [256, NCTX], BF16)
            self.vc = self.dint("vc", [NCTX, 256], BF16)
            self.kT_all = self.dint("kT_all", [512, NT], BF16)
            self.v_all = self.dint("v_all", [2 * NT, 256], BF16)
            self.dint("halo_mine", [D, 16])
            self.halo_all = self.dint("halo_all", [2 * D, 16])
        self.prologue()
        if mode in ("A", "F"):
            with contextlib.ExitStack() as es1:
                self.XC = self.sb(es1, "XC", [128, KD, NCTX], F32)
                self.load_stream(self.X, self.din("xT", [D, NT]), NT)
                self.load_stream(self.XC, self.din("ctxT", [D, NCTX]), NCTX)
                lat = [(t0, 512, "X") for t0 in range(0, NT, 512)]
                ctx = [(NT, NCTX, "XC")]
                self.norm(0, 0, lat + ctx)
                if self.dbg("norm00"):
                    return self.finish()
                self.gmlp(0, 0, lat + ctx)
                if self.dbg("gmlp0") or self.cfg.get("stop") in ("gmlp_dbg", "gmlp_dbg2"):
                    return self.finish()
                self.norm(0, 1, lat + ctx)
                self.ffn(0, lat + ctx, [(self.din("f_w_gate_0", [D, cfg["FFN"]]), self.din("f_w_up_0", [D, cfg["FFN"]]),
                                         self.din("f_w_down_0", [cfg["FFN"], D]))], cfg["FFN"])
                self.norm(1, 0, lat + ctx)
                if mode == "A":
                    self.kT_mine = self.dout("kT_mine", [256, NT], BF16)
                    self.v_mine = self.dout("v_mine", [NT, 256], BF16)
                    self.kcT = self.dout("kcT", [256, NCTX], BF16)
                    self.vc = self.dout("vc", [NCTX, 256], BF16)
                    self.qkv(want_q=False)
                    self.store_stream(self.X, self.dout("xT_out", [D, NT]), NT)
                    return self.finish()
            lat = [(t0, 512, "X") for t0 in range(0, NT, 512)]
            es2 = contextlib.ExitStack()
            self.QT = self.sb(es2, "QT", [128, KD, NT], BF16)
            self.qkv(want_q=True, want_kv=True, nbuf=1)
            self.allgather([(self.kT_mine, self.kT_all), (self.v_mine, self.v_all)])
            if self.cfg.get("ext_scratch", False):
                with self.phase() as (S, sb):
                    new = {}
                    for nm, T in [("kT_all", self.kT_all), ("v_all", self.v_all), ("kcT", self.kcT), ("vc", self.vc)]:
                        dd = self.dout("o_" + nm, list(T.shape), BF16)
                        S.dma("sp", None, lambda e, dd=dd, T=T: e.dma_start(out=dd, in_=T))
                        new[nm] = dd
                self.kT_all, self.v_all, self.kcT, self.vc = new["kT_all"], new["v_all"], new["kcT"], new["vc"]
            if self.cfg.get("stop") == "f1d":
                with self.phase() as (S, sb):
                    for nm, T in [("o_kT_all", self.kT_all), ("o_v_all", self.v_all), ("o_kcT", self.kcT), ("o_vc", self.vc)]:
                        dd = self.dout(nm, list(T.shape), BF16)
                        S.dma("sp", None, lambda e, dd=dd, T=T: e.dma_start(out=dd, in_=T))
                self.store_stream(self.X, self.dout("yT", [D, NT]), NT)
                return self.finish()
            if self.cfg.get("stop") == "f1":
                self.store_stream(self.X, self.dout("yT", [D, NT]), NT)
                return self.finish()
            with es2:
                if self.cfg.get("stop") == "f1b":
                    self.dump("dbg_q", self.QT[:].rearrange("p k n -> p (k n)"), [128, KD * NT], BF16)
                    self.dump("dbg_h", self.H[:].rearrange("p k n -> p (k n)"), [128, KD * self.NTOT], BF16)
                    self.store_stream(self.X, self.dout("yT", [D, NT]), NT)
                    return self.finish()
                self.attention()
                if self.cfg.get("stop") == "f2a":
                    self.dump("dbg_h", self.H[:].rearrange("p k n -> p (k n)"), [128, KD * self.NTOT], BF16)
                    self.dump("dbg_q", self.QT[:].rearrange("p k n -> p (k n)"), [128, KD * NT], BF16)
                    self.store_stream(self.X, self.dout("yT", [D, NT]), NT)
                    return self.finish()
            self.oproj()
            if self.cfg.get("stop") == "f2":
                self.store_stream(self.X, self.dout("yT", [D, NT]), NT)
                return self.finish()
            self.norm(1, 1, lat, router=self.din("m_router_0", [D, cfg["NE"]]))
            self.ffn(1, lat, [(self.din("m_w_gate_0", [cfg["NE"], D, cfg["EDIM"]])[e],
                               self.din("m_w_up_0", [cfg["NE"], D, cfg["EDIM"]])[e],
                               self.din("m_w_down_0", [cfg["NE"], cfg["EDIM"], D])[e]) for e in range(cfg["NE"])],
                     cfg["EDIM"], moe=True)
            if self.cfg.get("stop") == "f3":
                self.store_stream(self.X, self.dout("yT", [D, NT]), NT)
                return self.finish()
            self.pool_norm_halo()
            self.allgather([(self.dram["halo_mine"], self.halo_all)])
            if self.cfg.get("stop") == "f4":
                self.store_stream(self.X, self.dout("yT", [D, NT]), NT)
                return self.finish()
            self.pool_mixer()
            self.norm(2, 1, lat)
            self.ffn(2, lat, [(self.din("f_w_gate_1", [D, cfg["FFN"]]), self.din("f_w_up_1", [D, cfg["FFN"]]),
                               self.din("f_w_down_1", [cfg["FFN"], D]))], cfg["FFN"])
            if self.cfg.get("stop") == "f5":
                self.store_stream(self.X, self.dout("yT", [D, NT]), NT)
                return self.finish()
            self.norm(3, 0, lat)
            self.gmlp(3, 1, lat)
            self.norm(3, 1, lat, router=self.din("m_router_1", [D, cfg["NE"]]))
            self.ffn(3, lat, [(self.din("m_w_gate_1", [cfg["NE"], D, cfg["EDIM"]])[e],
                               self.din("m_w_up_1", [cfg["NE"], D, cfg["EDIM"]])[e],
                               self.din("m_w_down_1", [cfg["NE"], cfg["EDIM"], D])[e]) for e in range(cfg["NE"])],
                     cfg["EDIM"], moe=True)
            self.store_stream(self.X, self.dout("yT", [D, NT]), NT)
            return self.finish()
        if mode == "B":
            with contextlib.ExitStack() as es1:
                self.load_stream(self.X, self.din("xT", [D, NT]), NT)
                lat = [(t0, 512, "X") for t0 in range(0, NT, 512)]
                self.norm(1, 0, lat)
                self.kT_all = self.din("kT_all", [512, NT], BF16)
                self.v_all = self.din("v_all", [2 * NT, 256], BF16)
                self.kcT = self.din("kcT", [256, NCTX], BF16)
                self.vc = self.din("vc", [NCTX, 256], BF16)
                self.QT = self.sb(es1, "QT", [128, KD, NT], BF16)
                self.qkv(want_q=True, want_kv=False)
                self.attention()
            self.oproj()
            if self.cfg.get("stop") == "attn":
                self.store_stream(self.X, self.dout("xT_out", [D, NT]), NT)
                return self.finish()
            self.norm(1, 1, lat, router=self.din("m_router_0", [D, cfg["NE"]]))
            if self.cfg.get("stop") == "route":
                self.store_stream(self.X, self.dout("xT_out", [D, NT]), NT)
                self.dump("d_LOG", self.LOG[:].rearrange("p c e -> p (c e)"), [128, (NT // 128) * cfg["NE"]])
                self.dump("d_CMB", self.CMB[:].rearrange("p c e -> p (c e)"), [128, (NT // 128) * cfg["NE"]])
                self.dump("dbg_h", self.H[:].rearrange("p k n -> p (k n)"), [128, KD * self.NTOT], BF16)
                return self.finish()
            self.ffn(1, lat, [(self.din("m_w_gate_0", [cfg["NE"], D, cfg["EDIM"]])[e],
                               self.din("m_w_up_0", [cfg["NE"], D, cfg["EDIM"]])[e],
                               self.din("m_w_down_0", [cfg["NE"], cfg["EDIM"], D])[e]) for e in range(cfg["NE"])],
                     cfg["EDIM"], moe=True)
            self.store_stream(self.X, self.dout("xT_out", [D, NT]), NT)
            self.pool_norm_halo(store_only=True)
            return self.finish()
        if mode == "C":
            self.load_stream(self.X, self.din("xT", [D, NT]), NT)
            lat = [(t0, 512, "X") for t0 in range(0, NT, 512)]
            if self.cfg.get("stop") == "f4":
                self.store_stream(self.X, self.dout("yT", [D, NT]), NT)
                return self.finish()
            self.pool_mixer()
            self.norm(2, 1, lat)
            self.ffn(2, lat, [(self.din("f_w_gate_1", [D, cfg["FFN"]]), self.din("f_w_up_1", [D, cfg["FFN"]]),
                               self.din("f_w_down_1", [cfg["FFN"], D]))], cfg["FFN"])
            if self.cfg.get("stop") == "f5":
                self.store_stream(self.X, self.dout("yT", [D, NT]), NT)
                return self.finish()
            self.norm(3, 0, lat)
            self.gmlp(3, 1, lat)
            self.norm(3, 1, lat, router=self.din("m_router_1", [D, cfg["NE"]]))
            self.ffn(3, lat, [(self.din("m_w_gate_1", [cfg["NE"], D, cfg["EDIM"]])[e],
                               self.din("m_w_up_1", [cfg["NE"], D, cfg["EDIM"]])[e],
                               self.din("m_w_down_1", [cfg["NE"], cfg["EDIM"], D])[e]) for e in range(cfg["NE"])],
                     cfg["EDIM"], moe=True)
            self.store_stream(self.X, self.dout("yT", [D, NT]), NT)
            return self.finish()

    def finish(self):
        return self.nc

    def allgather(self, pairs):
        nc = self.nc
        if self.cfg.get("fake_gather"):
            with self.phase() as (S, sb):
                for src, dst in pairs:
                    n = src.shape[0]
                    for r in range(2):
                        S.dma("sp", None, lambda e, src=src, dst=dst, r=r, n=n: e.dma_start(out=dst[r * n:(r + 1) * n, :], in_=src))
            return
        with contextlib.ExitStack() as es:
            sems = [self.es.enter_context(nc.semaphore(f"cc{self.uid}_{i}")) for i in range(len(pairs))]
            self.uid += 1
            blk = es.enter_context(nc.Block())

            def body(g):
                for (src, dst), s in zip(pairs, sems):
                    g.collective_compute("AllGather", ALU.bypass, replica_groups=[[0, 1], [2, 3], [4, 5], [6, 7]],
                                         ins=[src], outs=[dst]).then_inc(s)
                for s in sems:
                    g.wait_ge(s, 1)
            blk.gpsimd(body)

    def dump(self, name, T, shape, dt=F32):
        d = self.dout(name, shape, dt)
        with self.phase() as (S, sb):
            c = S.new_chan()
            S.dma("sp", c, lambda e: e.dma_start(out=d, in_=T))

    def dbg(self, tag):
        if self.cfg.get("stop") != tag:
            return False
        NT = self.NT
        self.dump("dbg_modc", self.MODC[:].rearrange("p l c w -> p (l c w)"), [128, len(self.layers) * 96])
        self.dump("dbg_h", self.H[:].rearrange("p k n -> p (k n)"), [128, KD * self.NTOT], BF16)
        self.dump("dbg_x", self.X[:].rearrange("p k n -> p (k n)"), [128, KD * NT])
        if hasattr(self, "XC"):
            self.dump("dbg_xc", self.XC[:].rearrange("p k n -> p (k n)"), [128, KD * NCTX])
        return True


    def load_stream(self, T, src, n):
        with self.phase() as (S, sb):
            c = S.new_chan()
            S.dma("sp", c, lambda e: e.dma_start(out=T[:, :, 0:n], in_=src.rearrange("(k p) n -> p k n", p=128)),
                  writes=["T"])

    def store_stream(self, T, dst, n):
        with self.phase() as (S, sb):
            c = S.new_chan()
            S.dma("sp", c, lambda e: e.dma_start(out=dst.rearrange("(k p) n -> p k n", p=128), in_=T[:, :, 0:n]),
                  reads=["T"])

    def prologue(self):
        nc = self.nc
        crow = self.din("crow", [2, D])
        ident_d = self.din("ident", [128, 128])
        ps = self.ps
        with self.phase() as (S, sb):
            c0 = S.new_chan()
            S.dma("sp", c0, lambda e: e.dma_start(out=self.ident[:], in_=ident_d), writes=["ident"])
            S.op("dve", lambda e: e.memset(self.ones_f[:], 1.0), writes=["ones_f"])
            S.op("dve", lambda e: e.memset(self.ones_b[:], 1.0 / D), writes=["ones_b"])
            S.op("dve", lambda e: e.memset(self.one_b[:], 1.0), writes=["one_b"])
            S.op("dve", lambda e: e.memset(self.epsc[:], EPS), writes=["epsc"])
            crow_sb = sb("crow", [2, D], F32)
            srow = sb("srow", [2, D], F32)
            scol = sb("scol", [128, KD, 2], F32)
            S.dma("sp", c0, lambda e: e.dma_start(out=crow_sb[:], in_=crow), writes=["crow"])
            S.op("act", lambda e: e.activation(out=srow[:], in_=crow_sb[:], func=AF.Silu), reads=["crow"], writes=["srow"])
            for k in range(KD):
                S.op("pe", lambda e, k=k: e.matmul(ps[0][:, 2 * k:2 * k + 2], lhsT=srow[0:2, k * 128:(k + 1) * 128],
                                                   rhs=self.ident[0:2, 0:2], start=True, stop=True),
                     reads=["srow", "ident"], writes=[("ps", 0)])
            S.op("dve", lambda e: e.tensor_copy(out=scol[:].rearrange("p k w -> p (k w)"), in_=ps[0][:, 0:16]),
                 reads=[("ps", 0)], writes=["scol"])
            wb = [sb(f"adaw{i}", [128, KD, 512], F32) for i in range(2)]
            R = sb("R", [2, 6 * D], F32)
            adab = sb("adab", [2, 6 * D], F32)
            g2 = sb("g2", [2, 2, D], F32)
            cnt = 0
            for li, layer in enumerate(self.layers):
                adaw = self.din(f"ada_w_{layer}", [D, 6 * D])
                adab_d = self.din(f"ada_b_{layer}", [1, 6 * D])
                ng_d = self.din(f"norm_g_{layer}", [1, 2 * D])
                for r in range(2):
                    S.dma("sp", c0, lambda e, r=r, adab_d=adab_d: e.dma_start(out=adab[r:r + 1, :], in_=adab_d), writes=["adab"])
                    S.dma("sp", c0, lambda e, r=r, ng_d=ng_d: e.dma_start(out=g2[r:r + 1, :, :].rearrange("p a d -> p (a d)"), in_=ng_d),
                          writes=["g2"])
                for j in range(12):
                    b = cnt % 2
                    cnt += 1
                    S.dma("sp", None, lambda e, b=b, j=j, adaw=adaw: e.dma_start(
                        out=wb[b][:], in_=adaw[:, j * 512:(j + 1) * 512].rearrange("(k p) n -> p k n", p=128)), writes=[("wb", b)])
                    pb = 1 + (j % 2)
                    for k in range(KD):
                        S.op("pe", lambda e, b=b, k=k, pb=pb: e.matmul(ps[pb][0:2, :], lhsT=scol[:, k, :], rhs=wb[b][:, k, :],
                                                                      start=(k == 0), stop=(k == KD - 1)),
                             reads=["scol", ("wb", b)], writes=[("ps", pb)])
                    S.op("dve", lambda e, j=j, pb=pb: e.tensor_tensor(out=R[:, j * 512:(j + 1) * 512], in0=ps[pb][0:2, :],
                                                                      in1=adab[:, j * 512:(j + 1) * 512], op=ALU.add),
                         reads=[("ps", pb), "adab"], writes=["R"])
                for s in range(2):
                    sl = slice((3 * s + 1) * D, (3 * s + 2) * D)
                    S.op("dve", lambda e, s=s, sl=sl: e.scalar_tensor_tensor(out=R[:, sl], in0=R[:, sl], scalar=1.0, in1=g2[:, s, :],
                                                                             op0=ALU.add, op1=ALU.mult),
                         reads=["R", "g2"], writes=["R"])
                for c in range(48):
                    S.op("pe", lambda e, c=c: e.matmul(ps[3][:, 2 * c:2 * c + 2], lhsT=R[0:2, c * 128:(c + 1) * 128],
                                                       rhs=self.ident[0:2, 0:2], start=True, stop=True),
                         reads=["R", "ident"], writes=[("ps", 3)])
                S.op("dve", lambda e, li=li: e.tensor_copy(out=self.MODC[:, li, :, :].rearrange("p c w -> p (c w)"), in_=ps[3][:, 0:96]),
                     reads=[("ps", 3)], writes=["MODC"])

    def norm(self, layer, sub, tiles, router=None, out32=None, out32_off=0):
        ps = self.ps
        NE = self.cfg["NE"]
        with self.phase() as (S, sb):
            sq = [sb(f"sq{i}", [128, KD, 512], BF16) for i in range(2)]
            rstd = [sb(f"rstd{i}", [128, 512], F32) for i in range(2)]
            tt = [sb(f"tt{i}", [128, KD, 512], F32) for i in range(2)]
            if router is not None:
                RW = sb("RW", [128, KD, NE], F32)
                h32 = [sb(f"h32{i}", [128, KD, 512], F32) for i in range(2)]
                c0 = S.new_chan()
                S.dma("sp", c0, lambda e: e.dma_start(out=RW[:], in_=router.rearrange("(k p) e -> p k e", p=128)), writes=["RW"])
            for ti, (t0, n, st) in enumerate(tiles):
                b = ti % 2
                which = 1 if st == "XC" else 0
                src = self.XC if st == "XC" else self.X
                s0 = t0 - self.NT if st == "XC" else t0
                S.op("act", lambda e, b=b, src=src, s0=s0, n=n: e.activation(out=sq[b][:, :, 0:n], in_=src[:, :, s0:s0 + n], func=AF.Square),
                     reads=[("X", st, s0)], writes=[("sq", b)])
                for k in range(KD):
                    S.op("pe", lambda e, b=b, k=k, n=n: e.matmul(ps[b][:, 0:n], lhsT=self.ones_b[:], rhs=sq[b][:, k, 0:n],
                                                                 start=(k == 0), stop=(k == KD - 1)),
                         reads=[("sq", b)], writes=[("ps", b)])
                S.op("act", lambda e, b=b, n=n: e.activation(out=rstd[b][:, 0:n], in_=ps[b][:, 0:n], func=AF.Sqrt, bias=self.epsc[:, 0:1], scale=1.0),
                     reads=[("ps", b)], writes=[("rstd", b)])
                S.op("dve", lambda e, b=b, n=n: e.reciprocal(out=rstd[b][:, 0:n], in_=rstd[b][:, 0:n]),
                     reads=[("rstd", b)], writes=[("rstd", b)])
                for k in range(KD):
                    S.op("dve", lambda e, b=b, k=k, n=n, src=src, s0=s0, which=which: e.scalar_tensor_tensor(
                        out=tt[b][:, k, 0:n], in0=src[:, k, s0:s0 + n], scalar=self.modc(layer, 3 * sub + 1, k, which),
                        in1=rstd[b][:, 0:n], op0=ALU.mult, op1=ALU.mult),
                        reads=[("X", st, s0), ("rstd", b)], writes=[("tt", b, k)])
                    if out32 is not None:
                        S.op("act", lambda e, b=b, k=k, n=n, t0=t0, which=which: e.activation(
                            out=out32[:, k, out32_off + t0:out32_off + t0 + n], in_=tt[b][:, k, 0:n], func=AF.Identity,
                            bias=self.modc(layer, 3 * sub, k, which), scale=1.0),
                            reads=[("tt", b, k)], writes=[("H32P", t0)])
                        continue
                    S.op("act", lambda e, b=b, k=k, n=n, t0=t0, which=which: e.activation(
                        out=self.H[:, k, t0:t0 + n], in_=tt[b][:, k, 0:n], func=AF.Identity,
                        bias=self.modc(layer, 3 * sub, k, which), scale=1.0),
                        reads=[("tt", b, k)], writes=[("H", t0)])
                    if router is not None:
                        S.op("dve", lambda e, b=b, k=k, n=n, which=which: e.tensor_scalar(
                            out=h32[b][:, k, 0:n], in0=tt[b][:, k, 0:n], scalar1=self.modc(layer, 3 * sub, k, which), scalar2=None,
                            op0=ALU.add),
                            reads=[("tt", b, k)], writes=[("h32", b)])
                if router is not None:
                    for c in range(n // 128):
                        ch = t0 // 128 + c
                        pb = 2 + (ch % 2)
                        for k in range(KD):
                            S.op("pe", lambda e, b=b, k=k, c=c, pb=pb: e.matmul(ps[pb][:, 0:NE], lhsT=h32[b][:, k, c * 128:(c + 1) * 128],
                                                                               rhs=RW[:, k, :], start=(k == 0), stop=(k == KD - 1)),
                                 reads=[("h32", b), "RW"], writes=[("ps", pb)])
                        S.op("act", lambda e, ch=ch, pb=pb: e.activation(out=self.LOG[:, ch, :], in_=ps[pb][:, 0:NE], func=AF.Copy),
                             reads=[("ps", pb)], writes=["LOG"])
            if router is not None:
                self.route(S, sb)

    def route(self, S, sb):
        NE = self.cfg["NE"]
        nch = self.NT // 128
        LOG = self.LOG
        CMB = self.CMB
        v1 = sb("v1", [128, nch], F32)
        v2 = sb("v2", [128, nch], F32)
        m1 = sb("m1", [128, nch, NE], F32)
        m2 = sb("m2", [128, nch, NE], F32)
        L2 = sb("L2", [128, nch, NE], F32)
        dd = sb("dd", [128, nch], F32)
        e2 = sb("e2", [128, nch], F32)
        g1 = sb("g1", [128, nch], F32)
        g2 = sb("g2", [128, nch], F32)

        def bc(t):
            return t[:].unsqueeze(2).broadcast_to([128, nch, NE])
        S.op("dve", lambda e: e.tensor_reduce(out=v1[:], in_=LOG[:], axis=AX.X, op=ALU.max), reads=["LOG"], writes=["v1"])
        S.op("dve", lambda e: e.tensor_tensor(out=m1[:], in0=LOG[:], in1=bc(v1), op=ALU.is_equal), reads=["LOG", "v1"], writes=["m1"])
        S.op("dve", lambda e: e.scalar_tensor_tensor(out=L2[:], in0=m1[:], scalar=-1e30, in1=LOG[:], op0=ALU.mult, op1=ALU.add),
             reads=["m1", "LOG"], writes=["L2"])
        S.op("dve", lambda e: e.tensor_reduce(out=v2[:], in_=L2[:], axis=AX.X, op=ALU.max), reads=["L2"], writes=["v2"])
        S.op("dve", lambda e: e.tensor_tensor(out=m2[:], in0=L2[:], in1=bc(v2), op=ALU.is_equal), reads=["L2", "v2"], writes=["m2"])
        S.op("dve", lambda e: e.tensor_tensor(out=dd[:], in0=v2[:], in1=v1[:], op=ALU.subtract), reads=["v1", "v2"], writes=["dd"])
        S.op("act", lambda e: e.activation(out=e2[:], in_=dd[:], func=AF.Exp), reads=["dd"], writes=["e2"])
        S.op("dve", lambda e: e.tensor_scalar(out=g2[:], in0=e2[:], scalar1=1.0, scalar2=None, op0=ALU.add), reads=["e2"], writes=["g2"])
        S.op("dve", lambda e: e.reciprocal(out=g1[:], in_=g2[:]), reads=["g2"], writes=["g1"])
        S.op("dve", lambda e: e.tensor_tensor(out=g2[:], in0=e2[:], in1=g1[:], op=ALU.mult), reads=["e2", "g1"], writes=["g2"])
        S.op("dve", lambda e: e.tensor_tensor(out=m1[:], in0=m1[:], in1=bc(g1), op=ALU.mult), reads=["m1", "g1"], writes=["m1"])
        S.op("dve", lambda e: e.tensor_tensor(out=m2[:], in0=m2[:], in1=bc(g2), op=ALU.mult), reads=["m2", "g2"], writes=["m2"])
        S.op("dve", lambda e: e.tensor_tensor(out=CMB[:], in0=m1[:], in1=m2[:], op=ALU.add), reads=["m1", "m2"], writes=["CMB"])

    def ffn(self, layer, tiles, experts, F, moe=False):
        ps = self.ps
        NT = self.NT
        nfc = F // 128
        slices = _fslices(nfc)
        with self.phase() as (S, sb):
            WG = [sb(f"WG{i}", [128, KD, 512], BF16) for i in range(3)]
            WU = [sb(f"WU{i}", [128, KD, 512], BF16) for i in range(3)]
            WD = [sb(f"WD{i}", [128, 4, D], BF16) for i in range(3)]
            A = [sb(f"A{i}", [128, 4, 512], BF16) for i in range(2)]
            sg = [sb(f"sg{i}", [128, 512], BF16 if not moe else F32) for i in range(2)]
            wch = [S.new_chan() for _ in range(3)]
            if moe:
                BE = [sb(f"BE{i}", [128, NT], F32) for i in range(2)]
                Dg = [sb(f"Dg{i}", [128, 128], F32) for i in range(2)]
            work = [(e, s) for e in range(len(experts)) for s in range(len(slices))]

            def load(q):
                e, s = work[q]
                f0, nf = slices[s]
                wg, wu, wd = experts[e]
                r = q % 3
                S.dma("pool", wch[r], lambda en: en.dma_start(
                    out=WG[r][:, :, 0:nf * 128], in_=wg[:, f0 * 128:(f0 + nf) * 128].rearrange("(k p) n -> p k n", p=128)),
                    writes=[("WG", r)])
                S.dma("pool", wch[r], lambda en: en.dma_start(
                    out=WU[r][:, :, 0:nf * 128], in_=wu[:, f0 * 128:(f0 + nf) * 128].rearrange("(k p) n -> p k n", p=128)),
                    writes=[("WU", r)])
                S.dma("pool", wch[r], lambda en: en.dma_start(
                    out=WD[r][:, 0:nf, :], in_=wd[f0 * 128:(f0 + nf) * 128, :].rearrange("(c p) n -> p c n", p=128)),
                    writes=[("WD", r)])

            def build_be(e):
                eb = e % 2
                for ch in range(NT // 128):
                    db = ch % 2
                    pb = 6 + ((ch // 4) % 2)
                    S.op("dve", lambda en, ch=ch, db=db, e=e: en.tensor_scalar(
                        out=Dg[db][:], in0=self.ident[:], scalar1=self.CMB[:, ch, e:e + 1], scalar2=None, op0=ALU.mult),
                        reads=["CMB", "ident"], writes=[("Dg", db)])
                    S.op("pe", lambda en, ch=ch, db=db, pb=pb: en.matmul(ps[pb][:, (ch % 4) * 128:(ch % 4 + 1) * 128], lhsT=self.ones_f[:],
                                                                        rhs=Dg[db][:], start=True, stop=True),
                         reads=[("Dg", db)], writes=[("ps", pb)])
                    if ch % 4 == 3:
                        t0 = (ch // 4) * 512
                        S.op("act", lambda en, eb=eb, pb=pb, t0=t0: en.activation(out=BE[eb][:, t0:t0 + 512], in_=ps[pb][:], func=AF.Copy),
                             reads=[("ps", pb)], writes=[("BE", eb, t0)])

            items = [(q, ti) for q in range(len(work)) for ti in range(len(tiles))]
            cnt = {"gu": 0}

            def gu(idx):
                q, ti = items[idx]
                e, s = work[q]
                f0, nf = slices[s]
                t0, n, st = tiles[ti]
                r = q % 3
                ab = idx % 2
                for fc in range(nf):
                    j = cnt["gu"] % 2
                    cnt["gu"] += 1
                    for k in range(KD):
                        S.op("pe", lambda en, r=r, fc=fc, k=k, j=j, t0=t0, n=n: en.matmul(
                            ps[j][:, 0:n], lhsT=WG[r][:, k, fc * 128:(fc + 1) * 128], rhs=self.H[:, k, t0:t0 + n],
                            start=(k == 0), stop=(k == KD - 1)), reads=[("WG", r), ("H", t0)], writes=[("ps", j)])
                    for k in range(KD):
                        S.op("pe", lambda en, r=r, fc=fc, k=k, j=j, t0=t0, n=n: en.matmul(
                            ps[2 + j][:, 0:n], lhsT=WU[r][:, k, fc * 128:(fc + 1) * 128], rhs=self.H[:, k, t0:t0 + n],
                            start=(k == 0), stop=(k == KD - 1)), reads=[("WU", r), ("H", t0)], writes=[("ps", 2 + j)])
                    S.op("act", lambda en, j=j, n=n: en.activation(out=sg[j][:, 0:n], in_=ps[j][:, 0:n], func=AF.Silu),
                         reads=[("ps", j)], writes=[("sg", j)])
                    if not moe:
                        S.op("dve", lambda en, j=j, n=n, ab=ab, fc=fc: en.tensor_tensor(
                            out=A[ab][:, fc, 0:n], in0=sg[j][:, 0:n], in1=ps[2 + j][:, 0:n], op=ALU.mult),
                            reads=[("sg", j), ("ps", 2 + j)], writes=[("A", ab)])
                    else:
                        S.op("dve", lambda en, j=j, n=n: en.tensor_tensor(
                            out=sg[j][:, 0:n], in0=sg[j][:, 0:n], in1=ps[2 + j][:, 0:n], op=ALU.mult),
                            reads=[("sg", j), ("ps", 2 + j)], writes=[("sg", j)])
                        S.op("dve", lambda en, j=j, n=n, ab=ab, fc=fc, e=e, t0=t0: en.tensor_tensor(
                            out=A[ab][:, fc, 0:n], in0=sg[j][:, 0:n], in1=BE[e % 2][:, t0:t0 + n], op=ALU.mult),
                            reads=[("sg", j), ("BE", e % 2, t0)], writes=[("A", ab)])

            def down(idx):
                q, ti = items[idx]
                e, s = work[q]
                f0, nf = slices[s]
                t0, n, st = tiles[ti]
                r = q % 3
                ab = idx % 2
                which = 1 if st == "XC" else 0
                dst = self.XC if st == "XC" else self.X
                s0 = t0 - NT if st == "XC" else t0
                for dc in range(KD):
                    pb = 4 + (dc % 4)
                    for fc in range(nf):
                        S.op("pe", lambda en, r=r, fc=fc, dc=dc, pb=pb, ab=ab, n=n, nf=nf: en.matmul(
                            ps[pb][:, 0:n], lhsT=WD[r][:, fc, dc * 128:(dc + 1) * 128], rhs=A[ab][:, fc, 0:n],
                            start=(fc == 0), stop=(fc == nf - 1)), reads=[("WD", r), ("A", ab)], writes=[("ps", pb)])
                    S.op("dve", lambda en, dc=dc, pb=pb, n=n, dst=dst, s0=s0, which=which: en.scalar_tensor_tensor(
                        out=dst[:, dc, s0:s0 + n], in0=ps[pb][:, 0:n], scalar=self.modc(layer, 5, dc, which),
                        in1=dst[:, dc, s0:s0 + n], op0=ALU.mult, op1=ALU.add),
                        reads=[("ps", pb), ("X", st, s0)], writes=[("X", st, s0)])

            load(0)
            if len(work) > 1:
                load(1)
            if moe:
                build_be(0)
            for idx in range(len(items)):
                q, ti = items[idx]
                gu(idx)
                if idx > 0:
                    down(idx - 1)
                if ti == 0:
                    if q + 2 < len(work):
                        load(q + 2)
                    e, s = work[q]
                    if moe and s == min(1, len(slices) - 1) and e + 1 < len(experts):
                        build_be(e + 1)
            down(len(items) - 1)

    def row_to_cols(self, S, row, nchunks, dst, pb, rname, wname):
        ps = self.ps
        for c in range(nchunks):
            S.op("pe", lambda e, c=c: e.matmul(ps[pb][:, c:c + 1], lhsT=row[0:1, c * 128:(c + 1) * 128], rhs=self.ident[0:1, 0:1],
                                               start=True, stop=True), reads=[rname, "ident"], writes=[("ps", pb)])
        S.op("dve", lambda e: e.tensor_copy(out=dst[:, 0:nchunks], in_=ps[pb][:, 0:nchunks]), reads=[("ps", pb)], writes=[wname])

    def gmlp(self, layer, j, tiles):
        ps = self.ps
        NT = self.NT
        w_in = self.din(f"a_w_in_{j}", [D, 4096])
        v_g = self.din(f"a_v_g_{j}", [1, 2048])
        w_s = self.din(f"a_ws_{j}", [8, 128, 128])
        b_s = self.din(f"a_bs_{j}", [1, 1024])
        w_out = self.din(f"a_w_out_{j}", [2048, D])
        with self.phase() as (S, sb):
            WIN = [sb(f"WIN{i}", [128, KD, 512], BF16) for i in range(2)]
            WOUT = [sb(f"WOUT{i}", [128, 16, 128], BF16) for i in range(2)]
            U = sb("U", [128, 16, 512], BF16)
            vg = sb("vg", [128, 4, 2048], F32)
            vn = [sb(f"vn{i}", [128, 2048], BF16) for i in range(2)]
            ssq = sb("ssq", [128, 4], F32)
            VGC = sb("VGC", [128, 16], F32)
            BSB = sb("BSB", [128, 8, 128], F32)
            WST = sb("WST", [128, 8, 128], BF16)
            mx = [sb(f"mx{i}", [128, 4, 128], F32) for i in range(2)]
            WS = vg[:, 0, 0:1024].rearrange("p (g s) -> p g s", g=8)
            vrow = vg[0:1, 1, 0:2048]
            c0 = S.new_chan()
            wch = [S.new_chan() for _ in range(2)]
            och = [S.new_chan() for _ in range(2)]
            S.dma("sp", c0, lambda e: e.dma_start(out=vrow, in_=v_g), writes=[("vg", 1)])
            S.dma("sp", c0, lambda e: e.dma_start(out=BSB[:].rearrange("p g t -> p (g t)"), in_=b_s.broadcast_to([128, 1024])), writes=["BSB"])
            S.dma("sp", c0, lambda e: e.dma_start(out=WS, in_=w_s.rearrange("g t s -> t g s")), writes=[("vg", 0)])
            for g in range(8):
                pb = 6 + g // 4
                S.op("pe", lambda e, g=g, pb=pb: e.matmul(ps[pb][:, (g % 4) * 128:(g % 4 + 1) * 128], lhsT=WS[:, g, :], rhs=self.ident[:],
                                                          start=True, stop=True), reads=[("vg", 0), "ident"], writes=[("ps", pb)])
            for h in range(2):
                S.op("dve", lambda e, h=h: e.tensor_copy(out=WST[:, 4 * h:4 * h + 4, :].rearrange("p g t -> p (g t)"), in_=ps[6 + h][:]),
                     reads=[("ps", 6 + h)], writes=["WST"])
            self.row_to_cols(S, vrow, 16, VGC, 5, ("vg", 1), "VGC")
            wcnt = {"in": 0, "out": 0, "pv": 0, "pm": 0}

            def load_in(sl):
                r = wcnt["in"] % 2
                wcnt["in"] += 1
                S.dma("pool", wch[r], lambda e, r=r, sl=sl: e.dma_start(
                    out=WIN[r][:], in_=w_in[:, sl * 512:(sl + 1) * 512].rearrange("(k p) n -> p k n", p=128)), writes=[("WIN", r)])
                return r

            def load_out(dc):
                r = wcnt["out"] % 2
                wcnt["out"] += 1
                S.dma("pool", och[r], lambda e, r=r, dc=dc: e.dma_start(
                    out=WOUT[r][:], in_=w_out[:, dc * 128:(dc + 1) * 128].rearrange("(c p) n -> p c n", p=128)), writes=[("WOUT", r)])
                return r

            for (t0, n, st) in tiles:
                which = 1 if st == "XC" else 0
                dst = self.XC if st == "XC" else self.X
                s0 = t0 - NT if st == "XC" else t0
                nch = n // 128
                for sl in range(4):
                    r = load_in(4 + sl)
                    for c in range(nch):
                        pb = wcnt["pv"] % 2
                        wcnt["pv"] += 1
                        for k in range(KD):
                            S.op("pe", lambda e, r=r, k=k, c=c, pb=pb, t0=t0: e.matmul(
                                ps[pb][:], lhsT=self.H[:, k, t0 + c * 128:t0 + (c + 1) * 128], rhs=WIN[r][:, k, :],
                                start=(k == 0), stop=(k == KD - 1)), reads=[("WIN", r), ("H", t0)], writes=[("ps", pb)])
                        S.op("act", lambda e, c=c, sl=sl, pb=pb: e.activation(out=vg[:, c, sl * 512:(sl + 1) * 512], in_=ps[pb][:],
                                                                            func=AF.Gelu_apprx_tanh),
                             reads=[("ps", pb)], writes=[("vg", c)])
                for sl in range(4):
                    r = load_in(sl)
                    for fc in range(4):
                        pb = 2 + (fc % 2)
                        for k in range(KD):
                            S.op("pe", lambda e, r=r, k=k, fc=fc, pb=pb, t0=t0, n=n: e.matmul(
                                ps[pb][:, 0:n], lhsT=WIN[r][:, k, fc * 128:(fc + 1) * 128], rhs=self.H[:, k, t0:t0 + n],
                                start=(k == 0), stop=(k == KD - 1)), reads=[("WIN", r), ("H", t0)], writes=[("ps", pb)])
                        S.op("act", lambda e, sl=sl, fc=fc, pb=pb, n=n: e.activation(out=U[:, sl * 4 + fc, 0:n], in_=ps[pb][:, 0:n],
                                                                                  func=AF.Gelu_apprx_tanh),
                             reads=[("ps", pb)], writes=["U"])
                for c in range(nch):
                    S.op("act", lambda e, c=c: e.activation(out=vn[c % 2][:], in_=vg[:, c, :], func=AF.Square, accum_out=ssq[:, c:c + 1]),
                         reads=[("vg", c)], writes=[("vn", c % 2), "ssq"])
                S.op("act", lambda e, nch=nch: e.activation(out=ssq[:, 0:nch], in_=ssq[:, 0:nch], func=AF.Sqrt, bias=self.epsc[:, 0:1], scale=1.0 / 2048),
                     reads=["ssq"], writes=["ssq"])
                S.op("dve", lambda e, nch=nch: e.reciprocal(out=ssq[:, 0:nch], in_=ssq[:, 0:nch]), reads=["ssq"], writes=["ssq"])
                for c in range(nch):
                    vb = c % 2
                    S.op("dve", lambda e, c=c, vb=vb: e.tensor_scalar(out=vn[vb][:], in0=vg[:, c, :], scalar1=ssq[:, c:c + 1], scalar2=None,
                                                                     op0=ALU.mult),
                         reads=[("vg", c), "ssq"], writes=[("vn", vb)])
                    for q4 in range(4):
                        pb = 4 + (wcnt["pm"] % 2)
                        mb = wcnt["pm"] % 2
                        wcnt["pm"] += 1
                        for f4 in range(4):
                            fc = q4 * 4 + f4
                            S.op("pe", lambda e, vb=vb, fc=fc, f4=f4, pb=pb: e.matmul(
                                ps[pb][:, f4 * 128:(f4 + 1) * 128], lhsT=vn[vb][:, fc * 128:(fc + 1) * 128], rhs=WST[:, fc // 2, :],
                                start=True, stop=True), reads=[("vn", vb), "WST"], writes=[("ps", pb)])
                        for f4 in range(4):
                            fc = q4 * 4 + f4
                            S.op("dve", lambda e, fc=fc, f4=f4, pb=pb, mb=mb: e.scalar_tensor_tensor(
                                out=mx[mb][:, f4, :], in0=ps[pb][:, f4 * 128:(f4 + 1) * 128], scalar=VGC[:, fc:fc + 1],
                                in1=BSB[:, fc // 2, :], op0=ALU.mult, op1=ALU.add),
                                reads=[("ps", pb), "BSB", "VGC"], writes=[("mx", mb)])
                        S.op("dve", lambda e, q4=q4, c=c, mb=mb: e.tensor_tensor(
                            out=U[:, 4 * q4:4 * q4 + 4, c * 128:(c + 1) * 128], in0=U[:, 4 * q4:4 * q4 + 4, c * 128:(c + 1) * 128],
                            in1=mx[mb][:], op=ALU.mult), reads=[("mx", mb), "U"], writes=["U"])
                if self.cfg.get("stop") == "gmlp_dbg":
                    cd = S.new_chan()
                    for nm, T, shp, dt in [("d_vg", vg[:].rearrange("p c f -> p (c f)"), [128, 4 * 2048], F32), ("d_ssq", ssq[:], [128, 4], F32),
                                           ("d_U", U[:].rearrange("p c f -> p (c f)"), [128, 16 * 512], BF16), ("d_VGC", VGC[:], [128, 16], F32),
                                           ("d_WST", WST[:].rearrange("p c f -> p (c f)"), [128, 1024], BF16),
                                           ("d_vn", vn[1][:], [128, 2048], BF16)]:
                        dd = self.dout(nm, shp, dt)
                        S.dma("sp", cd, lambda e, dd=dd, T=T: e.dma_start(out=dd, in_=T),
                              reads=[("vg", c) for c in range(4)] + ["ssq", "U", "VGC", "WST", ("vn", 0), ("vn", 1)])
                    break
                for dc in range(KD):
                    r = load_out(dc)
                    pb = 6 + (dc % 2)
                    for fc in range(16):
                        S.op("pe", lambda e, r=r, fc=fc, pb=pb, n=n: e.matmul(
                            ps[pb][:, 0:n], lhsT=WOUT[r][:, fc, :], rhs=U[:, fc, 0:n],
                            start=(fc == 0), stop=(fc == 15)), reads=[("WOUT", r), "U"], writes=[("ps", pb)])
                    S.op("dve", lambda e, dc=dc, pb=pb, n=n, dst=dst, s0=s0, which=which: e.scalar_tensor_tensor(
                        out=dst[:, dc, s0:s0 + n], in0=ps[pb][:, 0:n], scalar=self.modc(layer, 2, dc, which),
                        in1=dst[:, dc, s0:s0 + n], op0=ALU.mult, op1=ALU.add),
                        reads=[("ps", pb), ("X", st, s0)], writes=[("X", st, s0)])
                if self.cfg.get("stop") == "gmlp_dbg2":
                    cd = S.new_chan()
                    for nm, T, shp, dt in [("d_W0", WOUT[0][:].rearrange("p c f -> p (c f)"), [128, 2048], BF16),
                                           ("d_W1", WOUT[1][:].rearrange("p c f -> p (c f)"), [128, 2048], BF16),
                                           ("d_X", self.X[:, :, 0:512], [128, 8, 512], F32)]:
                        dd = self.dout(nm, shp, dt)
                        S.dma("sp", cd, lambda e, dd=dd, T=T: e.dma_start(out=dd, in_=T),
                              reads=[("WOUT", 0), ("WOUT", 1), ("X", "X", 0)])
                    break

    def qkv(self, want_q=True, want_kv=True, nbuf=2):
        ps = self.ps
        NT = self.NT
        wqkv = self.din("b_w_qkv_0", [D, 1536])
        qg = self.din("b_q_g_0", [64, 1])
        kg = self.din("b_k_g_0", [64, 1])
        cosd = self.din("cosT", [128, NT])
        sind = self.din("sinT", [128, NT])
        rotd = self.din("rotm", [128, 128])
        blkd = self.din("blk64", [128, 128])
        with self.phase() as (S, sb):
            W = sb("Wqkv", [128, KD, 1536], BF16)
            COS = sb("COS", [128, NT], F32)
            SIN = sb("SIN", [128, NT], F32)
            ROT = sb("ROT", [128, 128], F32)
            BLKf = sb("BLKf", [128, 128], F32)
            BLK = sb("BLK", [128, 128], BF16)
            GQ = sb("GQ", [128, 1], F32)
            GK = sb("GK", [128, 1], F32)
            qf = [sb(f"qf{i}", [128, 512], F32) for i in range(nbuf)]
            sq = [sb(f"sqq{i}", [128, 512], BF16) for i in range(nbuf)]
            rs = [sb(f"rsq{i}", [128, 512], F32) for i in range(nbuf)]
            t1 = [sb(f"t1{i}", [128, 512], F32) for i in range(nbuf)]
            c0 = S.new_chan()
            c1 = S.new_chan()
            c2 = S.new_chan()
            for i in range(3):
                S.dma("pool", c0, lambda e, i=i: e.dma_start(out=W[:, :, i * 512:(i + 1) * 512],
                                                            in_=wqkv[:, i * 512:(i + 1) * 512].rearrange("(k p) n -> p k n", p=128)),
                      writes=[("W", i)])
            S.dma("sp", c1, lambda e: e.dma_start(out=COS[:], in_=cosd), writes=["COS"])
            S.dma("sp", c1, lambda e: e.dma_start(out=SIN[:], in_=sind), writes=["SIN"])
            S.dma("sp", c1, lambda e: e.dma_start(out=ROT[:], in_=rotd), writes=["ROT"])
            S.dma("sp", c1, lambda e: e.dma_start(out=BLKf[:], in_=blkd), writes=["BLKf"])
            for h in range(2):
                S.dma("sp", c1, lambda e, h=h: e.dma_start(out=GQ[h * 64:(h + 1) * 64, :], in_=qg), writes=["GQ"])
                S.dma("sp", c1, lambda e, h=h: e.dma_start(out=GK[h * 64:(h + 1) * 64, :], in_=kg), writes=["GK"])
            S.op("dve", lambda e: e.tensor_copy(out=BLK[:], in_=BLKf[:]), reads=["BLKf"], writes=["BLK"])
            cnt = {"n": 0}

            def qk_chunk(t0, n, col0, gcol, rope, cpos, out_ap, wres):
                b = cnt["n"] % nbuf
                cnt["n"] += 1
                pq, pm, pr = ps[b], ps[2 + b], ps[4 + b]
                wi = col0 // 512
                for k in range(KD):
                    S.op("pe", lambda e, k=k: e.matmul(pq[:, 0:n], lhsT=W[:, k, col0:col0 + 128], rhs=self.H[:, k, t0:t0 + n],
                                                       start=(k == 0), stop=(k == KD - 1)), reads=[("W", wi), ("H", t0)], writes=[("ps", b)])
                S.op("act", lambda e: e.activation(out=qf[b][:, 0:n], in_=pq[:, 0:n], func=AF.Copy), reads=[("ps", b)], writes=[("qf", b)])
                S.op("act", lambda e: e.activation(out=sq[b][:, 0:n], in_=pq[:, 0:n], func=AF.Square), reads=[("ps", b)], writes=[("sq", b)])
                S.op("pe", lambda e: e.matmul(pm[:, 0:n], lhsT=BLK[:], rhs=sq[b][:, 0:n], start=True, stop=True),
                     reads=[("sq", b), "BLK"], writes=[("ps", 2 + b)])
                S.op("act", lambda e: e.activation(out=rs[b][:, 0:n], in_=pm[:, 0:n], func=AF.Sqrt, bias=self.epsc[:, 0:1], scale=1.0),
                     reads=[("ps", 2 + b)], writes=[("rs", b)])
                S.op("dve", lambda e: e.reciprocal(out=rs[b][:, 0:n], in_=rs[b][:, 0:n]),
                     reads=[("rs", b)], writes=[("rs", b)])
                S.op("dve", lambda e: e.scalar_tensor_tensor(out=qf[b][:, 0:n], in0=qf[b][:, 0:n], scalar=gcol[:, 0:1], in1=rs[b][:, 0:n],
                                                             op0=ALU.mult, op1=ALU.mult),
                     reads=[("qf", b), ("rs", b), "GQ", "GK"], writes=[("qf", b)])
                if not rope:
                    S.op("act", lambda e: e.activation(out=out_ap, in_=qf[b][:, 0:n], func=AF.Copy), reads=[("qf", b)], writes=[wres])
                    return
                S.op("pe", lambda e: e.matmul(pr[:, 0:n], lhsT=ROT[:], rhs=qf[b][:, 0:n], start=True, stop=True),
                     reads=[("qf", b), "ROT"], writes=[("ps", 4 + b)])
                S.op("dve", lambda e: e.tensor_tensor(out=t1[b][:, 0:n], in0=qf[b][:, 0:n], in1=COS[:, cpos:cpos + n], op=ALU.mult),
                     reads=[("qf", b), "COS"], writes=[("t1", b)])
                S.op("dve", lambda e: e.tensor_tensor(out=rs[b][:, 0:n], in0=pr[:, 0:n], in1=SIN[:, cpos:cpos + n], op=ALU.mult),
                     reads=[("ps", 4 + b), "SIN"], writes=[("rs", b)])
                S.op("dve", lambda e: e.tensor_tensor(out=out_ap, in0=t1[b][:, 0:n], in1=rs[b][:, 0:n], op=ALU.add),
                     reads=[("t1", b), ("rs", b)], writes=[wres])

            if want_kv:
                KM = sb("KM", [128, 2, NT], BF16)
                KC = sb("KC", [128, 2, NCTX], BF16)
                VM = sb("VM", [128, self.NTOT // 128, 256], BF16)
                for t0 in range(0, NT, 512):
                    for kc in range(2):
                        qk_chunk(t0, 512, 1024 + kc * 128, GK, True, t0, KM[:, kc, t0:t0 + 512], "KM")
                for kc in range(2):
                    qk_chunk(NT, NCTX, 1024 + kc * 128, GK, False, 0, KC[:, kc, :], "KC")
                for c in range(self.NTOT // 128):
                    pb = 6 + (c % 2)
                    for k in range(KD):
                        S.op("pe", lambda e, k=k, c=c, pb=pb: e.matmul(ps[pb][:, 0:256], lhsT=self.H[:, k, c * 128:(c + 1) * 128],
                                                                      rhs=W[:, k, 1280:1536], start=(k == 0), stop=(k == KD - 1)),
                             reads=[("W", 2), ("H", (c * 128) // 512 * 512)], writes=[("ps", pb)])
                    S.op("act", lambda e, c=c, pb=pb: e.activation(out=VM[:, c, :], in_=ps[pb][:, 0:256], func=AF.Copy),
                         reads=[("ps", pb)], writes=["VM"])
                nlc = NT // 128
                S.dma("sp", c2, lambda e: e.dma_start(out=self.kT_mine.rearrange("(k p) n -> p k n", p=128), in_=KM[:]), reads=["KM"])
                S.dma("sp", c2, lambda e: e.dma_start(out=self.kcT.rearrange("(k p) n -> p k n", p=128), in_=KC[:]), reads=["KC"])
                S.dma("sp", c2, lambda e: e.dma_start(out=self.v_mine.rearrange("(c p) f -> p c f", p=128), in_=VM[:, 0:nlc, :]), reads=["VM"])
                S.dma("sp", c2, lambda e: e.dma_start(out=self.vc.rearrange("(c p) f -> p c f", p=128), in_=VM[:, nlc:nlc + 2, :]), reads=["VM"])
            if want_q:
                for t0 in range(0, NT, 512):
                    for c in range(KD):
                        qk_chunk(t0, 512, c * 128, GQ, True, t0, self.QT[:, c, t0:t0 + 512], ("QT", t0))

    def attention(self):
        ps = self.ps
        NT = self.NT
        NK = 2 * NT + NCTX
        nkc = NK // 128
        OT = self.H
        with self.phase() as (S, sb):
            KDp = [sb(f"KD{i}", [128, NK], BF16) for i in range(2)]
            VA = [sb(f"VA{i}", [128, nkc, 128], BF16) for i in range(2)]
            Pm = [sb(f"Pm{i}", [128, 512], BF16) for i in range(4)]
            rinv = [sb(f"rinv{i}", [64, 512], F32) for i in range(2)]
            kch = [S.new_chan() for _ in range(2)]
            cnt = {"s": 0, "o": 0}
            for i in range(2):
                S.op("dve", lambda e, i=i: e.memset(VA[i][:, :, 64:128], 1.0), writes=[("VA", i)])
            for g in range(4):
                gb = g % 2
                for half in range(2):
                    prt = slice(half * 64, half * 64 + 64)
                    S.dma("sp", kch[gb], lambda e, prt=prt, g=g, gb=gb: e.dma_start(out=KDp[gb][prt, 0:NCTX], in_=self.kcT[g * 64:(g + 1) * 64, :]),
                          writes=[("KD", gb)])
                    for r in range(2):
                        S.dma("sp", kch[gb], lambda e, prt=prt, g=g, gb=gb, r=r: e.dma_start(
                            out=KDp[gb][prt, NCTX + r * NT:NCTX + (r + 1) * NT], in_=self.kT_all[r * 256 + g * 64:r * 256 + (g + 1) * 64, :]),
                            writes=[("KD", gb)])
                S.dma("sp", kch[gb], lambda e, g=g, gb=gb: e.dma_start(
                    out=VA[gb][:, 0:2, 0:64], in_=self.vc[:, g * 64:(g + 1) * 64].rearrange("(c p) f -> p c f", p=128)), writes=[("VA", gb)])
                S.dma("sp", kch[gb], lambda e, g=g, gb=gb: e.dma_start(
                    out=VA[gb][:, 2:nkc, 0:64], in_=self.v_all[:, g * 64:(g + 1) * 64].rearrange("(c p) f -> p c f", p=128)), writes=[("VA", gb)])
                for hh in range(4):
                    h = g * 4 + hh
                    ch, hb = h // 2, 64 * (h % 2)
                    for t0 in range(0, NT, 512):
                        ob = 6 + (cnt["o"] % 2)
                        rb = cnt["o"] % 2
                        cnt["o"] += 1

                        def s_mm(kc):
                            sbk = cnt["s"] % 4
                            cnt["s"] += 1
                            S.op("pe", lambda e, kc=kc, sbk=sbk, gb=gb, hb=hb, ch=ch, t0=t0: e.matmul(
                                ps[sbk][:], lhsT=KDp[gb][hb:hb + 64, kc * 128:(kc + 1) * 128], rhs=self.QT[hb:hb + 64, ch, t0:t0 + 512],
                                start=True, stop=True), reads=[("KD", gb), ("QT", t0)], writes=[("ps", sbk)])
                            S.op("act", lambda e, sbk=sbk: e.activation(out=Pm[sbk][:], in_=ps[sbk][:], func=AF.Exp, scale=0.125),
                                 reads=[("ps", sbk)], writes=[("Pm", sbk)])
                            return sbk

                        def pv_mm(kc, sbk):
                            S.op("pe", lambda e, kc=kc, sbk=sbk, ob=ob, gb=gb: e.matmul(ps[ob][:], lhsT=VA[gb][:, kc, :], rhs=Pm[sbk][:],
                                                                         start=(kc == 0), stop=(kc == nkc - 1)),
                                 reads=[("VA", gb), ("Pm", sbk)], writes=[("ps", ob)])
                        la = 2
                        sbks = [s_mm(kc) for kc in range(min(la, nkc))]
                        for kc in range(nkc):
                            if kc + la < nkc:
                                sbks.append(s_mm(kc + la))
                            pv_mm(kc, sbks[kc])
                        S.op("dve", lambda e, ob=ob, rb=rb: e.reciprocal(out=rinv[rb][:], in_=ps[ob][64:128, :]),
                             reads=[("ps", ob)], writes=[("rinv", rb)])
                        S.op("dve", lambda e, ob=ob, rb=rb, ch=ch, hb=hb, t0=t0: e.tensor_tensor(
                            out=OT[hb:hb + 64, ch, t0:t0 + 512], in0=ps[ob][0:64, :], in1=rinv[rb][:], op=ALU.mult),
                            reads=[("ps", ob), ("rinv", rb)], writes=[("H", t0)])

    def oproj(self):
        ps = self.ps
        NT = self.NT
        wo = self.din("b_w_o_0", [D, D])
        with self.phase() as (S, sb):
            WO = sb("WO", [128, KD, D], BF16)
            c0 = S.new_chan()
            for i in range(2):
                S.dma("pool", c0, lambda e, i=i: e.dma_start(out=WO[:, :, i * 512:(i + 1) * 512],
                                                            in_=wo[:, i * 512:(i + 1) * 512].rearrange("(k p) n -> p k n", p=128)),
                      writes=[("WO", i)])
            for t0 in range(0, NT, 512):
                for dc in range(KD):
                    pb = dc % 2
                    for c in range(KD):
                        S.op("pe", lambda e, c=c, dc=dc, pb=pb, t0=t0: e.matmul(ps[pb][:], lhsT=WO[:, c, dc * 128:(dc + 1) * 128],
                                                                              rhs=self.H[:, c, t0:t0 + 512], start=(c == 0), stop=(c == KD - 1)),
                             reads=[("WO", dc // 4), ("H", t0)], writes=[("ps", pb)])
                    S.op("dve", lambda e, dc=dc, pb=pb, t0=t0: e.scalar_tensor_tensor(
                        out=self.X[:, dc, t0:t0 + 512], in0=ps[pb][:], scalar=self.modc(1, 2, dc, 0), in1=self.X[:, dc, t0:t0 + 512],
                        op0=ALU.mult, op1=ALU.add), reads=[("ps", pb), ("X", "X", t0)], writes=[("X", "X", t0)])

    def pool_norm_halo(self, store_only=False):
        NT = self.NT
        halo = self.dout("halo_mine", [D, 16]) if self.mode != "F" else self.dram["halo_mine"]
        with contextlib.ExitStack() as es1:
            HPA = self.sb(es1, "HPA", [128, KD, 512], F32)
            HPB = self.sb(es1, "HPB", [128, KD, 512], F32)
            self.norm(2, 0, [(0, 512, "X")], out32=HPA, out32_off=0)
            self.norm(2, 0, [(NT - 512, 512, "X")], out32=HPB, out32_off=-(NT - 512))
            with self.phase() as (S, sb):
                c0 = S.new_chan()
                hv = halo.rearrange("(k p) n -> p k n", p=128)
                S.dma("sp", c0, lambda e: e.dma_start(out=hv[:, :, 0:8], in_=HPA[:, :, 0:8]))
                S.dma("sp", c0, lambda e: e.dma_start(out=hv[:, :, 8:16], in_=HPB[:, :, 504:512]))

    def pool_mixer(self):
        ps = self.ps
        NT = self.NT
        lat = [(t0, 512, "X") for t0 in range(0, NT, 512)]
        pw = self.din("p_w_0", [4, 256, 256])
        pscale = self.din("p_scale_0", [1, D])
        flags = self.din("flags", [128, 2])
        halo_all = self.halo_all if self.mode == "F" else self.din("halo_all", [2 * D, 16])
        W2 = NT + 16
        li2 = self.layers.index(2)
        with contextlib.ExitStack() as es1:
            with self.phase() as (S, sb):
                RSTD = sb("RSTD", [128, NT], F32)
                sq = [sb(f"psq{i}", [128, KD, 512], BF16) for i in range(2)]
                HPk = [sb(f"HPk{i}", [128, W2], F32) for i in range(2)]
                TA = sb("TA", [128, W2], F32)
                TB = sb("TB", [128, W2], F32)
                ON = sb("ON", [128, W2], F32)
                RC = sb("RC", [128, NT], F32)
                FL = sb("FL", [128, 2], F32)
                HL = sb("HL", [128, KD, 16], F32)
                PW = sb("PW", [128, 4, 2, 256], BF16)
                PSR = sb("PSR", [1, D], F32)
                PSC = sb("PSC", [128, KD], F32)
                PG = sb("PG", [128, KD], F32)
                for ti, t0 in enumerate(range(0, NT, 512)):
                    b = ti % 2
                    S.op("act", lambda e, b=b, t0=t0: e.activation(out=sq[b][:], in_=self.X[:, :, t0:t0 + 512], func=AF.Square),
                         reads=[("X", "X", t0)], writes=[("sq", b)])
                    for k in range(KD):
                        S.op("pe", lambda e, b=b, k=k: e.matmul(ps[2 + b][:], lhsT=self.ones_b[:], rhs=sq[b][:, k, :],
                                                                start=(k == 0), stop=(k == KD - 1)), reads=[("sq", b)], writes=[("ps", 2 + b)])
                    S.op("act", lambda e, b=b, t0=t0: e.activation(out=RSTD[:, t0:t0 + 512], in_=ps[2 + b][:], func=AF.Sqrt, bias=self.epsc[:, 0:1], scale=1.0),
                         reads=[("ps", 2 + b)], writes=[("RSTD", t0)])
                    S.op("dve", lambda e, t0=t0: e.reciprocal(out=RSTD[:, t0:t0 + 512], in_=RSTD[:, t0:t0 + 512]),
                         reads=[("RSTD", t0)], writes=[("RSTD", t0)])
                c0 = S.new_chan()
                c1 = S.new_chan()
                S.dma("sp", c0, lambda e: e.dma_start(out=FL[:], in_=flags), writes=["FL"])
                S.dma("sp", c0, lambda e: e.dma_start(out=PSR[:], in_=pscale), writes=["PSR"])
                S.dma("sp", c0, lambda e: e.dma_start(out=HL[:, :, 0:8], in_=halo_all[0:D, 8:16].rearrange("(k p) n -> p k n", p=128)), writes=["HL"])
                S.dma("sp", c0, lambda e: e.dma_start(out=HL[:, :, 8:16], in_=halo_all[D:2 * D, 0:8].rearrange("(k p) n -> p k n", p=128)), writes=["HL"])
                S.dma("pool", c1, lambda e: e.dma_start(out=PW[:].rearrange("p j c n -> p (j c) n"),
                                                        in_=pw.rearrange("j (c p) n -> p (j c) n", p=128)), writes=["PW"])
                self.row_to_cols(S, PSR, KD, PSC, 7, "PSR", "PSC")
                S.op("dve", lambda e: e.tensor_tensor(out=PG[:], in0=PSC[:], in1=self.MODC[:, self.layers.index(2), 16:24, 0], op=ALU.mult),
                     reads=["PSC"], writes=["PG"])
                S.op("dve", lambda e: e.tensor_scalar(out=HL[:, :, 0:8], in0=HL[:, :, 0:8], scalar1=FL[:, 0:1], scalar2=None, op0=ALU.mult),
                     reads=["HL", "FL"], writes=["HL"])
                S.op("dve", lambda e: e.tensor_scalar(out=HL[:, :, 8:16], in0=HL[:, :, 8:16], scalar1=FL[:, 1:2], scalar2=None, op0=ALU.mult),
                     reads=["HL", "FL"], writes=["HL"])
                S.op("dve", lambda e: e.memset(ON[:], 1.0), writes=["ON"])
                S.op("dve", lambda e: e.tensor_scalar(out=ON[:, 0:8], in0=ON[:, 0:8], scalar1=FL[:, 0:1], scalar2=None, op0=ALU.mult),
                     reads=["FL", "ON"], writes=["ON"])
                S.op("dve", lambda e: e.tensor_scalar(out=ON[:, NT + 8:NT + 16], in0=ON[:, NT + 8:NT + 16], scalar1=FL[:, 1:2], scalar2=None, op0=ALU.mult),
                     reads=["FL", "ON"], writes=["ON"])

                def wsum(srcf, j, eng, rname):
                    bufs = [TA, TB]
                    cur = srcf
                    lo, hi = 0, W2
                    steps = [(1, 0)] + [(2 ** (l - 1), 2 ** (l - 1)) for l in range(1, j + 1)]
                    for si, (a, b) in enumerate(steps):
                        dst = bufs[si % 2]
                        nlo, nhi = lo + a, hi - b
                        S.op(eng, lambda e, cur=cur, dst=dst, a=a, b=b, nlo=nlo, nhi=nhi: e.tensor_tensor(
                            out=dst[:, nlo:nhi], in0=cur[:, nlo - a:nhi - a], in1=cur[:, nlo + b:nhi + b], op=ALU.add),
                            reads=rname + ["TA", "TB"], writes=["TA" if si % 2 == 0 else "TB"])
                        cur, lo, hi = dst, nlo, nhi
                    return cur
                xall = [("X", "X", t0) for t0 in range(0, NT, 512)]
                for k in range(KD):
                    j = k // 2
                    hb = k % 2
                    if k % 2 == 0:
                        res = wsum(ON, j, "dve", ["ON"])
                        S.op("dve", lambda e, res=res: e.reciprocal(out=RC[:], in_=res[:, 8:8 + NT]), reads=["TA", "TB"], writes=["RC"])
                    hp = HPk[hb]
                    S.op("dve", lambda e, k=k, hp=hp: e.scalar_tensor_tensor(
                        out=hp[:, 8:8 + NT], in0=self.X[:, k, 0:NT], scalar=self.MODC[:, li2, 8 + k, 0:1], in1=RSTD[:],
                        op0=ALU.mult, op1=ALU.mult), reads=xall + [("RSTD", t0) for t0 in range(0, NT, 512)], writes=[("HPk", hb)])
                    S.op("act", lambda e, k=k, hp=hp: e.activation(out=hp[:, 8:8 + NT], in_=hp[:, 8:8 + NT], func=AF.Identity,
                                                                   bias=self.MODC[:, li2, k, 0:1], scale=1.0),
                         reads=[("HPk", hb)], writes=[("HPk", hb)])
                    S.op("act", lambda e, k=k, hp=hp: e.activation(out=hp[:, 0:8], in_=HL[:, k, 0:8], func=AF.Copy), reads=["HL"], writes=[("HPk", hb)])
                    S.op("act", lambda e, k=k, hp=hp: e.activation(out=hp[:, NT + 8:NT + 16], in_=HL[:, k, 8:16], func=AF.Copy), reads=["HL"], writes=[("HPk", hb)])
                    res = wsum(hp, j, "dve", [("HPk", hb)])
                    S.op("dve", lambda e, res=res: e.tensor_tensor(out=res[:, 8:8 + NT], in0=res[:, 8:8 + NT], in1=RC[:], op=ALU.mult),
                         reads=["TA", "TB", "RC"], writes=["TA", "TB"])
                    S.op("dve", lambda e, k=k, res=res, hp=hp: e.tensor_tensor(out=self.H[:, k, 0:NT], in0=res[:, 8:8 + NT], in1=hp[:, 8:8 + NT], op=ALU.subtract),
                         reads=["TA", "TB", ("HPk", hb)], writes=[("H", t0) for t0 in range(0, NT, 512)])
                for t0 in range(0, NT, 512):
                    for dc in range(KD):
                        j, m = dc // 2, dc % 2
                        pb = dc % 2
                        for kk in range(2):
                            S.op("pe", lambda e, j=j, m=m, kk=kk, pb=pb, t0=t0: e.matmul(
                                ps[pb][:], lhsT=PW[:, j, kk, m * 128:(m + 1) * 128], rhs=self.H[:, 2 * j + kk, t0:t0 + 512],
                                start=(kk == 0), stop=(kk == 1)), reads=["PW", ("H", t0)], writes=[("ps", pb)])
                        S.op("dve", lambda e, dc=dc, pb=pb, t0=t0: e.scalar_tensor_tensor(
                            out=self.X[:, dc, t0:t0 + 512], in0=ps[pb][:], scalar=PG[:, dc:dc + 1], in1=self.X[:, dc, t0:t0 + 512],
                            op0=ALU.mult, op1=ALU.add), reads=[("ps", pb), "PG", ("X", "X", t0)], writes=[("X", "X", t0)])


def _consts(NT, hf):
    ident = np.eye(128, dtype=np.float32)
    blk = np.zeros((128, 128), np.float32)
    blk[:64, :64] = 1.0 / 64
    blk[64:, 64:] = 1.0 / 64
    rot = np.zeros((128, 128), np.float32)
    for p in range(128):
        d = p % 64
        b = (d // 16) % 2
        if b == 0:
            rot[p + 16, p] = -1.0
        else:
            rot[p - 16, p] = 1.0
    pos = hf * NT + np.arange(NT)
    r, c = pos // GRID_W, pos % GRID_W
    inv = (10000.0 ** (-np.arange(16, dtype=np.float32) / 16)).astype(np.float32)
    ang = np.stack([r, c], -1).astype(np.float32)[..., None] * inv
    cosT = np.zeros((128, NT), np.float32)
    sinT = np.zeros((128, NT), np.float32)
    for p in range(128):
        d = p % 64
        a, f = d // 32, d % 16
        cosT[p] = np.cos(ang[:, a, f])
        sinT[p] = np.sin(ang[:, a, f])
    flags = np.zeros((128, 2), np.float32)
    flags[:, 0] = 1.0 if hf == 1 else 0.0
    flags[:, 1] = 1.0 if hf == 0 else 0.0
    return dict(ident=ident, blk64=blk, rotm=rot, cosT=cosT, sinT=sinT, flags=flags)


_NC_CACHE = {}


def _get_nc(cfg, mode):
    key = (tuple(sorted(cfg.items())), mode)
    if key not in _NC_CACHE:
        b = Builder(cfg, mode)
        b.build()
        _NC_CACHE[key] = b
    return _NC_CACHE[key]


def _run(cfg, mode, in_maps):
    b = _get_nc(cfg, mode)
    maps = []
    for m in in_maps:
        maps.append({k: np.ascontiguousarray(m[k]) for k in b.input_names})
    res = run_bass_kernel_spmd(b.nc, maps, core_ids=list(range(len(maps))))
    return res.results


def kernel_impl(cfg, inp):
    NT = cfg["NT"]
    SEQ = 2 * NT
    x = np.asarray(inp["x"])
    B = x.shape[0]
    ncores = 2 * B
    w = {k: np.asarray(v) for k, v in inp.items()}
    common = []
    for core in range(ncores):
        b, hf = core // 2, core % 2
        m = dict(_consts(NT, hf))
        m["crow"] = np.stack([w["c"][b], w["c_ctx"]], 0)
        for i in range(4):
            m[f"ada_w_{i}"] = w["ada_w"][i]
            m[f"ada_b_{i}"] = w["ada_b"][i][None, :]
            m[f"norm_g_{i}"] = w["norm_g"][i].reshape(1, 2 * D)
        for j in range(2):
            m[f"a_w_in_{j}"] = w["a_w_in"][j]
            m[f"a_v_g_{j}"] = w["a_v_g"][j][None, :]
            m[f"a_ws_{j}"] = w["a_ws"][j]
            m[f"a_bs_{j}"] = w["a_bs"][j].reshape(1, 1024)
            m[f"a_w_out_{j}"] = w["a_w_out"][j]
            m[f"f_w_gate_{j}"] = w["f_w_gate"][j]
            m[f"f_w_up_{j}"] = w["f_w_up"][j]
            m[f"f_w_down_{j}"] = w["f_w_down"][j]
            m[f"m_router_{j}"] = w["m_router"][j]
            m[f"m_w_gate_{j}"] = w["m_w_gate"][j]
            m[f"m_w_up_{j}"] = w["m_w_up"][j]
            m[f"m_w_down_{j}"] = w["m_w_down"][j]
        m["b_w_qkv_0"] = w["b_w_qkv"][0]
        m["b_q_g_0"] = w["b_q_g"][0].reshape(64, 1)
        m["b_k_g_0"] = w["b_k_g"][0].reshape(64, 1)
        m["b_w_o_0"] = w["b_w_o"][0]
        m["p_w_0"] = w["p_w"][0]
        m["p_scale_0"] = w["p_scale"][0][None, :]
        m["xT"] = x[b, hf * NT:(hf + 1) * NT, :].T
        m["ctxT"] = w["ctx"][b].T
        common.append(m)
    if cfg.get("fused", True):
        rf = _run(cfg, "F", common)
        out = np.empty((B, SEQ, D), np.float32)
        for core in range(ncores):
            b, hf = core // 2, core % 2
            out[b, hf * NT:(hf + 1) * NT, :] = rf[core]["yT"].T
        return out
    ra = _run(cfg, "A", common)
    for core in range(ncores):
        pair = [ra[(core // 2) * 2], ra[(core // 2) * 2 + 1]]
        m = common[core]
        m["xT"] = ra[core]["xT_out"]
        m["kT_all"] = np.concatenate([pair[0]["kT_mine"], pair[1]["kT_mine"]], 0)
        m["v_all"] = np.concatenate([pair[0]["v_mine"], pair[1]["v_mine"]], 0)
        m["kcT"] = ra[core]["kcT"]
        m["vc"] = ra[core]["vc"]
    rb = _run(cfg, "B", common)
    for core in range(ncores):
        pair = [rb[(core // 2) * 2], rb[(core // 2) * 2 + 1]]
        m = common[core]
        m["xT"] = rb[core]["xT_out"]
        m["halo_all"] = np.concatenate([pair[0]["halo_mine"], pair[1]["halo_mine"]], 0)
    rc = _run(cfg, "C", common)
    out = np.empty((B, SEQ, D), np.float32)
    for core in range(ncores):
        b, hf = core // 2, core % 2
        out[b, hf * NT:(hf + 1) * NT, :] = rc[core]["yT"].T
    return out


def kernel(**inputs):
    return kernel_impl(FULL_CFG, inputs)
```

```python
import contextlib
import numpy as np
import ml_dtypes
import concourse.bass as bass
import concourse.mybir as mybir
from concourse.bass_utils import run_bass_kernel_spmd

F32 = mybir.dt.float32
BF16 = mybir.dt.bfloat16
AF = mybir.ActivationFunctionType
ALU = mybir.AluOpType
AX = mybir.AxisListType

COMPUTE = ("pe", "act", "dve", "pool")
EPS = 1e-6
D = 1024
KD = 8
NCTX = 256
GRID_W = 64
POOL_WINDOWS = (2, 4, 8, 16)

FULL_CFG = dict(NT=2048, FFN=2816, EDIM=3584, NE=8)


class _Op:
    __slots__ = ("idx", "eng", "fn", "is_dma", "chan", "deps", "sig", "count")

    def __init__(self, idx, eng, fn, is_dma, chan):
        self.idx, self.eng, self.fn, self.is_dma, self.chan = idx, eng, fn, is_dma, chan
        self.deps = set()
        self.sig = False
        self.count = 0


class Sched:
    PHASE_ID = 0

    def __init__(self, nc):
        self.nc = nc
        self.ops = []
        self.last_w = {}
        self.readers = {}
        self.n_chan = 0
        self.auto_chan = {}

    def new_chan(self):
        self.n_chan += 1
        return self.n_chan - 1

    def _add(self, eng, fn, reads, writes, is_dma=False, chan=None):
        op = _Op(len(self.ops), eng, fn, is_dma, chan)
        for r in reads:
            w = self.last_w.get(r)
            if w is not None:
                op.deps.add(w)
        for r in writes:
            w = self.last_w.get(r)
            if w is not None:
                op.deps.add(w)
            for i in self.readers.get(r, {}).values():
                op.deps.add(i)
        for r in reads:
            self.readers.setdefault(r, {})[("dma", op.idx) if is_dma else eng] = op.idx
        for r in writes:
            self.last_w[r] = op.idx
            self.readers[r] = {}
        op.deps.discard(op.idx)
        self.ops.append(op)
        return op

    def op(self, eng, fn, reads=(), writes=()):
        return self._add(eng, fn, reads, writes)

    def dma(self, q, chan, fn, reads=(), writes=()):
        if writes:
            if writes[0] not in self.auto_chan:
                self.auto_chan[writes[0]] = self.new_chan()
            chan = self.auto_chan[writes[0]]
        else:
            chan = self.new_chan()
        return self._add(q, fn, reads, writes, is_dma=True, chan=chan)

    def emit(self):
        nc, ops = self.nc, self.ops
        if not ops:
            return
        for op in ops:
            for d in list(op.deps):
                a = ops[d]
                if (not a.is_dma) and a.eng == "pe" and op.eng == "pe" and not op.is_dma:
                    op.deps.discard(d)
                    continue
                a.sig = True
        eng_cnt = {e: 0 for e in COMPUTE}
        chan_cnt = [0] * self.n_chan
        for op in ops:
            if op.is_dma:
                chan_cnt[op.chan] += 16
                op.count = chan_cnt[op.chan]
            elif op.sig:
                eng_cnt[op.eng] += 1
                op.count = eng_cnt[op.eng]
        SEM_MAX = 30000
        Sched.PHASE_ID += 1
        pid = Sched.PHASE_ID
        eng_sems = {e: [nc.alloc_semaphore(name=f"p{pid}_{e}{i}")
                        for i in range(eng_cnt[e] // SEM_MAX + 1)] for e in COMPUTE}
        chan_sems = [nc.alloc_semaphore(name=f"p{pid}_ch{i}") for i in range(self.n_chan)]
        all_sems = [s for l in eng_sems.values() for s in l] + chan_sems
        assert all(c < 60000 for c in chan_cnt), chan_cnt
        with contextlib.ExitStack() as es:
            block = es.enter_context(nc.Block())

            def semval(a):
                if a.is_dma:
                    return chan_sems[a.chan], ("c", a.chan), a.count
                k = (a.count - 1) // SEM_MAX
                return eng_sems[a.eng][k], (a.eng, k), a.count - k * SEM_MAX

            def gen(engname):
                def body(eng):
                    waited = {}
                    for op in ops:
                        if op.eng != engname:
                            continue
                        need = {}
                        for d in op.deps:
                            s, key, v = semval(ops[d])
                            if waited.get(key, 0) >= v:
                                continue
                            if need.get(key, (None, 0))[1] < v:
                                need[key] = (s, v)
                        for key, (s, v) in need.items():
                            eng.wait_ge(s, v)
                            waited[key] = v
                        ins = op.fn(eng)
                        if op.is_dma:
                            ins.then_inc(semval(op)[0], 16)
                        elif op.sig:
                            ins.then_inc(semval(op)[0], 1)
                    finals = set(op.chan for op in ops if op.is_dma and op.eng == engname)
                    for c in finals:
                        if waited.get(("c", c), 0) < chan_cnt[c]:
                            eng.wait_ge(chan_sems[c], chan_cnt[c])
                return body

            used = {op.eng for op in ops}
            if "pe" in used:
                block.tensor(gen("pe"))
            if "act" in used:
                block.scalar(gen("act"))
            if "dve" in used:
                block.vector(gen("dve"))
            if "pool" in used:
                block.gpsimd(gen("pool"))
            if "sp" in used:
                block.sync(gen("sp"))
        nc.clear_and_free_semaphores(all_sems)
        nc.all_engine_barrier()


def _fslices(nchunks, maxn=4):
    out, f = [], 0
    while f < nchunks:
        n = min(maxn, nchunks - f)
        out.append((f, n))
        f += n
    return out


class Builder:
    def __init__(self, cfg, mode):
        self.cfg, self.mode = cfg, mode
        self.NT = cfg["NT"]
        self.NTOT = self.NT + NCTX
        self.nc = bass.Bass("TRN2", target_bir_lowering=False)
        self.es = contextlib.ExitStack()
        self.dram = {}
        self.input_names = set()
        self.uid = 0

    def din(self, name, shape, dt=F32):
        if name not in self.dram:
            self.dram[name] = self.nc.dram_tensor(name, list(shape), dt, kind="ExternalInput").ap()
            self.input_names.add(name)
        return self.dram[name]

    def dout(self, name, shape, dt=F32):
        self.dram[name] = self.nc.dram_tensor(name, list(shape), dt, kind="ExternalOutput").ap()
        return self.dram[name]

    def dint(self, name, shape, dt=F32):
        self.dram[name] = self.nc.dram_tensor(name, list(shape), dt).ap()
        return self.dram[name]

    def sb(self, es, name, shape, dt):
        self.uid += 1
        return es.enter_context(self.nc.sbuf_tensor(f"{name}_{self.uid}", list(shape), dt))

    @contextlib.contextmanager
    def phase(self):
        S = Sched(self.nc)
        with contextlib.ExitStack() as es:
            yield S, (lambda name, shape, dt: self.sb(es, name, shape, dt))
            S.emit()

    def modc(self, layer, slot, k, which):
        li = self.layers.index(layer)
        return self.MODC[:, li, slot * 8 + k, which:which + 1]

    def build(self):
        nc, cfg, mode, NT = self.nc, self.cfg, self.mode, self.NT
        es = self.es
        self.layers = {"A": [0, 1], "B": [1, 2], "C": [2, 3], "F": [0, 1, 2, 3]}[mode]
        self.X = self.sb(es, "X", [128, KD, NT], F32)
        self.H = self.sb(es, "H", [128, KD, self.NTOT], BF16)
        self.MODC = self.sb(es, "MODC", [128, len(self.layers), 48, 2], F32)
        self.ident = self.sb(es, "ident", [128, 128], F32)
        self.ones_f = self.sb(es, "ones_f", [128, 128], F32)
        self.ones_b = self.sb(es, "ones_b", [128, 128], BF16)
        self.one_b = self.sb(es, "one_b", [128, 128], BF16)
        self.epsc = self.sb(es, "epsc", [128, 1], F32)
        self.ps = [es.enter_context(nc.psum_tensor(f"ps{i}", [128, 512], F32)) for i in range(8)]
        self.LOG = self.sb(es, "LOG", [128, NT // 128, cfg["NE"]], F32)
        self.CMB = self.sb(es, "CMB", [128, NT // 128, cfg["NE"]], F32)

        if mode == "F":
            self.kT_mine = self.dint("kT_mine", [256, NT], BF16)
            self.v_mine = self.dint("v_mine", [NT, 256], BF16)
            self.kcT = self.dint("kcT", [256, NCTX], BF16)
            self.vc = self.dint("vc", [NCTX, 256], BF16)
            self.kT_all = self.dint("kT_all", [512, NT], BF16)
            self.v_all = self.dint("v_all", [2 * NT, 256], BF16)
            self.dint("halo_mine", [D, 16])
            self.halo_all = self.dint("halo_all", [2 * D, 16])
        self.prologue()
        if mode in ("A", "F"):
            with contextlib.ExitStack() as es1:
                self.XC = self.sb(es1, "XC", [128, KD, NCTX], F32)
                self.load_stream(self.X, self.din("xT", [D, NT]), NT)
                self.load_stream(self.XC, self.din("ctxT", [D, NCTX]), NCTX)
                lat = [(t0, 512, "X") for t0 in range(0, NT, 512)]
                ctx = [(NT, NCTX, "XC")]
                self.norm(0, 0, lat + ctx)
                if self.dbg("norm00"):
                    return self.finish()
                self.gmlp(0, 0, lat + ctx)
                if self.dbg("gmlp0") or self.cfg.get("stop") in ("gmlp_dbg", "gmlp_dbg2"):
                    return self.finish()
                self.norm(0, 1, lat + ctx)
                self.ffn(0, lat + ctx, [(self.din("f_w_gate_0", [D, cfg["FFN"]]), self.din("f_w_up_0", [D, cfg["FFN"]]),
                                         self.din("f_w_down_0", [cfg["FFN"], D]))], cfg["FFN"])
                self.norm(1, 0, lat + ctx)
                if mode == "A":
                    self.kT_mine = self.dout("kT_mine", [256, NT], BF16)
                    self.v_mine = self.dout("v_mine", [NT, 256], BF16)
                    self.kcT = self.dout("kcT", [256, NCTX], BF16)
                    self.vc = self.dout("vc", [NCTX, 256], BF16)
                    self.qkv(want_q=False)
                    self.store_stream(self.X, self.dout("xT_out", [D, NT]), NT)
                    return self.finish()
            lat = [(t0, 512, "X") for t0 in range(0, NT, 512)]
            es2 = contextlib.ExitStack()
            self.QT = self.sb(es2, "QT", [128, KD, NT], BF16)
            self.qkv(want_q=True, want_kv=True, nbuf=1)
            self.allgather([(self.kT_mine, self.kT_all), (self.v_mine, self.v_all)])
            if self.cfg.get("ext_scratch", False):
                with self.phase() as (S, sb):
                    new = {}
                    for nm, T in [("kT_all", self.kT_all), ("v_all", self.v_all), ("kcT", self.kcT), ("vc", self.vc)]:
                        dd = self.dout("o_" + nm, list(T.shape), BF16)
                        S.dma("sp", None, lambda e, dd=dd, T=T: e.dma_start(out=dd, in_=T))
                        new[nm] = dd
                self.kT_all, self.v_all, self.kcT, self.vc = new["kT_all"], new["v_all"], new["kcT"], new["vc"]
            if self.cfg.get("stop") == "f1d":
                with self.phase() as (S, sb):
                    for nm, T in [("o_kT_all", self.kT_all), ("o_v_all", self.v_all), ("o_kcT", self.kcT), ("o_vc", self.vc)]:
                        dd = self.dout(nm, list(T.shape), BF16)
                        S.dma("sp", None, lambda e, dd=dd, T=T: e.dma_start(out=dd, in_=T))
                self.store_stream(self.X, self.dout("yT", [D, NT]), NT)
                return self.finish()
            if self.cfg.get("stop") == "f1":
                self.store_stream(self.X, self.dout("yT", [D, NT]), NT)
                return self.finish()
            with es2:
                if self.cfg.get("stop") == "f1b":
                    self.dump("dbg_q", self.QT[:].rearrange("p k n -> p (k n)"), [128, KD * NT], BF16)
                    self.dump("dbg_h", self.H[:].rearrange("p k n -> p (k n)"), [128, KD * self.NTOT], BF16)
                    self.store_stream(self.X, self.dout("yT", [D, NT]), NT)
                    return self.finish()
                self.attention()
                if self.cfg.get("stop") == "f2a":
                    self.dump("dbg_h", self.H[:].rearrange("p k n -> p (k n)"), [128, KD * self.NTOT], BF16)
                    self.dump("dbg_q", self.QT[:].rearrange("p k n -> p (k n)"), [128, KD * NT], BF16)
                    self.store_stream(self.X, self.dout("yT", [D, NT]), NT)
                    return self.finish()
            self.oproj()
            if self.cfg.get("stop") == "f2":
                self.store_stream(self.X, self.dout("yT", [D, NT]), NT)
                return self.finish()
            self.norm(1, 1, lat, router=self.din("m_router_0", [D, cfg["NE"]]))
            self.ffn(1, lat, [(self.din("m_w_gate_0", [cfg["NE"], D, cfg["EDIM"]])[e],
                               self.din("m_w_up_0", [cfg["NE"], D, cfg["EDIM"]])[e],
                               self.din("m_w_down_0", [cfg["NE"], cfg["EDIM"], D])[e]) for e in range(cfg["NE"])],
                     cfg["EDIM"], moe=True)
            if self.cfg.get("stop") == "f3":
                self.store_stream(self.X, self.dout("yT", [D, NT]), NT)
                return self.finish()
            self.pool_norm_halo()
            self.allgather([(self.dram["halo_mine"], self.halo_all)])
            if self.cfg.get("stop") == "f4":
                self.store_stream(self.X, self.dout("yT", [D, NT]), NT)
                return self.finish()
            self.pool_mixer()
            self.norm(2, 1, lat)
            self.ffn(2, lat, [(self.din("f_w_gate_1", [D, cfg["FFN"]]), self.din("f_w_up_1", [D, cfg["FFN"]]),
                               self.din("f_w_down_1", [cfg["FFN"], D]))], cfg["FFN"])
            if self.cfg.get("stop") == "f5":
                self.store_stream(self.X, self.dout("yT", [D, NT]), NT)
                return self.finish()
            self.norm(3, 0, lat)
            self.gmlp(3, 1, lat)
            self.norm(3, 1, lat, router=self.din("m_router_1", [D, cfg["NE"]]))
            self.ffn(3, lat, [(self.din("m_w_gate_1", [cfg["NE"], D, cfg["EDIM"]])[e],
                               self.din("m_w_up_1", [cfg["NE"], D, cfg["EDIM"]])[e],
                               self.din("m_w_down_1", [cfg["NE"], cfg["EDIM"], D])[e]) for e in range(cfg["NE"])],
                     cfg["EDIM"], moe=True)
            self.store_stream(self.X, self.dout("yT", [D, NT]), NT)
            return self.finish()
        if mode == "B":
            with contextlib.ExitStack() as es1:
                self.load_stream(self.X, self.din("xT", [D, NT]), NT)
                lat = [(t0, 512, "X") for t0 in range(0, NT, 512)]
                self.norm(1, 0, lat)
                self.kT_all = self.din("kT_all", [512, NT], BF16)
                self.v_all = self.din("v_all", [2 * NT, 256], BF16)
                self.kcT = self.din("kcT", [256, NCTX], BF16)
                self.vc = self.din("vc", [NCTX, 256], BF16)
                self.QT = self.sb(es1, "QT", [128, KD, NT], BF16)
                self.qkv(want_q=True, want_kv=False)
                self.attention()
            self.oproj()
            if self.cfg.get("stop") == "attn":
                self.store_stream(self.X, self.dout("xT_out", [D, NT]), NT)
                return self.finish()
            self.norm(1, 1, lat, router=self.din("m_router_0", [D, cfg["NE"]]))
            if self.cfg.get("stop") == "route":
                self.store_stream(self.X, self.dout("xT_out", [D, NT]), NT)
                self.dump("d_LOG", self.LOG[:].rearrange("p c e -> p (c e)"), [128, (NT // 128) * cfg["NE"]])
                self.dump("d_CMB", self.CMB[:].rearrange("p c e -> p (c e)"), [128, (NT // 128) * cfg["NE"]])
                self.dump("dbg_h", self.H[:].rearrange("p k n -> p (k n)"), [128, KD * self.NTOT], BF16)
                return self.finish()
            self.ffn(1, lat, [(self.din("m_w_gate_0", [cfg["NE"], D, cfg["EDIM"]])[e],
                               self.din("m_w_up_0", [cfg["NE"], D, cfg["EDIM"]])[e],
                               self.din("m_w_down_0", [cfg["NE"], cfg["EDIM"], D])[e]) for e in range(cfg["NE"])],
                     cfg["EDIM"], moe=True)
            self.store_stream(self.X, self.dout("xT_out", [D, NT]), NT)
            self.pool_norm_halo(store_only=True)
            return self.finish()
        if mode == "C":
            self.load_stream(self.X, self.din("xT", [D, NT]), NT)
            lat = [(t0, 512, "X") for t0 in range(0, NT, 512)]
            if self.cfg.get("stop") == "f4":
                self.store_stream(self.X, self.dout("yT", [D, NT]), NT)
                return self.finish()
            self.pool_mixer()
            self.norm(2, 1, lat)
            self.ffn(2, lat, [(self.din("f_w_gate_1", [D, cfg["FFN"]]), self.din("f_w_up_1", [D, cfg["FFN"]]),
                               self.din("f_w_down_1", [cfg["FFN"], D]))], cfg["FFN"])
            if self.cfg.get("stop") == "f5":
                self.store_stream(self.X, self.dout("yT", [D, NT]), NT)
                return self.finish()
            self.norm(3, 0, lat)
            self.gmlp(3, 1, lat)
            self.norm(3, 1, lat, router=self.din("m_router_1", [D, cfg["NE"]]))
            self.ffn(3, lat, [(self.din("m_w_gate_1", [cfg["NE"], D, cfg["EDIM"]])[e],
                               self.din("m_w_up_1", [cfg["NE"], D, cfg["EDIM"]])[e],
                               self.din("m_w_down_1", [cfg["NE"], cfg["EDIM"], D])[e]) for e in range(cfg["NE"])],
                     cfg["EDIM"], moe=True)
            self.store_stream(self.X, self.dout("yT", [D, NT]), NT)
            return self.finish()

    def finish(self):
        return self.nc

    def allgather(self, pairs):
        nc = self.nc
        if self.cfg.get("fake_gather"):
            with self.phase() as (S, sb):
                for src, dst in pairs:
                    n = src.shape[0]
                    for r in range(2):
                        S.dma("sp", None, lambda e, src=src, dst=dst, r=r, n=n: e.dma_start(out=dst[r * n:(r + 1) * n, :], in_=src))
            return
        with contextlib.ExitStack() as es:
            sems = [self.es.enter_context(nc.semaphore(f"cc{self.uid}_{i}")) for i in range(len(pairs))]
            self.uid += 1
            blk = es.enter_context(nc.Block())

            def body(g):
                for (src, dst), s in zip(pairs, sems):
                    g.collective_compute("AllGather", ALU.bypass, replica_groups=[[0, 1], [2, 3], [4, 5], [6, 7]],
                                         ins=[src], outs=[dst]).then_inc(s)
                for s in sems:
                    g.wait_ge(s, 1)
            blk.gpsimd(body)

    def dump(self, name, T, shape, dt=F32):
        d = self.dout(name, shape, dt)
        with self.phase() as (S, sb):
            c = S.new_chan()
            S.dma("sp", c, lambda e: e.dma_start(out=d, in_=T))

    def dbg(self, tag):
        if self.cfg.get("stop") != tag:
            return False
        NT = self.NT
        self.dump("dbg_modc", self.MODC[:].rearrange("p l c w -> p (l c w)"), [128, len(self.layers) * 96])
        self.dump("dbg_h", self.H[:].rearrange("p k n -> p (k n)"), [128, KD * self.NTOT], BF16)
        self.dump("dbg_x", self.X[:].rearrange("p k n -> p (k n)"), [128, KD * NT])
        if hasattr(self, "XC"):
            self.dump("dbg_xc", self.XC[:].rearrange("p k n -> p (k n)"), [128, KD * NCTX])
        return True


    def load_stream(self, T, src, n):
        with self.phase() as (S, sb):
            c = S.new_chan()
            S.dma("sp", c, lambda e: e.dma_start(out=T[:, :, 0:n], in_=src.rearrange("(k p) n -> p k n", p=128)),
                  writes=["T"])

    def store_stream(self, T, dst, n):
        with self.phase() as (S, sb):
            c = S.new_chan()
            S.dma("sp", c, lambda e: e.dma_start(out=dst.rearrange("(k p) n -> p k n", p=128), in_=T[:, :, 0:n]),
                  reads=["T"])

    def prologue(self):
        nc = self.nc
        crow = self.din("crow", [2, D])
        ident_d = self.din("ident", [128, 128])
        ps = self.ps
        with self.phase() as (S, sb):
            c0 = S.new_chan()
            S.dma("sp", c0, lambda e: e.dma_start(out=self.ident[:], in_=ident_d), writes=["ident"])
            S.op("dve", lambda e: e.memset(self.ones_f[:], 1.0), writes=["ones_f"])
            S.op("dve", lambda e: e.memset(self.ones_b[:], 1.0 / D), writes=["ones_b"])
            S.op("dve", lambda e: e.memset(self.one_b[:], 1.0), writes=["one_b"])
            S.op("dve", lambda e: e.memset(self.epsc[:], EPS), writes=["epsc"])
            crow_sb = sb("crow", [2, D], F32)
            srow = sb("srow", [2, D], F32)
            scol = sb("scol", [128, KD, 2], F32)
            S.dma("sp", c0, lambda e: e.dma_start(out=crow_sb[:], in_=crow), writes=["crow"])
            S.op("act", lambda e: e.activation(out=srow[:], in_=crow_sb[:], func=AF.Silu), reads=["crow"], writes=["srow"])
            for k in range(KD):
                S.op("pe", lambda e, k=k: e.matmul(ps[0][:, 2 * k:2 * k + 2], lhsT=srow[0:2, k * 128:(k + 1) * 128],
                                                   rhs=self.ident[0:2, 0:2], start=True, stop=True),
                     reads=["srow", "ident"], writes=[("ps", 0)])
            S.op("dve", lambda e: e.tensor_copy(out=scol[:].rearrange("p k w -> p (k w)"), in_=ps[0][:, 0:16]),
                 reads=[("ps", 0)], writes=["scol"])
            wb = [sb(f"adaw{i}", [128, KD, 512], F32) for i in range(2)]
            R = sb("R", [2, 6 * D], F32)
            adab = sb("adab", [2, 6 * D], F32)
            g2 = sb("g2", [2, 2, D], F32)
            cnt = 0
            for li, layer in enumerate(self.layers):
                adaw = self.din(f"ada_w_{layer}", [D, 6 * D])
                adab_d = self.din(f"ada_b_{layer}", [1, 6 * D])
                ng_d = self.din(f"norm_g_{layer}", [1, 2 * D])
                for r in range(2):
                    S.dma("sp", c0, lambda e, r=r, adab_d=adab_d: e.dma_start(out=adab[r:r + 1, :], in_=adab_d), writes=["adab"])
                    S.dma("sp", c0, lambda e, r=r, ng_d=ng_d: e.dma_start(out=g2[r:r + 1, :, :].rearrange("p a d -> p (a d)"), in_=ng_d),
                          writes=["g2"])
                for j in range(12):
                    b = cnt % 2
                    cnt += 1
                    S.dma("sp", None, lambda e, b=b, j=j, adaw=adaw: e.dma_start(
                        out=wb[b][:], in_=adaw[:, j * 512:(j + 1) * 512].rearrange("(k p) n -> p k n", p=128)), writes=[("wb", b)])
                    pb = 1 + (j % 2)
                    for k in range(KD):
                        S.op("pe", lambda e, b=b, k=k, pb=pb: e.matmul(ps[pb][0:2, :], lhsT=scol[:, k, :], rhs=wb[b][:, k, :],
                                                                      start=(k == 0), stop=(k == KD - 1)),
                             reads=["scol", ("wb", b)], writes=[("ps", pb)])
                    S.op("dve", lambda e, j=j, pb=pb: e.tensor_tensor(out=R[:, j * 512:(j + 1) * 512], in0=ps[pb][0:2, :],
                                                                      in1=adab[:, j * 512:(j + 1) * 512], op=ALU.add),
                         reads=[("ps", pb), "adab"], writes=["R"])
                for s in range(2):
                    sl = slice((3 * s + 1) * D, (3 * s + 2) * D)
                    S.op("dve", lambda e, s=s, sl=sl: e.scalar_tensor_tensor(out=R[:, sl], in0=R[:, sl], scalar=1.0, in1=g2[:, s, :],
                                                                             op0=ALU.add, op1=ALU.mult),
                         reads=["R", "g2"], writes=["R"])
                for c in range(48):
                    S.op("pe", lambda e, c=c: e.matmul(ps[3][:, 2 * c:2 * c + 2], lhsT=R[0:2, c * 128:(c + 1) * 128],
                                                       rhs=self.ident[0:2, 0:2], start=True, stop=True),
                         reads=["R", "ident"], writes=[("ps", 3)])
                S.op("dve", lambda e, li=li: e.tensor_copy(out=self.MODC[:, li, :, :].rearrange("p c w -> p (c w)"), in_=ps[3][:, 0:96]),
                     reads=[("ps", 3)], writes=["MODC"])

    def norm(self, layer, sub, tiles, router=None, out32=None, out32_off=0):
        ps = self.ps
        NE = self.cfg["NE"]
        with self.phase() as (S, sb):
            sq = [sb(f"sq{i}", [128, KD, 512], BF16) for i in range(2)]
            rstd = [sb(f"rstd{i}", [128, 512], F32) for i in range(2)]
            tt = [sb(f"tt{i}", [128, KD, 512], F32) for i in range(2)]
            if router is not None:
                RW = sb("RW", [128, KD, NE], F32)
                h32 = [sb(f"h32{i}", [128, KD, 512], F32) for i in range(2)]
                c0 = S.new_chan()
                S.dma("sp", c0, lambda e: e.dma_start(out=RW[:], in_=router.rearrange("(k p) e -> p k e", p=128)), writes=["RW"])
            for ti, (t0, n, st) in enumerate(tiles):
                b = ti % 2
                which = 1 if st == "XC" else 0
                src = self.XC if st == "XC" else self.X
                s0 = t0 - self.NT if st == "XC" else t0
                S.op("act", lambda e, b=b, src=src, s0=s0, n=n: e.activation(out=sq[b][:, :, 0:n], in_=src[:, :, s0:s0 + n], func=AF.Square),
                     reads=[("X", st, s0)], writes=[("sq", b)])
                for k in range(KD):
                    S.op("pe", lambda e, b=b, k=k, n=n: e.matmul(ps[b][:, 0:n], lhsT=self.ones_b[:], rhs=sq[b][:, k, 0:n],
                                                                 start=(k == 0), stop=(k == KD - 1)),
                         reads=[("sq", b)], writes=[("ps", b)])
                S.op("act", lambda e, b=b, n=n: e.activation(out=rstd[b][:, 0:n], in_=ps[b][:, 0:n], func=AF.Sqrt, bias=self.epsc[:, 0:1], scale=1.0),
                     reads=[("ps", b)], writes=[("rstd", b)])
                S.op("dve", lambda e, b=b, n=n: e.reciprocal(out=rstd[b][:, 0:n], in_=rstd[b][:, 0:n]),
                     reads=[("rstd", b)], writes=[("rstd", b)])
                for k in range(KD):
                    S.op("dve", lambda e, b=b, k=k, n=n, src=src, s0=s0, which=which: e.scalar_tensor_tensor(
                        out=tt[b][:, k, 0:n], in0=src[:, k, s0:s0 + n], scalar=self.modc(layer, 3 * sub + 1, k, which),
                        in1=rstd[b][:, 0:n], op0=ALU.mult, op1=ALU.mult),
                        reads=[("X", st, s0), ("rstd", b)], writes=[("tt", b, k)])
                    if out32 is not None:
                        S.op("act", lambda e, b=b, k=k, n=n, t0=t0, which=which: e.activation(
                            out=out32[:, k, out32_off + t0:out32_off + t0 + n], in_=tt[b][:, k, 0:n], func=AF.Identity,
                            bias=self.modc(layer, 3 * sub, k, which), scale=1.0),
                            reads=[("tt", b, k)], writes=[("H32P", t0)])
                        continue
                    S.op("act", lambda e, b=b, k=k, n=n, t0=t0, which=which: e.activation(
                        out=self.H[:, k, t0:t0 + n], in_=tt[b][:, k, 0:n], func=AF.Identity,
                        bias=self.modc(layer, 3 * sub, k, which), scale=1.0),
                        reads=[("tt", b, k)], writes=[("H", t0)])
                    if router is not None:
                        S.op("dve", lambda e, b=b, k=k, n=n, which=which: e.tensor_scalar(
                            out=h32[b][:, k, 0:n], in0=tt[b][:, k, 0:n], scalar1=self.modc(layer, 3 * sub, k, which), scalar2=None,
                            op0=ALU.add),
                            reads=[("tt", b, k)], writes=[("h32", b)])
                if router is not None:
                    for c in range(n // 128):
                        ch = t0 // 128 + c
                        pb = 2 + (ch % 2)
                        for k in range(KD):
                            S.op("pe", lambda e, b=b, k=k, c=c, pb=pb: e.matmul(ps[pb][:, 0:NE], lhsT=h32[b][:, k, c * 128:(c + 1) * 128],
                                                                               rhs=RW[:, k, :], start=(k == 0), stop=(k == KD - 1)),
                                 reads=[("h32", b), "RW"], writes=[("ps", pb)])
                        S.op("act", lambda e, ch=ch, pb=pb: e.activation(out=self.LOG[:, ch, :], in_=ps[pb][:, 0:NE], func=AF.Copy),
                             reads=[("ps", pb)], writes=["LOG"])
            if router is not None:
                self.route(S, sb)

    def route(self, S, sb):
        NE = self.cfg["NE"]
        nch = self.NT // 128
        LOG = self.LOG
        CMB = self.CMB
        v1 = sb("v1", [128, nch], F32)
        v2 = sb("v2", [128, nch], F32)
        m1 = sb("m1", [128, nch, NE], F32)
        m2 = sb("m2", [128, nch, NE], F32)
        L2 = sb("L2", [128, nch, NE], F32)
        dd = sb("dd", [128, nch], F32)
        e2 = sb("e2", [128, nch], F32)
        g1 = sb("g1", [128, nch], F32)
        g2 = sb("g2", [128, nch], F32)

        def bc(t):
            return t[:].unsqueeze(2).broadcast_to([128, nch, NE])
        S.op("dve", lambda e: e.tensor_reduce(out=v1[:], in_=LOG[:], axis=AX.X, op=ALU.max), reads=["LOG"], writes=["v1"])
        S.op("dve", lambda e: e.tensor_tensor(out=m1[:], in0=LOG[:], in1=bc(v1), op=ALU.is_equal), reads=["LOG", "v1"], writes=["m1"])
        S.op("dve", lambda e: e.scalar_tensor_tensor(out=L2[:], in0=m1[:], scalar=-1e30, in1=LOG[:], op0=ALU.mult, op1=ALU.add),
             reads=["m1", "LOG"], writes=["L2"])
        S.op("dve", lambda e: e.tensor_reduce(out=v2[:], in_=L2[:], axis=AX.X, op=ALU.max), reads=["L2"], writes=["v2"])
        S.op("dve", lambda e: e.tensor_tensor(out=m2[:], in0=L2[:], in1=bc(v2), op=ALU.is_equal), reads=["L2", "v2"], writes=["m2"])
        S.op("dve", lambda e: e.tensor_tensor(out=dd[:], in0=v2[:], in1=v1[:], op=ALU.subtract), reads=["v1", "v2"], writes=["dd"])
        S.op("act", lambda e: e.activation(out=e2[:], in_=dd[:], func=AF.Exp), reads=["dd"], writes=["e2"])
        S.op("dve", lambda e: e.tensor_scalar(out=g2[:], in0=e2[:], scalar1=1.0, scalar2=None, op0=ALU.add), reads=["e2"], writes=["g2"])
        S.op("dve", lambda e: e.reciprocal(out=g1[:], in_=g2[:]), reads=["g2"], writes=["g1"])
        S.op("dve", lambda e: e.tensor_tensor(out=g2[:], in0=e2[:], in1=g1[:], op=ALU.mult), reads=["e2", "g1"], writes=["g2"])
        S.op("dve", lambda e: e.tensor_tensor(out=m1[:], in0=m1[:], in1=bc(g1), op=ALU.mult), reads=["m1", "g1"], writes=["m1"])
        S.op("dve", lambda e: e.tensor_tensor(out=m2[:], in0=m2[:], in1=bc(g2), op=ALU.mult), reads=["m2", "g2"], writes=["m2"])
        S.op("dve", lambda e: e.tensor_tensor(out=CMB[:], in0=m1[:], in1=m2[:], op=ALU.add), reads=["m1", "m2"], writes=["CMB"])

    def ffn(self, layer, tiles, experts, F, moe=False):
        ps = self.ps
        NT = self.NT
        nfc = F // 128
        slices = _fslices(nfc)
        with self.phase() as (S, sb):
            WG = [sb(f"WG{i}", [128, KD, 512], BF16) for i in range(3)]
            WU = [sb(f"WU{i}", [128, KD, 512], BF16) for i in range(3)]
            WD = [sb(f"WD{i}", [128, 4, D], BF16) for i in range(3)]
            A = [sb(f"A{i}", [128, 4, 512], BF16) for i in range(2)]
            sg = [sb(f"sg{i}", [128, 512], BF16 if not moe else F32) for i in range(2)]
            wch = [S.new_chan() for _ in range(3)]
            if moe:
                BE = [sb(f"BE{i}", [128, NT], F32) for i in range(2)]
                Dg = [sb(f"Dg{i}", [128, 128], F32) for i in range(2)]
            work = [(e, s) for e in range(len(experts)) for s in range(len(slices))]

            def load(q):
                e, s = work[q]
                f0, nf = slices[s]
                wg, wu, wd = experts[e]
                r = q % 3
                S.dma("pool", wch[r], lambda en: en.dma_start(
                    out=WG[r][:, :, 0:nf * 128], in_=wg[:, f0 * 128:(f0 + nf) * 128].rearrange("(k p) n -> p k n", p=128)),
                    writes=[("WG", r)])
                S.dma("pool", wch[r], lambda en: en.dma_start(
                    out=WU[r][:, :, 0:nf * 128], in_=wu[:, f0 * 128:(f0 + nf) * 128].rearrange("(k p) n -> p k n", p=128)),
                    writes=[("WU", r)])
                S.dma("pool", wch[r], lambda en: en.dma_start(
                    out=WD[r][:, 0:nf, :], in_=wd[f0 * 128:(f0 + nf) * 128, :].rearrange("(c p) n -> p c n", p=128)),
                    writes=[("WD", r)])

            def build_be(e):
                eb = e % 2
                for ch in range(NT // 128):
                    db = ch % 2
                    pb = 6 + ((ch // 4) % 2)
                    S.op("dve", lambda en, ch=ch, db=db, e=e: en.tensor_scalar(
                        out=Dg[db][:], in0=self.ident[:], scalar1=self.CMB[:, ch, e:e + 1], scalar2=None, op0=ALU.mult),
                        reads=["CMB", "ident"], writes=[("Dg", db)])
                    S.op("pe", lambda en, ch=ch, db=db, pb=pb: en.matmul(ps[pb][:, (ch % 4) * 128:(ch % 4 + 1) * 128], lhsT=self.ones_f[:],
                                                                        rhs=Dg[db][:], start=True, stop=True),
                         reads=[("Dg", db)], writes=[("ps", pb)])
                    if ch % 4 == 3:
                        t0 = (ch // 4) * 512
                        S.op("act", lambda en, eb=eb, pb=pb, t0=t0: en.activation(out=BE[eb][:, t0:t0 + 512], in_=ps[pb][:], func=AF.Copy),
                             reads=[("ps", pb)], writes=[("BE", eb, t0)])

            items = [(q, ti) for q in range(len(work)) for ti in range(len(tiles))]
            cnt = {"gu": 0}

            def gu(idx):
                q, ti = items[idx]
                e, s = work[q]
                f0, nf = slices[s]
                t0, n, st = tiles[ti]
                r = q % 3
                ab = idx % 2
                for fc in range(nf):
                    j = cnt["gu"] % 2
                    cnt["gu"] += 1
                    for k in range(KD):
                        S.op("pe", lambda en, r=r, fc=fc, k=k, j=j, t0=t0, n=n: en.matmul(
                            ps[j][:, 0:n], lhsT=WG[r][:, k, fc * 128:(fc + 1) * 128], rhs=self.H[:, k, t0:t0 + n],
                            start=(k == 0), stop=(k == KD - 1)), reads=[("WG", r), ("H", t0)], writes=[("ps", j)])
                    for k in range(KD):
                        S.op("pe", lambda en, r=r, fc=fc, k=k, j=j, t0=t0, n=n: en.matmul(
                            ps[2 + j][:, 0:n], lhsT=WU[r][:, k, fc * 128:(fc + 1) * 128], rhs=self.H[:, k, t0:t0 + n],
                            start=(k == 0), stop=(k == KD - 1)), reads=[("WU", r), ("H", t0)], writes=[("ps", 2 + j)])
                    S.op("act", lambda en, j=j, n=n: en.activation(out=sg[j][:, 0:n], in_=ps[j][:, 0:n], func=AF.Silu),
                         reads=[("ps", j)], writes=[("sg", j)])
                    if not moe:
                        S.op("dve", lambda en, j=j, n=n, ab=ab, fc=fc: en.tensor_tensor(
                            out=A[ab][:, fc, 0:n], in0=sg[j][:, 0:n], in1=ps[2 + j][:, 0:n], op=ALU.mult),
                            reads=[("sg", j), ("ps", 2 + j)], writes=[("A", ab)])
                    else:
                        S.op("dve", lambda en, j=j, n=n: en.tensor_tensor(
                            out=sg[j][:, 0:n], in0=sg[j][:, 0:n], in1=ps[2 + j][:, 0:n], op=ALU.mult),
                            reads=[("sg", j), ("ps", 2 + j)], writes=[("sg", j)])
                        S.op("dve", lambda en, j=j, n=n, ab=ab, fc=fc, e=e, t0=t0: en.tensor_tensor(
                            out=A[ab][:, fc, 0:n], in0=sg[j][:, 0:n], in1=BE[e % 2][:, t0:t0 + n], op=ALU.mult),
                            reads=[("sg", j), ("BE", e % 2, t0)], writes=[("A", ab)])

            def down(idx):
                q, ti = items[idx]
                e, s = work[q]
                f0, nf = slices[s]
                t0, n, st = tiles[ti]
                r = q % 3
                ab = idx % 2
                which = 1 if st == "XC" else 0
                dst = self.XC if st == "XC" else self.X
                s0 = t0 - NT if st == "XC" else t0
                for dc in range(KD):
                    pb = 4 + (dc % 4)
                    for fc in range(nf):
                        S.op("pe", lambda en, r=r, fc=fc, dc=dc, pb=pb, ab=ab, n=n, nf=nf: en.matmul(
                            ps[pb][:, 0:n], lhsT=WD[r][:, fc, dc * 128:(dc + 1) * 128], rhs=A[ab][:, fc, 0:n],
                            start=(fc == 0), stop=(fc == nf - 1)), reads=[("WD", r), ("A", ab)], writes=[("ps", pb)])
                    S.op("dve", lambda en, dc=dc, pb=pb, n=n, dst=dst, s0=s0, which=which: en.scalar_tensor_tensor(
                        out=dst[:, dc, s0:s0 + n], in0=ps[pb][:, 0:n], scalar=self.modc(layer, 5, dc, which),
                        in1=dst[:, dc, s0:s0 + n], op0=ALU.mult, op1=ALU.add),
                        reads=[("ps", pb), ("X", st, s0)], writes=[("X", st, s0)])

            load(0)
            if len(work) > 1:
                load(1)
            if moe:
                build_be(0)
            for idx in range(len(items)):
                q, ti = items[idx]
                gu(idx)
                if idx > 0:
                    down(idx - 1)
                if ti == 0:
                    if q + 2 < len(work):
                        load(q + 2)
                    e, s = work[q]
                    if moe and s == min(1, len(slices) - 1) and e + 1 < len(experts):
                        build_be(e + 1)
            down(len(items) - 1)

    def row_to_cols(self, S, row, nchunks, dst, pb, rname, wname):
        ps = self.ps
        for c in range(nchunks):
            S.op("pe", lambda e, c=c: e.matmul(ps[pb][:, c:c + 1], lhsT=row[0:1, c * 128:(c + 1) * 128], rhs=self.ident[0:1, 0:1],
                                               start=True, stop=True), reads=[rname, "ident"], writes=[("ps", pb)])
        S.op("dve", lambda e: e.tensor_copy(out=dst[:, 0:nchunks], in_=ps[pb][:, 0:nchunks]), reads=[("ps", pb)], writes=[wname])

    def gmlp(self, layer, j, tiles):
        ps = self.ps
        NT = self.NT
        w_in = self.din(f"a_w_in_{j}", [D, 4096])
        v_g = self.din(f"a_v_g_{j}", [1, 2048])
        w_s = self.din(f"a_ws_{j}", [8, 128, 128])
        b_s = self.din(f"a_bs_{j}", [1, 1024])
        w_out = self.din(f"a_w_out_{j}", [2048, D])
        with self.phase() as (S, sb):
            nwin = 3 if layer == 3 else 2
            WIN = [sb(f"WIN{i}", [128, KD, 512], BF16) for i in range(nwin)]
            WOUT = [sb(f"WOUT{i}", [128, 16, 128], BF16) for i in range(2)]
            U = sb("U", [128, 16, 512], BF16)
            vg = sb("vg", [128, 4, 2048], F32)
            vn = [sb(f"vn{i}", [128, 2048], BF16) for i in range(2)]
            ssq = sb("ssq", [128, 4], F32)
            VGC = sb("VGC", [128, 16], F32)
            BSB = sb("BSB", [128, 8, 128], F32)
            WST = sb("WST", [128, 8, 128], BF16)
            mx = [sb(f"mx{i}", [128, 4, 128], F32) for i in range(2)]
            WS = vg[:, 0, 0:1024].rearrange("p (g s) -> p g s", g=8)
            vrow = vg[0:1, 1, 0:2048]
            c0 = S.new_chan()
            wch = [S.new_chan() for _ in range(2)]
            och = [S.new_chan() for _ in range(2)]
            S.dma("sp", c0, lambda e: e.dma_start(out=vrow, in_=v_g), writes=[("vg", 1)])
            S.dma("sp", c0, lambda e: e.dma_start(out=BSB[:].rearrange("p g t -> p (g t)"), in_=b_s.broadcast_to([128, 1024])), writes=["BSB"])
            S.dma("sp", c0, lambda e: e.dma_start(out=WS, in_=w_s.rearrange("g t s -> t g s")), writes=[("vg", 0)])
            for g in range(8):
                pb = 6 + g // 4
                S.op("pe", lambda e, g=g, pb=pb: e.matmul(ps[pb][:, (g % 4) * 128:(g % 4 + 1) * 128], lhsT=WS[:, g, :], rhs=self.ident[:],
                                                          start=True, stop=True), reads=[("vg", 0), "ident"], writes=[("ps", pb)])
            for h in range(2):
                S.op("dve", lambda e, h=h: e.tensor_copy(out=WST[:, 4 * h:4 * h + 4, :].rearrange("p g t -> p (g t)"), in_=ps[6 + h][:]),
                     reads=[("ps", 6 + h)], writes=["WST"])
            self.row_to_cols(S, vrow, 16, VGC, 5, ("vg", 1), "VGC")
            wcnt = {"in": 0, "out": 0, "pv": 0, "pm": 0}

            def load_in(sl):
                r = wcnt["in"] % nwin
                wcnt["in"] += 1
                S.dma("pool", None, lambda e, r=r, sl=sl: e.dma_start(
                    out=WIN[r][:], in_=w_in[:, sl * 512:(sl + 1) * 512].rearrange("(k p) n -> p k n", p=128)), writes=[("WIN", r)])
                return r

            def load_out(dc):
                r = wcnt["out"] % 2
                wcnt["out"] += 1
                S.dma("pool", och[r], lambda e, r=r, dc=dc: e.dma_start(
                    out=WOUT[r][:], in_=w_out[:, dc * 128:(dc + 1) * 128].rearrange("(c p) n -> p c n", p=128)), writes=[("WOUT", r)])
                return r

            for (t0, n, st) in tiles:
                which = 1 if st == "XC" else 0
                dst = self.XC if st == "XC" else self.X
                s0 = t0 - NT if st == "XC" else t0
                nch = n // 128
                for sl in range(4):
                    r = load_in(4 + sl)
                    for c in range(nch):
                        pb = wcnt["pv"] % 2
                        wcnt["pv"] += 1
                        for k in range(KD):
                            S.op("pe", lambda e, r=r, k=k, c=c, pb=pb, t0=t0: e.matmul(
                                ps[pb][:], lhsT=self.H[:, k, t0 + c * 128:t0 + (c + 1) * 128], rhs=WIN[r][:, k, :],
                                start=(k == 0), stop=(k == KD - 1)), reads=[("WIN", r), ("H", t0)], writes=[("ps", pb)])
                        S.op("act", lambda e, c=c, sl=sl, pb=pb: e.activation(out=vg[:, c, sl * 512:(sl + 1) * 512], in_=ps[pb][:],
                                                                            func=AF.Gelu_apprx_tanh),
                             reads=[("ps", pb)], writes=[("vg", c)])
                for sl in range(4):
                    r = load_in(sl)
                    for fc in range(4):
                        pb = 2 + (fc % 2)
                        for k in range(KD):
                            S.op("pe", lambda e, r=r, k=k, fc=fc, pb=pb, t0=t0, n=n: e.matmul(
                                ps[pb][:, 0:n], lhsT=WIN[r][:, k, fc * 128:(fc + 1) * 128], rhs=self.H[:, k, t0:t0 + n],
                                start=(k == 0), stop=(k == KD - 1)), reads=[("WIN", r), ("H", t0)], writes=[("ps", pb)])
                        S.op("act", lambda e, sl=sl, fc=fc, pb=pb, n=n: e.activation(out=U[:, sl * 4 + fc, 0:n], in_=ps[pb][:, 0:n],
                                                                                  func=AF.Gelu_apprx_tanh),
                             reads=[("ps", pb)], writes=["U"])
                for c in range(nch):
                    S.op("act", lambda e, c=c: e.activation(out=vn[c % 2][:], in_=vg[:, c, :], func=AF.Square, accum_out=ssq[:, c:c + 1]),
                         reads=[("vg", c)], writes=[("vn", c % 2), "ssq"])
                S.op("act", lambda e, nch=nch: e.activation(out=ssq[:, 0:nch], in_=ssq[:, 0:nch], func=AF.Sqrt, bias=self.epsc[:, 0:1], scale=1.0 / 2048),
                     reads=["ssq"], writes=["ssq"])
                S.op("dve", lambda e, nch=nch: e.reciprocal(out=ssq[:, 0:nch], in_=ssq[:, 0:nch]), reads=["ssq"], writes=["ssq"])
                for c in range(nch):
                    vb = c % 2
                    S.op("dve", lambda e, c=c, vb=vb: e.tensor_scalar(out=vn[vb][:], in0=vg[:, c, :], scalar1=ssq[:, c:c + 1], scalar2=None,
                                                                     op0=ALU.mult),
                         reads=[("vg", c), "ssq"], writes=[("vn", vb)])
                    for q4 in range(4):
                        pb = 4 + (wcnt["pm"] % 2)
                        mb = wcnt["pm"] % 2
                        wcnt["pm"] += 1
                        for f4 in range(4):
                            fc = q4 * 4 + f4
                            S.op("pe", lambda e, vb=vb, fc=fc, f4=f4, pb=pb: e.matmul(
                                ps[pb][:, f4 * 128:(f4 + 1) * 128], lhsT=vn[vb][:, fc * 128:(fc + 1) * 128], rhs=WST[:, fc // 2, :],
                                start=True, stop=True), reads=[("vn", vb), "WST"], writes=[("ps", pb)])
                        for f4 in range(4):
                            fc = q4 * 4 + f4
                            S.op("dve", lambda e, fc=fc, f4=f4, pb=pb, mb=mb: e.scalar_tensor_tensor(
                                out=mx[mb][:, f4, :], in0=ps[pb][:, f4 * 128:(f4 + 1) * 128], scalar=VGC[:, fc:fc + 1],
                                in1=BSB[:, fc // 2, :], op0=ALU.mult, op1=ALU.add),
                                reads=[("ps", pb), "BSB", "VGC"], writes=[("mx", mb)])
                        S.op("dve", lambda e, q4=q4, c=c, mb=mb: e.tensor_tensor(
                            out=U[:, 4 * q4:4 * q4 + 4, c * 128:(c + 1) * 128], in0=U[:, 4 * q4:4 * q4 + 4, c * 128:(c + 1) * 128],
                            in1=mx[mb][:], op=ALU.mult), reads=[("mx", mb), "U"], writes=["U"])
                if self.cfg.get("stop") == "gmlp_dbg":
                    cd = S.new_chan()
                    for nm, T, shp, dt in [("d_vg", vg[:].rearrange("p c f -> p (c f)"), [128, 4 * 2048], F32), ("d_ssq", ssq[:], [128, 4], F32),
                                           ("d_U", U[:].rearrange("p c f -> p (c f)"), [128, 16 * 512], BF16), ("d_VGC", VGC[:], [128, 16], F32),
                                           ("d_WST", WST[:].rearrange("p c f -> p (c f)"), [128, 1024], BF16),
                                           ("d_vn", vn[1][:], [128, 2048], BF16)]:
                        dd = self.dout(nm, shp, dt)
                        S.dma("sp", cd, lambda e, dd=dd, T=T: e.dma_start(out=dd, in_=T),
                              reads=[("vg", c) for c in range(4)] + ["ssq", "U", "VGC", "WST", ("vn", 0), ("vn", 1)])
                    break
                for dc in range(KD):
                    r = load_out(dc)
                    pb = 6 + (dc % 2)
                    for fc in range(16):
                        S.op("pe", lambda e, r=r, fc=fc, pb=pb, n=n: e.matmul(
                            ps[pb][:, 0:n], lhsT=WOUT[r][:, fc, :], rhs=U[:, fc, 0:n],
                            start=(fc == 0), stop=(fc == 15)), reads=[("WOUT", r), "U"], writes=[("ps", pb)])
                    S.op("dve", lambda e, dc=dc, pb=pb, n=n, dst=dst, s0=s0, which=which: e.scalar_tensor_tensor(
                        out=dst[:, dc, s0:s0 + n], in0=ps[pb][:, 0:n], scalar=self.modc(layer, 2, dc, which),
                        in1=dst[:, dc, s0:s0 + n], op0=ALU.mult, op1=ALU.add),
                        reads=[("ps", pb), ("X", st, s0)], writes=[("X", st, s0)])
                if self.cfg.get("stop") == "gmlp_dbg2":
                    cd = S.new_chan()
                    for nm, T, shp, dt in [("d_W0", WOUT[0][:].rearrange("p c f -> p (c f)"), [128, 2048], BF16),
                                           ("d_W1", WOUT[1][:].rearrange("p c f -> p (c f)"), [128, 2048], BF16),
                                           ("d_X", self.X[:, :, 0:512], [128, 8, 512], F32)]:
                        dd = self.dout(nm, shp, dt)
                        S.dma("sp", cd, lambda e, dd=dd, T=T: e.dma_start(out=dd, in_=T),
                              reads=[("WOUT", 0), ("WOUT", 1), ("X", "X", 0)])
                    break

    def qkv(self, want_q=True, want_kv=True, nbuf=2):
        ps = self.ps
        NT = self.NT
        wqkv = self.din("b_w_qkv_0", [D, 1536])
        qg = self.din("b_q_g_0", [64, 1])
        kg = self.din("b_k_g_0", [64, 1])
        cosd = self.din("cosT", [128, NT])
        sind = self.din("sinT", [128, NT])
        rotd = self.din("rotm", [128, 128])
        blkd = self.din("blk64", [128, 128])
        with self.phase() as (S, sb):
            W = sb("Wqkv", [128, KD, 1536], BF16)
            COS = sb("COS", [128, NT], F32)
            SIN = sb("SIN", [128, NT], F32)
            ROT = sb("ROT", [128, 128], F32)
            BLKf = sb("BLKf", [128, 128], F32)
            BLK = sb("BLK", [128, 128], BF16)
            GQ = sb("GQ", [128, 1], F32)
            GK = sb("GK", [128, 1], F32)
            qf = [sb(f"qf{i}", [128, 512], F32) for i in range(nbuf)]
            sq = [sb(f"sqq{i}", [128, 512], BF16) for i in range(nbuf)]
            rs = [sb(f"rsq{i}", [128, 512], F32) for i in range(nbuf)]
            t1 = [sb(f"t1{i}", [128, 512], F32) for i in range(nbuf)]
            c0 = S.new_chan()
            c1 = S.new_chan()
            c2 = S.new_chan()
            for i in range(3):
                S.dma("pool", c0, lambda e, i=i: e.dma_start(out=W[:, :, i * 512:(i + 1) * 512],
                                                            in_=wqkv[:, i * 512:(i + 1) * 512].rearrange("(k p) n -> p k n", p=128)),
                      writes=[("W", i)])
            S.dma("sp", c1, lambda e: e.dma_start(out=COS[:], in_=cosd), writes=["COS"])
            S.dma("sp", c1, lambda e: e.dma_start(out=SIN[:], in_=sind), writes=["SIN"])
            S.dma("sp", c1, lambda e: e.dma_start(out=ROT[:], in_=rotd), writes=["ROT"])
            S.dma("sp", c1, lambda e: e.dma_start(out=BLKf[:], in_=blkd), writes=["BLKf"])
            for h in range(2):
                S.dma("sp", c1, lambda e, h=h: e.dma_start(out=GQ[h * 64:(h + 1) * 64, :], in_=qg), writes=["GQ"])
                S.dma("sp", c1, lambda e, h=h: e.dma_start(out=GK[h * 64:(h + 1) * 64, :], in_=kg), writes=["GK"])
            S.op("dve", lambda e: e.tensor_copy(out=BLK[:], in_=BLKf[:]), reads=["BLKf"], writes=["BLK"])
            cnt = {"n": 0}

            def qk_chunk(t0, n, col0, gcol, rope, cpos, out_ap, wres):
                b = cnt["n"] % nbuf
                cnt["n"] += 1
                pq, pm, pr = ps[b], ps[2 + b], ps[4 + b]
                wi = col0 // 512
                for k in range(KD):
                    S.op("pe", lambda e, k=k: e.matmul(pq[:, 0:n], lhsT=W[:, k, col0:col0 + 128], rhs=self.H[:, k, t0:t0 + n],
                                                       start=(k == 0), stop=(k == KD - 1)), reads=[("W", wi), ("H", t0)], writes=[("ps", b)])
                S.op("act", lambda e: e.activation(out=qf[b][:, 0:n], in_=pq[:, 0:n], func=AF.Copy), reads=[("ps", b)], writes=[("qf", b)])
                S.op("act", lambda e: e.activation(out=sq[b][:, 0:n], in_=pq[:, 0:n], func=AF.Square), reads=[("ps", b)], writes=[("sq", b)])
                S.op("pe", lambda e: e.matmul(pm[:, 0:n], lhsT=BLK[:], rhs=sq[b][:, 0:n], start=True, stop=True),
                     reads=[("sq", b), "BLK"], writes=[("ps", 2 + b)])
                S.op("act", lambda e: e.activation(out=rs[b][:, 0:n], in_=pm[:, 0:n], func=AF.Sqrt, bias=self.epsc[:, 0:1], scale=1.0),
                     reads=[("ps", 2 + b)], writes=[("rs", b)])
                S.op("dve", lambda e: e.reciprocal(out=rs[b][:, 0:n], in_=rs[b][:, 0:n]),
                     reads=[("rs", b)], writes=[("rs", b)])
                S.op("dve", lambda e: e.scalar_tensor_tensor(out=qf[b][:, 0:n], in0=qf[b][:, 0:n], scalar=gcol[:, 0:1], in1=rs[b][:, 0:n],
                                                             op0=ALU.mult, op1=ALU.mult),
                     reads=[("qf", b), ("rs", b), "GQ", "GK"], writes=[("qf", b)])
                if not rope:
                    S.op("act", lambda e: e.activation(out=out_ap, in_=qf[b][:, 0:n], func=AF.Copy), reads=[("qf", b)], writes=[wres])
                    return
                S.op("pe", lambda e: e.matmul(pr[:, 0:n], lhsT=ROT[:], rhs=qf[b][:, 0:n], start=True, stop=True),
                     reads=[("qf", b), "ROT"], writes=[("ps", 4 + b)])
                S.op("dve", lambda e: e.tensor_tensor(out=t1[b][:, 0:n], in0=qf[b][:, 0:n], in1=COS[:, cpos:cpos + n], op=ALU.mult),
                     reads=[("qf", b), "COS"], writes=[("t1", b)])
                S.op("dve", lambda e: e.tensor_tensor(out=rs[b][:, 0:n], in0=pr[:, 0:n], in1=SIN[:, cpos:cpos + n], op=ALU.mult),
                     reads=[("ps", 4 + b), "SIN"], writes=[("rs", b)])
                S.op("dve", lambda e: e.tensor_tensor(out=out_ap, in0=t1[b][:, 0:n], in1=rs[b][:, 0:n], op=ALU.add),
                     reads=[("t1", b), ("rs", b)], writes=[wres])

            if want_kv:
                KM = sb("KM", [128, 2, NT], BF16)
                KC = sb("KC", [128, 2, NCTX], BF16)
                VM = sb("VM", [128, self.NTOT // 128, 256], BF16)
                for t0 in range(0, NT, 512):
                    for kc in range(2):
                        qk_chunk(t0, 512, 1024 + kc * 128, GK, True, t0, KM[:, kc, t0:t0 + 512], "KM")
                for kc in range(2):
                    qk_chunk(NT, NCTX, 1024 + kc * 128, GK, False, 0, KC[:, kc, :], "KC")
                for c in range(self.NTOT // 128):
                    pb = 6 + (c % 2)
                    for k in range(KD):
                        S.op("pe", lambda e, k=k, c=c, pb=pb: e.matmul(ps[pb][:, 0:256], lhsT=self.H[:, k, c * 128:(c + 1) * 128],
                                                                      rhs=W[:, k, 1280:1536], start=(k == 0), stop=(k == KD - 1)),
                             reads=[("W", 2), ("H", (c * 128) // 512 * 512)], writes=[("ps", pb)])
                    S.op("act", lambda e, c=c, pb=pb: e.activation(out=VM[:, c, :], in_=ps[pb][:, 0:256], func=AF.Copy),
                         reads=[("ps", pb)], writes=["VM"])
                nlc = NT // 128
                S.dma("sp", c2, lambda e: e.dma_start(out=self.kT_mine.rearrange("(k p) n -> p k n", p=128), in_=KM[:]), reads=["KM"])
                S.dma("sp", c2, lambda e: e.dma_start(out=self.kcT.rearrange("(k p) n -> p k n", p=128), in_=KC[:]), reads=["KC"])
                S.dma("sp", c2, lambda e: e.dma_start(out=self.v_mine.rearrange("(c p) f -> p c f", p=128), in_=VM[:, 0:nlc, :]), reads=["VM"])
                S.dma("sp", c2, lambda e: e.dma_start(out=self.vc.rearrange("(c p) f -> p c f", p=128), in_=VM[:, nlc:nlc + 2, :]), reads=["VM"])
            if want_q:
                for t0 in range(0, NT, 512):
                    for c in range(KD):
                        qk_chunk(t0, 512, c * 128, GQ, True, t0, self.QT[:, c, t0:t0 + 512], ("QT", t0))

    def attention(self):
        ps = self.ps
        NT = self.NT
        NK = 2 * NT + NCTX
        nkc = NK // 128
        OT = self.H
        with self.phase() as (S, sb):
            KDp = [sb(f"KD{i}", [128, NK], BF16) for i in range(2)]
            VA = [sb(f"VA{i}", [128, nkc, 128], BF16) for i in range(2)]
            Pm = [sb(f"Pm{i}", [128, 512], BF16) for i in range(5)]
            rinv = [sb(f"rinv{i}", [64, 512], F32) for i in range(2)]
            kch = [S.new_chan() for _ in range(2)]
            cnt = {"s": 0, "o": 0}
            for i in range(2):
                S.op("dve", lambda e, i=i: e.memset(VA[i][:, :, 64:128], 1.0), writes=[("VA", i)])
            for g in range(4):
                gb = g % 2
                for half in range(2):
                    prt = slice(half * 64, half * 64 + 64)
                    S.dma("sp", kch[gb], lambda e, prt=prt, g=g, gb=gb: e.dma_start(out=KDp[gb][prt, 0:NCTX], in_=self.kcT[g * 64:(g + 1) * 64, :]),
                          writes=[("KD", gb)])
                    for r in range(2):
                        S.dma("sp", kch[gb], lambda e, prt=prt, g=g, gb=gb, r=r: e.dma_start(
                            out=KDp[gb][prt, NCTX + r * NT:NCTX + (r + 1) * NT], in_=self.kT_all[r * 256 + g * 64:r * 256 + (g + 1) * 64, :]),
                            writes=[("KD", gb)])
                S.dma("sp", kch[gb], lambda e, g=g, gb=gb: e.dma_start(
                    out=VA[gb][:, 0:2, 0:64], in_=self.vc[:, g * 64:(g + 1) * 64].rearrange("(c p) f -> p c f", p=128)), writes=[("VA", gb)])
                S.dma("sp", kch[gb], lambda e, g=g, gb=gb: e.dma_start(
                    out=VA[gb][:, 2:nkc, 0:64], in_=self.v_all[:, g * 64:(g + 1) * 64].rearrange("(c p) f -> p c f", p=128)), writes=[("VA", gb)])
                for hh in range(4):
                    h = g * 4 + hh
                    ch, hb = h // 2, 64 * (h % 2)
                    for t0 in range(0, NT, 512):
                        ob = 6 + (cnt["o"] % 2)
                        rb = cnt["o"] % 2
                        cnt["o"] += 1

                        def s_mm(kc):
                            sbk = cnt["s"] % 5
                            cnt["s"] += 1
                            S.op("pe", lambda e, kc=kc, sbk=sbk, gb=gb, hb=hb, ch=ch, t0=t0: e.matmul(
                                ps[sbk][:], lhsT=KDp[gb][hb:hb + 64, kc * 128:(kc + 1) * 128], rhs=self.QT[hb:hb + 64, ch, t0:t0 + 512],
                                start=True, stop=True), reads=[("KD", gb), ("QT", t0)], writes=[("ps", sbk)])
                            S.op("act", lambda e, sbk=sbk: e.activation(out=Pm[sbk][:], in_=ps[sbk][:], func=AF.Exp, scale=0.125),
                                 reads=[("ps", sbk)], writes=[("Pm", sbk)])
                            return sbk

                        def pv_mm(kc, sbk):
                            S.op("pe", lambda e, kc=kc, sbk=sbk, ob=ob, gb=gb: e.matmul(ps[ob][:], lhsT=VA[gb][:, kc, :], rhs=Pm[sbk][:],
                                                                         start=(kc == 0), stop=(kc == nkc - 1)),
                                 reads=[("VA", gb), ("Pm", sbk)], writes=[("ps", ob)])
                        la = 3
                        sbks = [s_mm(kc) for kc in range(min(la, nkc))]
                        for kc in range(nkc):
                            if kc + la < nkc:
                                sbks.append(s_mm(kc + la))
                            pv_mm(kc, sbks[kc])
                        S.op("dve", lambda e, ob=ob, rb=rb: e.reciprocal(out=rinv[rb][:], in_=ps[ob][64:128, :]),
                             reads=[("ps", ob)], writes=[("rinv", rb)])
                        S.op("dve", lambda e, ob=ob, rb=rb, ch=ch, hb=hb, t0=t0: e.tensor_tensor(
                            out=OT[hb:hb + 64, ch, t0:t0 + 512], in0=ps[ob][0:64, :], in1=rinv[rb][:], op=ALU.mult),
                            reads=[("ps", ob), ("rinv", rb)], writes=[("H", t0)])

    def oproj(self):
        ps = self.ps
        NT = self.NT
        wo = self.din("b_w_o_0", [D, D])
        with self.phase() as (S, sb):
            WO = sb("WO", [128, KD, D], BF16)
            c0 = S.new_chan()
            for i in range(2):
                S.dma("pool", c0, lambda e, i=i: e.dma_start(out=WO[:, :, i * 512:(i + 1) * 512],
                                                            in_=wo[:, i * 512:(i + 1) * 512].rearrange("(k p) n -> p k n", p=128)),
                      writes=[("WO", i)])
            for t0 in range(0, NT, 512):
                for dc in range(KD):
                    pb = dc % 2
                    for c in range(KD):
                        S.op("pe", lambda e, c=c, dc=dc, pb=pb, t0=t0: e.matmul(ps[pb][:], lhsT=WO[:, c, dc * 128:(dc + 1) * 128],
                                                                              rhs=self.H[:, c, t0:t0 + 512], start=(c == 0), stop=(c == KD - 1)),
                             reads=[("WO", dc // 4), ("H", t0)], writes=[("ps", pb)])
                    S.op("dve", lambda e, dc=dc, pb=pb, t0=t0: e.scalar_tensor_tensor(
                        out=self.X[:, dc, t0:t0 + 512], in0=ps[pb][:], scalar=self.modc(1, 2, dc, 0), in1=self.X[:, dc, t0:t0 + 512],
                        op0=ALU.mult, op1=ALU.add), reads=[("ps", pb), ("X", "X", t0)], writes=[("X", "X", t0)])

    def pool_norm_halo(self, store_only=False):
        NT = self.NT
        halo = self.dout("halo_mine", [D, 16]) if self.mode != "F" else self.dram["halo_mine"]
        with contextlib.ExitStack() as es1:
            HPA = self.sb(es1, "HPA", [128, KD, 512], F32)
            HPB = self.sb(es1, "HPB", [128, KD, 512], F32)
            self.norm(2, 0, [(0, 512, "X")], out32=HPA, out32_off=0)
            self.norm(2, 0, [(NT - 512, 512, "X")], out32=HPB, out32_off=-(NT - 512))
            with self.phase() as (S, sb):
                c0 = S.new_chan()
                hv = halo.rearrange("(k p) n -> p k n", p=128)
                S.dma("sp", c0, lambda e: e.dma_start(out=hv[:, :, 0:8], in_=HPA[:, :, 0:8]))
                S.dma("sp", c0, lambda e: e.dma_start(out=hv[:, :, 8:16], in_=HPB[:, :, 504:512]))

    def pool_mixer(self):
        ps = self.ps
        NT = self.NT
        lat = [(t0, 512, "X") for t0 in range(0, NT, 512)]
        pw = self.din("p_w_0", [4, 256, 256])
        pscale = self.din("p_scale_0", [1, D])
        flags = self.din("flags", [128, 2])
        halo_all = self.halo_all if self.mode == "F" else self.din("halo_all", [2 * D, 16])
        W2 = NT + 16
        li2 = self.layers.index(2)
        with contextlib.ExitStack() as es1:
            with self.phase() as (S, sb):
                RSTD = sb("RSTD", [128, NT], F32)
                sq = [sb(f"psq{i}", [128, KD, 512], BF16) for i in range(2)]
                HPk = [sb(f"HPk{i}", [128, W2], F32) for i in range(2)]
                TA = sb("TA", [128, W2], F32)
                TB = sb("TB", [128, W2], F32)
                ON = sb("ON", [128, W2], F32)
                RC = sb("RC", [128, NT], F32)
                FL = sb("FL", [128, 2], F32)
                HL = sb("HL", [128, KD, 16], F32)
                PW = sb("PW", [128, 4, 2, 256], BF16)
                PSR = sb("PSR", [1, D], F32)
                PSC = sb("PSC", [128, KD], F32)
                PG = sb("PG", [128, KD], F32)
                for ti, t0 in enumerate(range(0, NT, 512)):
                    b = ti % 2
                    S.op("act", lambda e, b=b, t0=t0: e.activation(out=sq[b][:], in_=self.X[:, :, t0:t0 + 512], func=AF.Square),
                         reads=[("X", "X", t0)], writes=[("sq", b)])
                    for k in range(KD):
                        S.op("pe", lambda e, b=b, k=k: e.matmul(ps[2 + b][:], lhsT=self.ones_b[:], rhs=sq[b][:, k, :],
                                                                start=(k == 0), stop=(k == KD - 1)), reads=[("sq", b)], writes=[("ps", 2 + b)])
                    S.op("act", lambda e, b=b, t0=t0: e.activation(out=RSTD[:, t0:t0 + 512], in_=ps[2 + b][:], func=AF.Sqrt, bias=self.epsc[:, 0:1], scale=1.0),
                         reads=[("ps", 2 + b)], writes=[("RSTD", t0)])
                    S.op("dve", lambda e, t0=t0: e.reciprocal(out=RSTD[:, t0:t0 + 512], in_=RSTD[:, t0:t0 + 512]),
                         reads=[("RSTD", t0)], writes=[("RSTD", t0)])
                c0 = S.new_chan()
                c1 = S.new_chan()
                S.dma("sp", c0, lambda e: e.dma_start(out=FL[:], in_=flags), writes=["FL"])
                S.dma("sp", c0, lambda e: e.dma_start(out=PSR[:], in_=pscale), writes=["PSR"])
                S.dma("sp", c0, lambda e: e.dma_start(out=HL[:, :, 0:8], in_=halo_all[0:D, 8:16].rearrange("(k p) n -> p k n", p=128)), writes=["HL"])
                S.dma("sp", c0, lambda e: e.dma_start(out=HL[:, :, 8:16], in_=halo_all[D:2 * D, 0:8].rearrange("(k p) n -> p k n", p=128)), writes=["HL"])
                S.dma("pool", c1, lambda e: e.dma_start(out=PW[:].rearrange("p j c n -> p (j c) n"),
                                                        in_=pw.rearrange("j (c p) n -> p (j c) n", p=128)), writes=["PW"])
                self.row_to_cols(S, PSR, KD, PSC, 7, "PSR", "PSC")
                S.op("dve", lambda e: e.tensor_tensor(out=PG[:], in0=PSC[:], in1=self.MODC[:, self.layers.index(2), 16:24, 0], op=ALU.mult),
                     reads=["PSC"], writes=["PG"])
                S.op("dve", lambda e: e.tensor_scalar(out=HL[:, :, 0:8], in0=HL[:, :, 0:8], scalar1=FL[:, 0:1], scalar2=None, op0=ALU.mult),
                     reads=["HL", "FL"], writes=["HL"])
                S.op("dve", lambda e: e.tensor_scalar(out=HL[:, :, 8:16], in0=HL[:, :, 8:16], scalar1=FL[:, 1:2], scalar2=None, op0=ALU.mult),
                     reads=["HL", "FL"], writes=["HL"])
                S.op("dve", lambda e: e.memset(ON[:], 1.0), writes=["ON"])
                S.op("dve", lambda e: e.tensor_scalar(out=ON[:, 0:8], in0=ON[:, 0:8], scalar1=FL[:, 0:1], scalar2=None, op0=ALU.mult),
                     reads=["FL", "ON"], writes=["ON"])
                S.op("dve", lambda e: e.tensor_scalar(out=ON[:, NT + 8:NT + 16], in0=ON[:, NT + 8:NT + 16], scalar1=FL[:, 1:2], scalar2=None, op0=ALU.mult),
                     reads=["FL", "ON"], writes=["ON"])

                def wsum(srcf, j, eng, rname):
                    bufs = [TA, TB]
                    cur = srcf
                    lo, hi = 0, W2
                    steps = [(1, 0)] + [(2 ** (l - 1), 2 ** (l - 1)) for l in range(1, j + 1)]
                    for si, (a, b) in enumerate(steps):
                        dst = bufs[si % 2]
                        nlo, nhi = lo + a, hi - b
                        S.op(eng, lambda e, cur=cur, dst=dst, a=a, b=b, nlo=nlo, nhi=nhi: e.tensor_tensor(
                            out=dst[:, nlo:nhi], in0=cur[:, nlo - a:nhi - a], in1=cur[:, nlo + b:nhi + b], op=ALU.add),
                            reads=rname + ["TA", "TB"], writes=["TA" if si % 2 == 0 else "TB"])
                        cur, lo, hi = dst, nlo, nhi
                    return cur
                xall = [("X", "X", t0) for t0 in range(0, NT, 512)]
                for k in range(KD):
                    j = k // 2
                    hb = k % 2
                    if k % 2 == 0:
                        res = wsum(ON, j, "dve", ["ON"])
                        S.op("dve", lambda e, res=res: e.reciprocal(out=RC[:], in_=res[:, 8:8 + NT]), reads=["TA", "TB"], writes=["RC"])
                    hp = HPk[hb]
                    S.op("dve", lambda e, k=k, hp=hp: e.scalar_tensor_tensor(
                        out=hp[:, 8:8 + NT], in0=self.X[:, k, 0:NT], scalar=self.MODC[:, li2, 8 + k, 0:1], in1=RSTD[:],
                        op0=ALU.mult, op1=ALU.mult), reads=xall + [("RSTD", t0) for t0 in range(0, NT, 512)], writes=[("HPk", hb)])
                    S.op("act", lambda e, k=k, hp=hp: e.activation(out=hp[:, 8:8 + NT], in_=hp[:, 8:8 + NT], func=AF.Identity,
                                                                   bias=self.MODC[:, li2, k, 0:1], scale=1.0),
                         reads=[("HPk", hb)], writes=[("HPk", hb)])
                    S.op("act", lambda e, k=k, hp=hp: e.activation(out=hp[:, 0:8], in_=HL[:, k, 0:8], func=AF.Copy), reads=["HL"], writes=[("HPk", hb)])
                    S.op("act", lambda e, k=k, hp=hp: e.activation(out=hp[:, NT + 8:NT + 16], in_=HL[:, k, 8:16], func=AF.Copy), reads=["HL"], writes=[("HPk", hb)])
                    res = wsum(hp, j, "dve", [("HPk", hb)])
                    S.op("dve", lambda e, res=res: e.tensor_tensor(out=res[:, 8:8 + NT], in0=res[:, 8:8 + NT], in1=RC[:], op=ALU.mult),
                         reads=["TA", "TB", "RC"], writes=["TA", "TB"])
                    S.op("dve", lambda e, k=k, res=res, hp=hp: e.tensor_tensor(out=self.H[:, k, 0:NT], in0=res[:, 8:8 + NT], in1=hp[:, 8:8 + NT], op=ALU.subtract),
                         reads=["TA", "TB", ("HPk", hb)], writes=[("H", t0) for t0 in range(0, NT, 512)])
                for t0 in range(0, NT, 512):
                    for dc in range(KD):
                        j, m = dc // 2, dc % 2
                        pb = dc % 2
                        for kk in range(2):
                            S.op("pe", lambda e, j=j, m=m, kk=kk, pb=pb, t0=t0: e.matmul(
                                ps[pb][:], lhsT=PW[:, j, kk, m * 128:(m + 1) * 128], rhs=self.H[:, 2 * j + kk, t0:t0 + 512],
                                start=(kk == 0), stop=(kk == 1)), reads=["PW", ("H", t0)], writes=[("ps", pb)])
                        S.op("dve", lambda e, dc=dc, pb=pb, t0=t0: e.scalar_tensor_tensor(
                            out=self.X[:, dc, t0:t0 + 512], in0=ps[pb][:], scalar=PG[:, dc:dc + 1], in1=self.X[:, dc, t0:t0 + 512],
                            op0=ALU.mult, op1=ALU.add), reads=[("ps", pb), "PG", ("X", "X", t0)], writes=[("X", "X", t0)])


def _consts(NT, hf):
    ident = np.eye(128, dtype=np.float32)
    blk = np.zeros((128, 128), np.float32)
    blk[:64, :64] = 1.0 / 64
    blk[64:, 64:] = 1.0 / 64
    rot = np.zeros((128, 128), np.float32)
    for p in range(128):
        d = p % 64
        b = (d // 16) % 2
        if b == 0:
            rot[p + 16, p] = -1.0
        else:
            rot[p - 16, p] = 1.0
    pos = hf * NT + np.arange(NT)
    r, c = pos // GRID_W, pos % GRID_W
    inv = (10000.0 ** (-np.arange(16, dtype=np.float32) / 16)).astype(np.float32)
    ang = np.stack([r, c], -1).astype(np.float32)[..., None] * inv
    cosT = np.zeros((128, NT), np.float32)
    sinT = np.zeros((128, NT), np.float32)
    for p in range(128):
        d = p % 64
        a, f = d // 32, d % 16
        cosT[p] = np.cos(ang[:, a, f])
        sinT[p] = np.sin(ang[:, a, f])
    flags = np.zeros((128, 2), np.float32)
    flags[:, 0] = 1.0 if hf == 1 else 0.0
    flags[:, 1] = 1.0 if hf == 0 else 0.0
    return dict(ident=ident, blk64=blk, rotm=rot, cosT=cosT, sinT=sinT, flags=flags)


_NC_CACHE = {}


def _get_nc(cfg, mode):
    key = (tuple(sorted(cfg.items())), mode)
    if key not in _NC_CACHE:
        b = Builder(cfg, mode)
        b.build()
        _NC_CACHE[key] = b
    return _NC_CACHE[key]


def _run(cfg, mode, in_maps):
    b = _get_nc(cfg, mode)
    maps = []
    for m in in_maps:
        maps.append({k: np.ascontiguousarray(m[k]) for k in b.input_names})
    res = run_bass_kernel_spmd(b.nc, maps, core_ids=list(range(len(maps))))
    return res.results


def kernel_impl(cfg, inp):
    NT = cfg["NT"]
    SEQ = 2 * NT
    x = np.asarray(inp["x"])
    B = x.shape[0]
    ncores = 2 * B
    w = {k: np.asarray(v) for k, v in inp.items()}
    common = []
    for core in range(ncores):
        b, hf = core // 2, core % 2
        m = dict(_consts(NT, hf))
        m["crow"] = np.stack([w["c"][b], w["c_ctx"]], 0)
        for i in range(4):
            m[f"ada_w_{i}"] = w["ada_w"][i]
            m[f"ada_b_{i}"] = w["ada_b"][i][None, :]
            m[f"norm_g_{i}"] = w["norm_g"][i].reshape(1, 2 * D)
        for j in range(2):
            m[f"a_w_in_{j}"] = w["a_w_in"][j]
            m[f"a_v_g_{j}"] = w["a_v_g"][j][None, :]
            m[f"a_ws_{j}"] = w["a_ws"][j]
            m[f"a_bs_{j}"] = w["a_bs"][j].reshape(1, 1024)
            m[f"a_w_out_{j}"] = w["a_w_out"][j]
            m[f"f_w_gate_{j}"] = w["f_w_gate"][j]
            m[f"f_w_up_{j}"] = w["f_w_up"][j]
            m[f"f_w_down_{j}"] = w["f_w_down"][j]
            m[f"m_router_{j}"] = w["m_router"][j]
            m[f"m_w_gate_{j}"] = w["m_w_gate"][j]
            m[f"m_w_up_{j}"] = w["m_w_up"][j]
            m[f"m_w_down_{j}"] = w["m_w_down"][j]
        m["b_w_qkv_0"] = w["b_w_qkv"][0]
        m["b_q_g_0"] = w["b_q_g"][0].reshape(64, 1)
        m["b_k_g_0"] = w["b_k_g"][0].reshape(64, 1)
        m["b_w_o_0"] = w["b_w_o"][0]
        m["p_w_0"] = w["p_w"][0]
        m["p_scale_0"] = w["p_scale"][0][None, :]
        m["xT"] = x[b, hf * NT:(hf + 1) * NT, :].T
        m["ctxT"] = w["ctx"][b].T
        common.append(m)
    if cfg.get("fused", True):
        rf = _run(cfg, "F", common)
        out = np.empty((B, SEQ, D), np.float32)
        for core in range(ncores):
            b, hf = core // 2, core % 2
            out[b, hf * NT:(hf + 1) * NT, :] = rf[core]["yT"].T
        return out
    ra = _run(cfg, "A", common)
    for core in range(ncores):
        pair = [ra[(core // 2) * 2], ra[(core // 2) * 2 + 1]]
        m = common[core]
        m["xT"] = ra[core]["xT_out"]
        m["kT_all"] = np.concatenate([pair[0]["kT_mine"], pair[1]["kT_mine"]], 0)
        m["v_all"] = np.concatenate([pair[0]["v_mine"], pair[1]["v_mine"]], 0)
        m["kcT"] = ra[core]["kcT"]
        m["vc"] = ra[core]["vc"]
    rb = _run(cfg, "B", common)
    for core in range(ncores):
        pair = [rb[(core // 2) * 2], rb[(core // 2) * 2 + 1]]
        m = common[core]
        m["xT"] = rb[core]["xT_out"]
        m["halo_all"] = np.concatenate([pair[0]["halo_mine"], pair[1]["halo_mine"]], 0)
    rc = _run(cfg, "C", common)
    out = np.empty((B, SEQ, D), np.float32)
    for core in range(ncores):
        b, hf = core // 2, core % 2
        out[b, hf * NT:(hf + 1) * NT, :] = rc[core]["yT"].T
    return out


def kernel(**inputs):
    return kernel_impl(FULL_CFG, inputs)
```
